# Optimizing a Trainium2 kernel written in Bass

```python
import jax, jax.numpy as jnp
from jax import lax
import numpy as np

D_MODEL = 1024
BATCH = 4
SEQ = 4096
DEPTH = 2

CHUNK = 64

D_A = 512
CONV_A_WIDTH = 31
D_B = 512
POOL_WINDOWS = (2, 4, 8, 16)
POOL_GROUPS = len(POOL_WINDOWS)
POOL_GROUP_DIM = D_B // POOL_GROUPS
D_MIX_EVEN = D_A + D_B
IN_EVEN = 2 * D_A + D_B

D_C = 512
CONV_C_WIDTH = 3
D_D = 512
SGU_BLOCK = 128
SGU_HEADS = 4
SGU_HEAD_DIM = D_D // SGU_HEADS
D_MIX_ODD = D_C + D_D
IN_ODD = 3 * D_C + 2 * D_D

D_FF_DENSE = 2816
N_EXPERTS = 8
TOP_K = 2
D_FF_EXPERT = 3584

ALPHA = (2 * DEPTH) ** 0.25
BETA = (8 * DEPTH) ** -0.25
LN_EPS = 1e-5

N_EVEN = (DEPTH + 1) // 2
N_ODD = DEPTH // 2

kernel_name = "hybrid_conv_pool_shortconv_sgu_moe_deepnorm"


def layer_norm(x, g, b):
    xf = x.astype(jnp.float32)
    mu = jnp.mean(xf, axis=-1, keepdims=True)
    var = jnp.mean(jnp.square(xf - mu), axis=-1, keepdims=True)
    y = (xf - mu) * lax.rsqrt(var + LN_EPS)
    return (y * g.astype(jnp.float32) + b.astype(jnp.float32)).astype(x.dtype)


def causal_depthwise_conv(x, w):
    k = w.shape[0]
    return lax.conv_general_dilated(
        x, w[:, None, :], window_strides=(1,), padding=((k - 1, 0),),
        dimension_numbers=("NWC", "WIO", "NWC"), feature_group_count=x.shape[-1])


def swiglu(x, w_gate, w_up, w_down):
    return (jax.nn.silu(x @ w_gate) * (x @ w_up)) @ w_down


def conformer_conv(a_in, conv_w, conv_b, norm_g, norm_b):
    val, gate = jnp.split(a_in, 2, axis=-1)
    h = val * jax.nn.sigmoid(gate)
    h = causal_depthwise_conv(h, conv_w) + conv_b
    h = layer_norm(h, norm_g, norm_b)
    return jax.nn.silu(h)


def multiscale_pool(b_in, group_w, scale):
    bsz, s, _ = b_in.shape
    xg = b_in.astype(jnp.float32).reshape(bsz, s, POOL_GROUPS, POOL_GROUP_DIM)
    csum = jnp.cumsum(xg, axis=1)
    pos = jnp.arange(s)
    outs = []
    for g, win in enumerate(POOL_WINDOWS):
        cs = csum[:, :, g]
        lag = jnp.pad(cs, ((0, 0), (win, 0), (0, 0)))[:, :s]
        cnt = jnp.minimum(pos + 1, win).astype(jnp.float32)[None, :, None]
        outs.append((cs - lag) / cnt - xg[:, :, g])
    pooled = jnp.stack(outs, axis=2).astype(b_in.dtype)
    mixed = jnp.einsum("bsgc,gcd->bsgd", pooled, group_w).reshape(bsz, s, D_B)
    return mixed * scale


def short_gated_conv(c_in, conv_w):
    gate_b, gate_c, v = jnp.split(c_in, 3, axis=-1)
    return gate_b * causal_depthwise_conv(gate_c * v, conv_w)


def spatial_gating(d_in, norm_g, norm_b, w_s, b_s):
    z = jax.nn.gelu(d_in)
    u, v = jnp.split(z, 2, axis=-1)
    v = layer_norm(v, norm_g, norm_b)
    bsz, s, _ = v.shape
    nb = s // SGU_BLOCK
    v = v.reshape(bsz, nb, SGU_BLOCK, SGU_HEADS, SGU_HEAD_DIM)
    mask = jnp.tril(jnp.ones((SGU_BLOCK, SGU_BLOCK), dtype=bool))
    w = jnp.where(mask[None], w_s, jnp.zeros_like(w_s))
    mixed = jnp.einsum("hij,bnjhc->bnihc", w, v) + b_s.T[None, None, :, :, None]
    return u * mixed.reshape(bsz, s, D_D)


def moe_swiglu(x, router, w_gate, w_up, w_down):
    bsz, s, d = x.shape
    t = x.reshape(-1, d)
    logits = (t @ router).astype(jnp.float32)
    top_logits, top_idx = lax.top_k(logits, TOP_K)
    top_w = jax.nn.softmax(top_logits, axis=-1)
    gates = jnp.sum(jax.nn.one_hot(top_idx, N_EXPERTS, dtype=jnp.float32) * top_w[..., None], axis=1)
    gates = gates.astype(t.dtype)
    out = jnp.zeros_like(t)
    for e in range(N_EXPERTS):
        out = out + gates[:, e:e + 1] * swiglu(t, w_gate[e], w_up[e], w_down[e])
    return out.reshape(bsz, s, d)


def even_layer(x, w_in, conv_a_w, conv_a_b, norm_a_g, norm_a_b, pool_w, pool_scale, w_out,
               ln1_g, ln1_b, ffn_w_gate, ffn_w_up, ffn_w_down, ln2_g, ln2_b):
    h = x @ w_in
    y_a = conformer_conv(h[..., :2 * D_A], conv_a_w, conv_a_b, norm_a_g, norm_a_b)
    y_b = multiscale_pool(h[..., 2 * D_A:], pool_w, pool_scale)
    mix = jnp.concatenate([y_a, y_b], axis=-1) @ w_out
    x = layer_norm(ALPHA * x + mix, ln1_g, ln1_b)
    x = layer_norm(ALPHA * x + swiglu(x, ffn_w_gate, ffn_w_up, ffn_w_down), ln2_g, ln2_b)
    return x


def odd_layer(x, w_in, conv_c_w, sgu_norm_g, sgu_norm_b, sgu_w, sgu_b, w_out,
              ln1_g, ln1_b, router, moe_w_gate, moe_w_up, moe_w_down, ln2_g, ln2_b):
    h = x @ w_in
    y_c = short_gated_conv(h[..., :3 * D_C], conv_c_w)
    y_d = spatial_gating(h[..., 3 * D_C:], sgu_norm_g, sgu_norm_b, sgu_w, sgu_b)
    mix = jnp.concatenate([y_c, y_d], axis=-1) @ w_out
    x = layer_norm(ALPHA * x + mix, ln1_g, ln1_b)
    x = layer_norm(ALPHA * x + moe_swiglu(x, router, moe_w_gate, moe_w_up, moe_w_down), ln2_g, ln2_b)
    return x


def setup_inputs(seed: int = 0) -> dict:
    key = jax.random.key(seed)
    ks = jax.random.split(key, 32)
    f32 = jnp.float32

    def nrm(k, shape, scale):
        return jax.random.normal(k, shape, f32) * scale

    def gain(k, shape):
        return 1.0 + 0.01 * jax.random.normal(k, shape, f32)

    d = D_MODEL
    return {
        "x": nrm(ks[0], (BATCH, SEQ, d), 1.0),
        "even_w_in": nrm(ks[1], (N_EVEN, d, IN_EVEN), d ** -0.5),
        "even_conv_a_w": nrm(ks[2], (N_EVEN, CONV_A_WIDTH, D_A), CONV_A_WIDTH ** -0.5),
        "even_conv_a_b": nrm(ks[3], (N_EVEN, D_A), 0.01),
        "even_norm_a_g": gain(ks[4], (N_EVEN, D_A)),
        "even_norm_a_b": nrm(ks[5], (N_EVEN, D_A), 0.01),
        "even_pool_w": nrm(ks[6], (N_EVEN, POOL_GROUPS, POOL_GROUP_DIM, POOL_GROUP_DIM), POOL_GROUP_DIM ** -0.5),
        "even_pool_scale": 1.0 + 0.05 * jax.random.normal(ks[7], (N_EVEN, D_B), f32),
        "even_w_out": nrm(ks[8], (N_EVEN, D_MIX_EVEN, d), D_MIX_EVEN ** -0.5 * BETA),
        "even_ln1_g": gain(ks[9], (N_EVEN, d)),
        "even_ln1_b": nrm(ks[10], (N_EVEN, d), 0.01),
        "even_ffn_w_gate": nrm(ks[11], (N_EVEN, d, D_FF_DENSE), d ** -0.5),
        "even_ffn_w_up": nrm(ks[12], (N_EVEN, d, D_FF_DENSE), d ** -0.5),
        "even_ffn_w_down": nrm(ks[13], (N_EVEN, D_FF_DENSE, d), D_FF_DENSE ** -0.5 * BETA),
        "even_ln2_g": gain(ks[14], (N_EVEN, d)),
        "even_ln2_b": nrm(ks[15], (N_EVEN, d), 0.01),
        "odd_w_in": nrm(ks[16], (N_ODD, d, IN_ODD), d ** -0.5),
        "odd_conv_c_w": nrm(ks[17], (N_ODD, CONV_C_WIDTH, D_C), CONV_C_WIDTH ** -0.5),
        "odd_sgu_norm_g": gain(ks[18], (N_ODD, D_D)),
        "odd_sgu_norm_b": nrm(ks[19], (N_ODD, D_D), 0.01),
        "odd_sgu_w": nrm(ks[20], (N_ODD, SGU_HEADS, SGU_BLOCK, SGU_BLOCK), SGU_BLOCK ** -0.5),
        "odd_sgu_b": gain(ks[21], (N_ODD, SGU_HEADS, SGU_BLOCK)),
        "odd_w_out": nrm(ks[22], (N_ODD, D_MIX_ODD, d), D_MIX_ODD ** -0.5 * BETA),
        "odd_ln1_g": gain(ks[23], (N_ODD, d)),
        "odd_ln1_b": nrm(ks[24], (N_ODD, d), 0.01),
        "odd_router": nrm(ks[25], (N_ODD, d, N_EXPERTS), d ** -0.5),
        "odd_moe_w_gate": nrm(ks[26], (N_ODD, N_EXPERTS, d, D_FF_EXPERT), d ** -0.5),
        "odd_moe_w_up": nrm(ks[27], (N_ODD, N_EXPERTS, d, D_FF_EXPERT), d ** -0.5),
        "odd_moe_w_down": nrm(ks[28], (N_ODD, N_EXPERTS, D_FF_EXPERT, d), D_FF_EXPERT ** -0.5 * BETA),
        "odd_ln2_g": gain(ks[29], (N_ODD, d)),
        "odd_ln2_b": nrm(ks[30], (N_ODD, d), 0.01),
    }


def reference(x, even_w_in, even_conv_a_w, even_conv_a_b, even_norm_a_g, even_norm_a_b,
              even_pool_w, even_pool_scale, even_w_out, even_ln1_g, even_ln1_b,
              even_ffn_w_gate, even_ffn_w_up, even_ffn_w_down, even_ln2_g, even_ln2_b,
              odd_w_in, odd_conv_c_w, odd_sgu_norm_g, odd_sgu_norm_b, odd_sgu_w, odd_sgu_b,
              odd_w_out, odd_ln1_g, odd_ln1_b, odd_router, odd_moe_w_gate, odd_moe_w_up,
              odd_moe_w_down, odd_ln2_g, odd_ln2_b):
    for layer in range(DEPTH):
        i = layer // 2
        if layer % 2 == 0:
            x = even_layer(x, even_w_in[i], even_conv_a_w[i], even_conv_a_b[i], even_norm_a_g[i],
                           even_norm_a_b[i], even_pool_w[i], even_pool_scale[i], even_w_out[i],
                           even_ln1_g[i], even_ln1_b[i], even_ffn_w_gate[i], even_ffn_w_up[i],
                           even_ffn_w_down[i], even_ln2_g[i], even_ln2_b[i])
        else:
            x = odd_layer(x, odd_w_in[i], odd_conv_c_w[i], odd_sgu_norm_g[i], odd_sgu_norm_b[i],
                          odd_sgu_w[i], odd_sgu_b[i], odd_w_out[i], odd_ln1_g[i], odd_ln1_b[i],
                          odd_router[i], odd_moe_w_gate[i], odd_moe_w_up[i], odd_moe_w_down[i],
                          odd_ln2_g[i], odd_ln2_b[i])
    return x
```

```python
import contextlib
import numpy as np
import concourse.bass as bass
import concourse.mybir as mybir
from concourse.bass_utils import run_bass_kernel_spmd

F32 = mybir.dt.float32
BF16 = mybir.dt.bfloat16
AF = mybir.ActivationFunctionType
ALU = mybir.AluOpType
AX = mybir.AxisListType

PE, ACT, DVE, POOL, SP = "tensor", "scalar", "vector", "gpsimd", "sync"
ENGS = [PE, ACT, DVE, POOL, SP]

HALO = 32
T = 2048
NT = HALO + T
D = 1024
ALPHA = 4.0 ** 0.25
EPS = 1e-5
FF_DENSE = 2816
FF_EXP = 3584
NEXP = 8


class Buf:
    __slots__ = ("name", "writer", "readers")

    def __init__(self, name=""):
        self.name = name
        self.writer = None
        self.readers = []


class Op:
    __slots__ = ("eng", "fn", "deps", "dma", "sem", "val", "signal")

    def __init__(self, eng, fn, dma):
        self.eng = eng
        self.fn = fn
        self.deps = []
        self.dma = dma
        self.sem = None
        self.val = 0
        self.signal = False


class Sched:
    def __init__(self, nc):
        self.nc = nc
        self.ops = {e: [] for e in ENGS}
        self.all_ops = []

    def add(self, eng, fn, reads=(), writes=(), dma_key=None):
        op = Op(eng, fn, dma_key)
        deps = []
        for r in reads:
            if r.writer is not None:
                deps.append(r.writer)
        for w in writes:
            if w.writer is not None:
                deps.append(w.writer)
            deps.extend(w.readers)
        seen = set()
        for d in deps:
            if d is op or id(d) in seen:
                continue
            seen.add(id(d))
            if d.eng == PE and eng == PE and d.dma is None and dma_key is None:
                continue
            op.deps.append(d)
            d.signal = True
        for r in reads:
            r.readers.append(op)
        for w in writes:
            w.writer = op
            w.readers = []
        self.ops[eng].append(op)
        self.all_ops.append(op)
        return op

    def last_ops(self):
        return [self.ops[e][-1] for e in ENGS if self.ops[e]]

    def emit(self, final_wait_ops=()):
        nc = self.nc
        eng_cnt = {e: 0 for e in ENGS}
        dma_keys = {}
        for op in self.all_ops:
            if op.dma is not None:
                ent = dma_keys.setdefault(op.dma, [len(dma_keys), 0])
                ent[1] += 16
                op.sem = ("dma", ent[0])
                op.val = ent[1]
                op.signal = True
            elif op.signal:
                eng_cnt[op.eng] += 1
                op.sem = ("eng", op.eng)
                op.val = eng_cnt[op.eng]
        with contextlib.ExitStack() as st:
            sems = {}
            for e in ENGS:
                sems[("eng", e)] = st.enter_context(nc.semaphore("s_" + e))
            for i in range(len(dma_keys)):
                sems[("dma", i)] = st.enter_context(nc.semaphore("d_%d" % i))
            block = st.enter_context(nc.Block())
            for e in ENGS:
                ops = self.ops[e]

                def body(engobj, ops=ops, e=e):
                    waited = {}
                    for op in ops:
                        for d in op.deps:
                            if waited.get(d.sem, 0) >= d.val:
                                continue
                            waited[d.sem] = d.val
                            engobj.wait_ge(sems[d.sem], d.val)
                        ins = op.fn(engobj)
                        if op.signal:
                            ins.then_inc(sems[op.sem], 16 if op.dma is not None else 1)
                    if e == SP:
                        for op in final_wait_ops:
                            if waited.get(op.sem, 0) >= op.val:
                                continue
                            waited[op.sem] = op.val
                            engobj.wait_ge(sems[op.sem], op.val)

                getattr(block, e)(body)


def sub_rows(s):
    return HALO if s == 0 else 128


def sub_col0(s):
    return 0 if s == 0 else HALO + 128 * (s - 1)


def build_nc(n_sub=4, dbg=False):
    nc = bass.Bass("TRN2", target_bir_lowering=False)

    def din(name, shape):
        return nc.dram_tensor(name, list(shape), F32, kind="ExternalInput").ap()

    x_c = din("x_c", [NT, D])
    e_w_in = din("e_w_in", [D, 1536])
    e_w_out = din("e_w_out", [D, D])
    e_wg = din("e_wg", [D, FF_DENSE])
    e_wu = din("e_wu", [D, FF_DENSE])
    e_wd = din("e_wd", [FF_DENSE, D])
    o_w_in = din("o_w_in", [D, 2560])
    o_w_out = din("o_w_out", [D, D])
    o_router = din("o_router", [D, NEXP])
    o_wg = din("o_wg", [NEXP, D, FF_EXP])
    o_wu = din("o_wu", [NEXP, D, FF_EXP])
    o_wd = din("o_wd", [NEXP, FF_EXP, D])
    lnrows = din("lnrows", [8, D])
    sgrows = din("sgrows", [3, 512])
    cvec_d = din("cvec", [128, 152])
    pool_w_d = din("pool_w", [4, 128, 128])
    sgu_w_d = din("sgu_w", [4, 128, 128])
    ident_d = din("ident", [128, 128])
    tril_d = din("tril", [128, 128])
    poolcorr_d = din("poolcorr", [128, 64])
    halomask_d = din("halomask", [128, 1])
    out_d = nc.dram_tensor("out", [T, D], F32, kind="ExternalOutput").ap()

    st = contextlib.ExitStack()
    with st:
        S = Sched(nc)

        def sb(name, shape, dt):
            return st.enter_context(nc.sbuf_tensor(name, list(shape), dt))

        resid = sb("resid", [128, 17, D], F32)
        xT = sb("xT", [128, 8, NT], BF16)
        gbt = sb("gb", [128, 2 * D], F32)
        gb = gbt[:, :].rearrange("p (i d) -> p i d", d=D)
        xb = sb("xb", [128, 2, D], BF16)
        identf = sb("identf", [128, 128], F32)
        identb = sb("identb", [128, 128], BF16)
        onesf = sb("onesf", [128, 128], F32)
        onesb = sb("onesb", [1, 128], BF16)
        epst = sb("epst", [128, 1], F32)
        cvec = sb("cvec_s", [128, 152], F32)
        poolw = sb("poolw", [128, 4, 128], BF16)
        sguwf = gbt[:, D:D + 512].rearrange("p (h j) -> p h j", j=128)
        trilf = gbt[:, D + 512:D + 640]
        wmT = sb("wmT", [128, 4, 128], BF16)
        sgg = sb("sgg", [128, 2, 512], F32)
        bsrow = gbt[0:1, 0:1536].rearrange("p (a b) -> p a b", b=512)
        bshl = sb("bshl", [1, 2, 512], BF16)
        poolcorr = sb("poolcorr_s", [128, 64], F32)
        halomask = sb("halomask_s", [128, 1], F32)
        rtr = sb("rtr", [128, 8, NEXP], BF16)
        gates = sb("gates", [128, 16, NEXP], F32)
        stats = sb("stats", [128, 2, 2, 6], F32)
        mv = sb("mv", [128, 2, 2], F32)
        sd = sb("sd", [128, 2, 1], F32)
        rstd = sb("rstd", [128, 2, 1], F32)
        nb = sb("nb", [128, 2, 1], F32)
        gsm = sb("gsm", [128, 2, 48], F32)
        M_BYTES = 82432
        Mr = sb("Mr", [128, M_BYTES // 2], BF16)
        Mf = Mr.bitcast(F32)

        def mview(off_bytes, dt, n):
            if dt == F32:
                assert off_bytes % 4 == 0
                return Mf[:, off_bytes // 4: off_bytes // 4 + n]
            assert off_bytes % 2 == 0
            return Mr[:, off_bytes // 2: off_bytes // 2 + n]

        pT = st.enter_context(nc.psum_tensor("pT", [128, 8, 128], BF16))
        banks = [st.enter_context(nc.psum_tensor("bank%d" % i, [128, 512], F32)) for i in range(7)]
        bank_bufs = [Buf("bank%d" % i) for i in range(7)]
        pT_b = Buf("pT")
        ring = [0]

        def ps_alloc():
            i = ring[0] % 7
            ring[0] += 1
            return banks[i], bank_bufs[i]

        resid_b = [Buf("resid%d" % s) for s in range(17)]
        xT_b = [Buf("xT%d" % s) for s in range(17)]
        gb_b = [Buf("gbg"), Buf("gbb")]
        xb_b = [Buf("xb0"), Buf("xb1")]
        st_b = [Buf(), Buf()]
        mv_b = [Buf(), Buf()]
        sd_b = [Buf(), Buf()]
        rstd_b = [Buf(), Buf()]
        nb_b = [Buf(), Buf()]
        gsm_b = [Buf(), Buf()]
        const_b = Buf("consts")
        gates_b = [Buf("gates%d" % s) for s in range(16)]
        wmT_b = Buf("wmT")
        cnt = {"ln": 0, "x": 0}
        out_ops = []

        def cload(dst, src, eng=SP, bufs=None):
            key = ("c", cnt["x"])
            cnt["x"] += 1
            return S.add(eng, lambda e: e.dma_start(out=dst, in_=src), writes=(bufs if bufs is not None else [const_b]), dma_key=key)

        cload(identf[:], ident_d)
        cload(cvec[:], cvec_d)
        cload(trilf, tril_d, bufs=[gb_b[1]])
        cload(poolcorr[:], poolcorr_d)
        cload(halomask[:], halomask_d)
        cload(sguwf, sgu_w_d.rearrange("h i j -> i h j"), bufs=[gb_b[1]])
        cload(sgg[:, 0, :], sgrows[0:1, :].partition_broadcast(128))
        cload(sgg[:, 1, :], sgrows[1:2, :].partition_broadcast(128))
        cload(bsrow[:, 0, :], sgrows[2:3, :], bufs=[gb_b[0]])
        cload(poolw[:], pool_w_d.rearrange("g c d -> c g d"), eng=POOL)
        cload(rtr[:], o_router.rearrange("(k p) e -> p k e", p=128), eng=POOL)
        S.add(DVE, lambda e: e.memset(onesf[:], 1.0), writes=[const_b])
        S.add(DVE, lambda e: e.memset(onesb[:], 1.0), writes=[const_b])
        S.add(DVE, lambda e: e.memset(epst[:], EPS), writes=[const_b])
        S.add(DVE, lambda e: e.tensor_copy(out=identb[:], in_=identf[:]), reads=[const_b], writes=[const_b])
        S.add(DVE, lambda e: e.tensor_copy(out=bshl[:, 0, :], in_=bsrow[:, 0, :]), reads=[gb_b[0]], writes=[const_b])
        S.add(DVE, lambda e: e.tensor_copy(out=bsrow[:, 1, :], in_=bshl[:, 0, :]), reads=[const_b], writes=[gb_b[0]])
        S.add(DVE, lambda e: e.tensor_tensor(out=bsrow[:, 2, :], in0=bsrow[:, 0, :], in1=bsrow[:, 1, :], op=ALU.subtract), reads=[gb_b[0]], writes=[gb_b[0]])
        S.add(DVE, lambda e: e.tensor_copy(out=bshl[:, 1, :], in_=bsrow[:, 2, :]), reads=[gb_b[0]], writes=[const_b])
        for h in range(4):
            bk, bkb = ps_alloc()
            S.add(PE, lambda e, h=h, bk=bk: e.transpose(out=bk[:, 0:128], in_=sguwf[:, h, :], identity=identf[:]), reads=[const_b, gb_b[1]], writes=[bkb])
            S.add(DVE, lambda e, h=h, bk=bk: e.tensor_tensor(out=wmT[:, h, :], in0=bk[:, 0:128], in1=trilf, op=ALU.mult), reads=[bkb, gb_b[1]], writes=[wmT_b])

        def load_gb(k, final):
            for i in range(2):
                S.add(SP, lambda e, i=i: e.dma_start(out=gb[:, i, :], in_=lnrows[2 * k + i: 2 * k + i + 1, :].partition_broadcast(128)),
                      writes=[gb_b[i]], dma_key=("gb", i))
                if not final:
                    S.add(POOL, lambda e, i=i: e.tensor_scalar(out=gb[:, i, :], in0=gb[:, i, :], scalar1=ALPHA, scalar2=None, op0=ALU.mult),
                          reads=[gb_b[i]], writes=[gb_b[i]])

        def make_xT(s, par):
            rows, c0 = sub_rows(s), sub_col0(s)

            def tr(e):
                ins = None
                for c in range(8):
                    ins = e.transpose(out=pT[:, c, 0:rows], in_=xb[0:rows, par, c * 128:(c + 1) * 128], identity=identb[0:rows, 0:rows])
                return ins
            S.add(PE, tr, reads=[xb_b[par], const_b], writes=[pT_b])
            S.add(ACT, lambda e: e.copy(out=xT[:, :, c0:c0 + rows], in_=pT[:, :, 0:rows]), reads=[pT_b], writes=[xT_b[s]])

        def ln_update(s, final):
            rows = sub_rows(s)
            par = cnt["ln"] % 2
            cnt["ln"] += 1
            r = resid[0:rows, s, :]

            def bn(e):
                e.bn_stats(out=stats[0:rows, par, 0, :], in_=resid[0:rows, s, 0:512])
                return e.bn_stats(out=stats[0:rows, par, 1, :], in_=resid[0:rows, s, 512:1024])
            S.add(DVE, bn, reads=[resid_b[s]], writes=[st_b[par]])
            S.add(DVE, lambda e: e.bn_aggr(out=mv[0:rows, par, :], in_=stats[0:rows, par, :, :]), reads=[st_b[par]], writes=[mv_b[par]])
            S.add(ACT, lambda e: e.activation(out=sd[0:rows, par, :], in_=mv[0:rows, par, 1:2], func=AF.Sqrt, bias=epst[0:rows, 0:1], scale=1.0),
                  reads=[mv_b[par], const_b], writes=[sd_b[par]])
            S.add(DVE, lambda e: e.reciprocal(out=rstd[0:rows, par, :], in_=sd[0:rows, par, :]), reads=[sd_b[par]], writes=[rstd_b[par]])
            S.add(DVE, lambda e: e.scalar_tensor_tensor(out=nb[0:rows, par, :], in0=mv[0:rows, par, 0:1], scalar=-1.0, in1=rstd[0:rows, par, :],
                                                        op0=ALU.mult, op1=ALU.mult), reads=[mv_b[par], rstd_b[par]], writes=[nb_b[par]])
            S.add(ACT, lambda e: e.activation(out=r, in_=r, func=AF.Identity, bias=nb[0:rows, par, :], scale=rstd[0:rows, par, :]),
                  reads=[resid_b[s], nb_b[par], rstd_b[par]], writes=[resid_b[s]])
            S.add(DVE, lambda e: e.tensor_tensor(out=r, in0=r, in1=gb[0:rows, 0, :], op=ALU.mult), reads=[resid_b[s], gb_b[0]], writes=[resid_b[s]])
            S.add(DVE, lambda e: e.tensor_tensor(out=r, in0=r, in1=gb[0:rows, 1, :], op=ALU.add), reads=[resid_b[s], gb_b[1]], writes=[resid_b[s]])
            if final:
                if s >= 1:
                    op = S.add(SP, lambda e: e.dma_start(out=out_d[128 * (s - 1):128 * s, :], in_=r), reads=[resid_b[s]], dma_key=("out", s))
                    out_ops.append(op)
            else:
                S.add(ACT, lambda e: e.activation(out=xb[0:rows, par, :], in_=r, func=AF.Identity, scale=1.0 / ALPHA), reads=[resid_b[s]], writes=[xb_b[par]])
                make_xT(s, par)

        def wload(dst, src, wbufs, key):
            return S.add(POOL, lambda e: e.dma_start(out=dst, in_=src), writes=wbufs, dma_key=key)

        def fresh_bufs(n, barrier):
            bl = []
            for i in range(n):
                b = Buf()
                b.readers = list(barrier)
                bl.append(b)
            return bl

        for s in range(17):
            rows, c0 = sub_rows(s), sub_col0(s)
            par = s % 2
            S.add(SP, lambda e, s=s, rows=rows, c0=c0: e.dma_start(out=resid[0:rows, s, :], in_=x_c[c0:c0 + rows, :]), writes=[resid_b[s]], dma_key=("x", s))
            S.add(ACT, lambda e, s=s, rows=rows, par=par: e.copy(out=xb[0:rows, par, :], in_=resid[0:rows, s, :]), reads=[resid_b[s]], writes=[xb_b[par]])
            make_xT(s, par)
            S.add(POOL, lambda e, s=s, rows=rows: e.tensor_scalar(out=resid[0:rows, s, :], in0=resid[0:rows, s, :], scalar1=ALPHA, scalar2=None, op0=ALU.mult),
                  reads=[resid_b[s]], writes=[resid_b[s]])

        CW, CB, NG, NB_, PS, CC = 0, 124, 128, 132, 136, 140

        def out_proj(ycat, ycat_b, wout, wout_b, subs, loc0):
            for si, s in enumerate(subs):
                rows = sub_rows(s)
                loc = loc0 + 128 * si
                for half in range(2):
                    bk, bkb = ps_alloc()

                    def mm(e, bk=bk, rows=rows, loc=loc, half=half):
                        ins = None
                        for k in range(8):
                            ins = e.matmul(bk[0:rows, :], lhsT=ycat[:, k, loc:loc + rows], rhs=wout[:, k, half * 512:(half + 1) * 512],
                                           start=(k == 0), stop=(k == 7))
                        return ins
                    S.add(PE, mm, reads=[ycat_b, wout_b], writes=[bkb])
                    S.add(DVE, lambda e, bk=bk, rows=rows, s=s, half=half: e.tensor_tensor(
                        out=resid[0:rows, s, half * 512:(half + 1) * 512], in0=resid[0:rows, s, half * 512:(half + 1) * 512], in1=bk[0:rows, :], op=ALU.add),
                        reads=[bkb, resid_b[s]], writes=[resid_b[s]])

        def mixer_even(final):
            bar = []
            win_b, wout_b = fresh_bufs(2, bar)
            tmp_bar = bar
            win = mview(0, BF16, 8 * 1536).rearrange("p (k f) -> p k f", f=1536)
            wout = mview(40960, BF16, 8 * 1024).rearrange("p (k f) -> p k f", f=1024)
            for k0 in (0, 4):
                wload(win[:, k0:k0 + 4, :], e_w_in[k0 * 128:(k0 + 4) * 128, :].rearrange("(k p) f -> p k f", p=128), [win_b], ("win", k0))
            for k0 in (0, 4):
                wload(wout[:, k0:k0 + 4, :], e_w_out[k0 * 128:(k0 + 4) * 128, :].rearrange("(k p) f -> p k f", p=128), [wout_b], ("wout", k0))
            load_gb(0, final)
            W = 256
            offA = 24576
            aT = [mview(offA + i * 4576, F32, 4 * 286).rearrange("p (c t) -> p c t", t=286) for i in range(2)]
            acc = mview(offA + 9152, F32, 4 * W).rearrange("p (c t) -> p c t", t=W)
            sigt = [mview(offA + 13248 + i * 1024, F32, W) for i in range(2)]
            assert 15296 <= 16384
            offB = 57344
            sqt = mview(offB, F32, 4 * W).rearrange("p (c t) -> p c t", t=W)
            meant = mview(offB + 4096, F32, W)
            vart = mview(offB + 5120, F32, W)
            rstt = mview(offB + 6144, F32, W)
            hbT = [mview(offB + 7168 + i * 4336, F32, 4 * 271).rearrange("p (c t) -> p c t", t=271) for i in range(2)]
            tmpA = mview(offB + 15840, F32, 271)
            tmpB = mview(offB + 16924, F32, 271)
            pooled = mview(offB + 18008, BF16, 4 * W).rearrange("p (c t) -> p c t", t=W)
            ycat = mview(offB + 20056, BF16, 8 * W).rearrange("p (c t) -> p c t", t=W)
            assert 24152 <= 24576
            tb = lambda: fresh_bufs(1, tmp_bar)[0]
            aT_b = [[tb() for _ in range(4)] for _ in range(2)]
            acc_b = [tb() for _ in range(4)]
            sig_b = [tb(), tb()]
            sq_b = [tb() for _ in range(4)]
            mean_b, var_b, rst_b = tb(), tb(), tb()
            hb_b = [[tb() for _ in range(4)] for _ in range(2)]
            tA_b, tB_b = tb(), tb()
            pooled_b = [tb() for _ in range(4)]
            ycat_b = tb()
            S.add(DVE, lambda e: e.memset(aT[0][:, :, 0:30], 0.0), writes=aT_b[0])
            S.add(DVE, lambda e: e.memset(hbT[0][:, :, 0:15], 0.0), writes=hb_b[0])
            tiles = [(0, HALO, [0])] + [(HALO + W * i, W, [1 + 2 * i, 2 + 2 * i]) for i in range(8)]
            for ti, (c0, w, subs) in enumerate(tiles):
                cur, nxt = ti % 2, (ti + 1) % 2
                xr = [xT_b[s] for s in subs]
                for ch in range(4):
                    bk, bkb = ps_alloc()

                    def mm(e, bk=bk, ch=ch, c0=c0, w=w):
                        ins = None
                        for part, oc in ((0, ch), (1, 4 + ch)):
                            for k in range(8):
                                ins = e.matmul(bk[:, part * 256: part * 256 + w], lhsT=win[:, k, oc * 128:(oc + 1) * 128], rhs=xT[:, k, c0:c0 + w],
                                               start=(k == 0), stop=(k == 7))
                        return ins
                    S.add(PE, mm, reads=[win_b] + xr, writes=[bkb])
                    sp = ch % 2
                    S.add(ACT, lambda e, bk=bk, w=w, sp=sp: e.activation(out=sigt[sp][:, 0:w], in_=bk[:, 256:256 + w], func=AF.Sigmoid), reads=[bkb], writes=[sig_b[sp]])
                    S.add(DVE, lambda e, bk=bk, w=w, sp=sp, ch=ch, cur=cur: e.tensor_tensor(out=aT[cur][:, ch, 30:30 + w], in0=bk[:, 0:w], in1=sigt[sp][:, 0:w], op=ALU.mult),
                          reads=[bkb, sig_b[sp]], writes=[aT_b[cur][ch]])
                for gp in range(4):
                    if gp % 2 == 0:
                        bkB, bkBb = ps_alloc()

                    def mm(e, bk=bkB, gp=gp, c0=c0, w=w):
                        ins = None
                        for k in range(8):
                            ins = e.matmul(bk[:, (gp % 2) * 256:(gp % 2) * 256 + w], lhsT=win[:, k, (8 + gp) * 128:(9 + gp) * 128], rhs=xT[:, k, c0:c0 + w],
                                           start=(k == 0), stop=(k == 7))
                        return ins
                    S.add(PE, mm, reads=[win_b] + xr, writes=[bkBb])
                    S.add(ACT, lambda e, bk=bkB, gp=gp, w=w, cur=cur: e.copy(out=hbT[cur][:, gp, 15:15 + w], in_=bk[:, (gp % 2) * 256:(gp % 2) * 256 + w]),
                          reads=[bkBb], writes=[hb_b[cur][gp]])
                for j in range(31):
                    for ch in range(4):
                        if j == 0:
                            S.add(DVE, lambda e, ch=ch, w=w, cur=cur: e.tensor_scalar(
                                out=acc[:, ch, 0:w], in0=aT[cur][:, ch, 0:w], scalar1=cvec[:, CW + ch * 31: CW + ch * 31 + 1], scalar2=cvec[:, CB + ch: CB + ch + 1],
                                op0=ALU.mult, op1=ALU.add), reads=[aT_b[cur][ch], const_b], writes=[acc_b[ch]])
                        else:
                            S.add(DVE, lambda e, ch=ch, w=w, cur=cur, j=j: e.scalar_tensor_tensor(
                                out=acc[:, ch, 0:w], in0=aT[cur][:, ch, j:j + w], scalar=cvec[:, CW + ch * 31 + j: CW + ch * 31 + j + 1], in1=acc[:, ch, 0:w],
                                op0=ALU.mult, op1=ALU.add), reads=[aT_b[cur][ch], acc_b[ch]], writes=[acc_b[ch]])
                if ti + 1 < len(tiles):
                    S.add(POOL, lambda e, w=w, cur=cur, nxt=nxt: e.tensor_copy(out=aT[nxt][:, :, 0:30], in_=aT[cur][:, :, w:w + 30]),
                          reads=aT_b[cur], writes=aT_b[nxt])
                for ch in range(4):
                    S.add(ACT, lambda e, ch=ch, w=w: e.activation(out=sqt[:, ch, 0:w], in_=acc[:, ch, 0:w], func=AF.Square), reads=[acc_b[ch]], writes=[sq_b[ch]])
                bk, bkb = ps_alloc()

                def mmst(e, bk=bk, w=w):
                    ins = None
                    for ch in range(4):
                        ins = e.matmul(bk[:, 0:w], lhsT=onesf[:], rhs=acc[:, ch, 0:w], start=(ch == 0), stop=(ch == 3))
                    for ch in range(4):
                        ins = e.matmul(bk[:, 256:256 + w], lhsT=onesf[:], rhs=sqt[:, ch, 0:w], start=(ch == 0), stop=(ch == 3))
                    return ins
                S.add(PE, mmst, reads=acc_b + sq_b + [const_b], writes=[bkb])
                S.add(DVE, lambda e, bk=bk, w=w: e.tensor_scalar(out=meant[:, 0:w], in0=bk[:, 0:w], scalar1=1.0 / 512, scalar2=None, op0=ALU.mult), reads=[bkb], writes=[mean_b])
                S.add(DVE, lambda e, w=w: e.tensor_tensor(out=vart[:, 0:w], in0=meant[:, 0:w], in1=meant[:, 0:w], op=ALU.mult), reads=[mean_b], writes=[var_b])
                S.add(DVE, lambda e, bk=bk, w=w: e.scalar_tensor_tensor(out=vart[:, 0:w], in0=bk[:, 256:256 + w], scalar=1.0 / 512, in1=vart[:, 0:w], op0=ALU.mult, op1=ALU.subtract),
                      reads=[bkb, var_b], writes=[var_b])
                S.add(ACT, lambda e, w=w: e.activation(out=rstt[:, 0:w], in_=vart[:, 0:w], func=AF.Sqrt, bias=epst[:, 0:1], scale=1.0), reads=[var_b, const_b], writes=[rst_b])
                S.add(DVE, lambda e, w=w: e.reciprocal(out=rstt[:, 0:w], in_=rstt[:, 0:w]), reads=[rst_b], writes=[rst_b])
                for ch in range(4):
                    S.add(DVE, lambda e, ch=ch, w=w: e.tensor_tensor(out=acc[:, ch, 0:w], in0=acc[:, ch, 0:w], in1=meant[:, 0:w], op=ALU.subtract),
                          reads=[acc_b[ch], mean_b], writes=[acc_b[ch]])
                for ch in range(4):
                    S.add(DVE, lambda e, ch=ch, w=w: e.tensor_tensor(out=acc[:, ch, 0:w], in0=acc[:, ch, 0:w], in1=rstt[:, 0:w], op=ALU.mult),
                          reads=[acc_b[ch], rst_b], writes=[acc_b[ch]])
                for ch in range(4):
                    S.add(ACT, lambda e, ch=ch, w=w: e.activation(out=ycat[:, ch, 0:w], in_=acc[:, ch, 0:w], func=AF.Silu, bias=cvec[:, NB_ + ch: NB_ + ch + 1],
                                                                 scale=cvec[:, NG + ch: NG + ch + 1]), reads=[acc_b[ch], const_b], writes=[ycat_b])
                E = 15 + w
                for gp in range(4):
                    hsrc = hbT[cur]
                    S.add(DVE, lambda e, gp=gp, E=E, hsrc=hsrc: e.tensor_tensor(out=tmpA[:, 1:E], in0=hsrc[:, gp, 1:E], in1=hsrc[:, gp, 0:E - 1], op=ALU.add),
                          reads=[hb_b[cur][gp]], writes=[tA_b])
                    wsrc, wsb = tmpA, tA_b
                    if gp >= 1:
                        S.add(DVE, lambda e, E=E: e.tensor_tensor(out=tmpB[:, 3:E], in0=tmpA[:, 3:E], in1=tmpA[:, 1:E - 2], op=ALU.add), reads=[tA_b], writes=[tB_b])
                        wsrc, wsb = tmpB, tB_b
                    if gp >= 2:
                        S.add(DVE, lambda e, E=E: e.tensor_tensor(out=tmpA[:, 7:E], in0=tmpB[:, 7:E], in1=tmpB[:, 3:E - 4], op=ALU.add), reads=[tB_b], writes=[tA_b])
                        wsrc, wsb = tmpA, tA_b
                    if gp >= 3:
                        S.add(DVE, lambda e, E=E: e.tensor_tensor(out=tmpB[:, 15:E], in0=tmpA[:, 15:E], in1=tmpA[:, 7:E - 8], op=ALU.add), reads=[tA_b], writes=[tB_b])
                        wsrc, wsb = tmpB, tB_b
                    if ti == 1:
                        S.add(DVE, lambda e, gp=gp, wsrc=wsrc: e.tensor_tensor(out=wsrc[:, 15:31], in0=wsrc[:, 15:31], in1=poolcorr[:, gp * 16:(gp + 1) * 16], op=ALU.mult),
                              reads=[wsb, const_b], writes=[wsb])
                    S.add(DVE, lambda e, gp=gp, wsrc=wsrc, E=E, w=w, hsrc=hsrc: e.scalar_tensor_tensor(
                        out=pooled[:, gp, 0:w], in0=wsrc[:, 15:E], scalar=1.0 / (2 ** (gp + 1)), in1=hsrc[:, gp, 15:E], op0=ALU.mult, op1=ALU.subtract),
                        reads=[wsb, hb_b[cur][gp]], writes=[pooled_b[gp]])
                    if gp % 2 == 0:
                        bkP, bkPb = ps_alloc()
                    S.add(PE, lambda e, bk=bkP, gp=gp, w=w: e.matmul(bk[:, (gp % 2) * 256:(gp % 2) * 256 + w], lhsT=poolw[:, gp, :], rhs=pooled[:, gp, 0:w], start=True, stop=True),
                          reads=[pooled_b[gp], const_b], writes=[bkPb])
                    S.add(ACT, lambda e, bk=bkP, gp=gp, w=w: e.activation(out=ycat[:, 4 + gp, 0:w], in_=bk[:, (gp % 2) * 256:(gp % 2) * 256 + w], func=AF.Identity,
                                                                          scale=cvec[:, PS + gp: PS + gp + 1]), reads=[bkPb, const_b], writes=[ycat_b])
                if ti + 1 < len(tiles):
                    S.add(POOL, lambda e, w=w, cur=cur, nxt=nxt: e.tensor_copy(out=hbT[nxt][:, :, 0:15], in_=hbT[cur][:, :, w:w + 15]),
                          reads=hb_b[cur], writes=hb_b[nxt])
                out_proj(ycat, ycat_b, wout, wout_b, subs, 0)
                for s in subs:
                    ln_update(s, final)

        def ffn(experts, with_halo, final):
            bar = S.last_ops()
            wg_b = fresh_bufs(2, bar)
            wu_b = fresh_bufs(2, bar)
            wd_b = fresh_bufs(2, bar)
            hT_b = [fresh_bufs(4, bar) for _ in range(2)]
            sg_b = fresh_bufs(2, bar)
            wgv = [mview(i * 24576, BF16, 4096).rearrange("p (k f) -> p k f", f=512) for i in range(2)]
            wuv = [mview(i * 24576 + 8192, BF16, 4096).rearrange("p (k f) -> p k f", f=512) for i in range(2)]
            wdv = [mview(i * 24576 + 16384, BF16, 4096).rearrange("p (c d) -> p c d", d=1024) for i in range(2)]
            hTv = [mview(49152 + i * 4096, BF16, 2048).rearrange("p (c t) -> p c t", t=512) for i in range(2)]
            sgv = [mview(57344 + i * 2048, F32, 512) for i in range(2)]
            tiles = ([(0, HALO, [0])] if with_halo else []) + [(HALO + 512 * i, 512, [1 + 4 * i + q for q in range(4)]) for i in range(4)]
            items = []
            groups = []
            for (wg_ap, wu_ap, wd_ap, F, eidx) in experts:
                nfc = F // 128
                for f0 in range(0, nfc, 4):
                    n = min(4, nfc - f0)
                    gi = len(groups)
                    slot = gi % 2
                    groups.append((wg_ap, wu_ap, wd_ap, f0, n, slot))
                    for ti_, tl in enumerate(tiles):
                        items.append((slot, n, tl, eidx, gi if ti_ == 0 else None))

            def load_group(gi):
                wg_ap, wu_ap, wd_ap, f0, n, slot = groups[gi]
                wload(wgv[slot][:, :, 0:n * 128], wg_ap[:, f0 * 128:(f0 + n) * 128].rearrange("(k p) f -> p k f", p=128), [wg_b[slot]], ("wg", slot))
                wload(wuv[slot][:, :, 0:n * 128], wu_ap[:, f0 * 128:(f0 + n) * 128].rearrange("(k p) f -> p k f", p=128), [wu_b[slot]], ("wu", slot))
                wload(wdv[slot][:, 0:n, :], wd_ap[f0 * 128:(f0 + n) * 128, :].rearrange("(c p) d -> p c d", p=128), [wd_b[slot]], ("wd", slot))

            def step1(it, hp):
                slot, n, (c0, w, subs), eidx, _g = it
                xr = [xT_b[s] for s in subs]
                for fc in range(n):
                    bg, bgb = ps_alloc()
                    bu, bub = ps_alloc()

                    def mm(e, bg=bg, bu=bu, fc=fc, slot=slot, c0=c0, w=w):
                        ins = None
                        for k in range(8):
                            ins = e.matmul(bg[:, 0:w], lhsT=wgv[slot][:, k, fc * 128:(fc + 1) * 128], rhs=xT[:, k, c0:c0 + w], start=(k == 0), stop=(k == 7))
                        for k in range(8):
                            ins = e.matmul(bu[:, 0:w], lhsT=wuv[slot][:, k, fc * 128:(fc + 1) * 128], rhs=xT[:, k, c0:c0 + w], start=(k == 0), stop=(k == 7))
                        return ins
                    S.add(PE, mm, reads=[wg_b[slot], wu_b[slot]] + xr, writes=[bgb, bub])
                    sp = fc % 2
                    S.add(ACT, lambda e, bg=bg, w=w, sp=sp: e.activation(out=sgv[sp][:, 0:w], in_=bg[:, 0:w], func=AF.Silu), reads=[bgb], writes=[sg_b[sp]])
                    S.add(DVE, lambda e, bu=bu, w=w, sp=sp, fc=fc, hp=hp: e.tensor_tensor(out=hTv[hp][:, fc, 0:w], in0=bu[:, 0:w], in1=sgv[sp][:, 0:w], op=ALU.mult),
                          reads=[bub, sg_b[sp]], writes=[hT_b[hp][fc]])

            def step2(it, hp):
                slot, n, (c0, w, subs), eidx, _g = it
                for si, s in enumerate(subs):
                    rows = sub_rows(s)
                    loc = 128 * si
                    for half in range(2):
                        bk, bkb = ps_alloc()

                        def mm(e, bk=bk, rows=rows, loc=loc, half=half, slot=slot, n=n, hp=hp):
                            ins = None
                            for fc in range(n):
                                ins = e.matmul(bk[0:rows, :], lhsT=hTv[hp][:, fc, loc:loc + rows], rhs=wdv[slot][:, fc, half * 512:(half + 1) * 512],
                                               start=(fc == 0), stop=(fc == n - 1))
                            return ins
                        S.add(PE, mm, reads=hT_b[hp][0:n] + [wd_b[slot]], writes=[bkb])
                        rs = resid[0:rows, s, half * 512:(half + 1) * 512]
                        if eidx is None:
                            S.add(DVE, lambda e, bk=bk, rows=rows, rs=rs: e.tensor_tensor(out=rs, in0=rs, in1=bk[0:rows, :], op=ALU.add),
                                  reads=[bkb, resid_b[s]], writes=[resid_b[s]])
                        else:
                            S.add(DVE, lambda e, bk=bk, rows=rows, rs=rs, s=s, eidx=eidx: e.scalar_tensor_tensor(
                                out=rs, in0=bk[0:rows, :], scalar=gates[0:rows, s - 1, eidx:eidx + 1], in1=rs, op0=ALU.mult, op1=ALU.add),
                                reads=[bkb, resid_b[s], gates_b[s - 1]], writes=[resid_b[s]])

            load_group(0)
            for i, it in enumerate(items):
                step1(it, i % 2)
                if i >= 1:
                    step2(items[i - 1], (i - 1) % 2)
                if it[4] is not None and it[4] + 1 < len(groups):
                    load_group(it[4] + 1)
            step2(items[-1], (len(items) - 1) % 2)

        def mixer_odd(final, bar):
            win_b, wout_b = fresh_bufs(2, bar)
            win = mview(0, BF16, 8 * 2560).rearrange("p (k f) -> p k f", f=2560)
            wout = mview(40960, BF16, 8 * 1024).rearrange("p (k f) -> p k f", f=1024)
            for k0 in (0, 4):
                wload(win[:, k0:k0 + 4, :], o_w_in[k0 * 128:(k0 + 4) * 128, :].rearrange("(k p) f -> p k f", p=128), [win_b], ("win", k0))
            for k0 in (0, 4):
                wload(wout[:, k0:k0 + 4, :], o_w_out[k0 * 128:(k0 + 4) * 128, :].rearrange("(k p) f -> p k f", p=128), [wout_b], ("wout", k0))
            load_gb(2, final)
            W = 256
            offB = 57344
            cvT = [mview(offB + i * 4128, F32, 4 * 258).rearrange("p (c t) -> p c t", t=258) for i in range(2)]
            vct = [mview(offB + 8256 + i * 1024, F32, W) for i in range(2)]
            acc = mview(offB + 10304, F32, 4 * W).rearrange("p (c t) -> p c t", t=W)
            uT = mview(offB + 14400, BF16, 4 * W).rearrange("p (c t) -> p c t", t=W)
            vg = mview(offB + 16448, F32, 512)
            vln = [mview(offB + 18496 + i * 1024, BF16, 512) for i in range(2)]
            ycat = mview(offB + 20544, BF16, 8 * W).rearrange("p (c t) -> p c t", t=W)
            assert offB + 24640 <= M_BYTES
            tb = lambda: fresh_bufs(1, bar)[0]
            cv_b = [[tb() for _ in range(4)] for _ in range(2)]
            vct_b = [tb(), tb()]
            acc_b = [tb() for _ in range(4)]
            uT_b = [tb() for _ in range(4)]
            vg_b = tb()
            vln_b = [tb(), tb()]
            ycat_b = tb()
            tiles = [(0, HALO, [0])] + [(HALO + W * i, W, [1 + 2 * i, 2 + 2 * i]) for i in range(8)]
            S.add(DVE, lambda e: e.memset(cvT[0][:, :, 0:2], 0.0), writes=cv_b[0])
            vcnt = 0
            for ti, (c0, w, subs) in enumerate(tiles):
                cur, nxt = ti % 2, (ti + 1) % 2
                xr = [xT_b[s] for s in subs]
                halo = (ti == 0)
                for ch in range(4):
                    bk, bkb = ps_alloc()

                    def mm(e, bk=bk, ch=ch, c0=c0, w=w):
                        ins = None
                        for part, oc in ((0, 4 + ch), (1, 8 + ch)):
                            for k in range(8):
                                ins = e.matmul(bk[:, part * 256: part * 256 + w], lhsT=win[:, k, oc * 128:(oc + 1) * 128], rhs=xT[:, k, c0:c0 + w],
                                               start=(k == 0), stop=(k == 7))
                        return ins
                    S.add(PE, mm, reads=[win_b] + xr, writes=[bkb])
                    sp = ch % 2
                    if halo:
                        S.add(ACT, lambda e, bk=bk, w=w, sp=sp: e.activation(out=vct[sp][:, 0:w], in_=bk[:, 256:256 + w], func=AF.Identity, scale=halomask[:, 0:1]),
                              reads=[bkb, const_b], writes=[vct_b[sp]])
                    else:
                        S.add(ACT, lambda e, bk=bk, w=w, sp=sp: e.copy(out=vct[sp][:, 0:w], in_=bk[:, 256:256 + w]), reads=[bkb], writes=[vct_b[sp]])
                    S.add(DVE, lambda e, bk=bk, w=w, sp=sp, ch=ch, cur=cur: e.tensor_tensor(out=cvT[cur][:, ch, 2:2 + w], in0=bk[:, 0:w], in1=vct[sp][:, 0:w], op=ALU.mult),
                          reads=[bkb, vct_b[sp]], writes=[cv_b[cur][ch]])
                if ti + 1 < len(tiles):
                    S.add(POOL, lambda e, w=w, cur=cur, nxt=nxt: e.tensor_copy(out=cvT[nxt][:, :, 0:2], in_=cvT[cur][:, :, w:w + 2]), reads=cv_b[cur], writes=cv_b[nxt])
                if halo:
                    continue
                for ch in range(4):
                    S.add(DVE, lambda e, ch=ch, w=w, cur=cur: e.tensor_scalar(out=acc[:, ch, 0:w], in0=cvT[cur][:, ch, 0:w], scalar1=cvec[:, CC + ch * 3: CC + ch * 3 + 1],
                                                                             scalar2=None, op0=ALU.mult), reads=[cv_b[cur][ch], const_b], writes=[acc_b[ch]])
                    for j in (1, 2):
                        S.add(DVE, lambda e, ch=ch, w=w, cur=cur, j=j: e.scalar_tensor_tensor(
                            out=acc[:, ch, 0:w], in0=cvT[cur][:, ch, j:j + w], scalar=cvec[:, CC + ch * 3 + j: CC + ch * 3 + j + 1], in1=acc[:, ch, 0:w],
                            op0=ALU.mult, op1=ALU.add), reads=[cv_b[cur][ch], acc_b[ch], const_b], writes=[acc_b[ch]])
                for ch in range(4):
                    if ch % 2 == 0:
                        bk, bkb = ps_alloc()

                    def mm(e, bk=bk, ch=ch, c0=c0, w=w):
                        ins = None
                        for k in range(8):
                            ins = e.matmul(bk[:, (ch % 2) * 256:(ch % 2) * 256 + w], lhsT=win[:, k, ch * 128:(ch + 1) * 128], rhs=xT[:, k, c0:c0 + w],
                                           start=(k == 0), stop=(k == 7))
                        return ins
                    S.add(PE, mm, reads=[win_b] + xr, writes=[bkb])
                    S.add(DVE, lambda e, bk=bk, ch=ch, w=w: e.tensor_tensor(out=ycat[:, ch, 0:w], in0=bk[:, (ch % 2) * 256:(ch % 2) * 256 + w], in1=acc[:, ch, 0:w], op=ALU.mult),
                          reads=[bkb, acc_b[ch]], writes=[ycat_b])
                for ch in range(4):
                    if ch % 2 == 0:
                        bk, bkb = ps_alloc()

                    def mm(e, bk=bk, ch=ch, c0=c0, w=w):
                        ins = None
                        for k in range(8):
                            ins = e.matmul(bk[:, (ch % 2) * 256:(ch % 2) * 256 + w], lhsT=win[:, k, (12 + ch) * 128:(13 + ch) * 128], rhs=xT[:, k, c0:c0 + w],
                                           start=(k == 0), stop=(k == 7))
                        return ins
                    S.add(PE, mm, reads=[win_b] + xr, writes=[bkb])
                    S.add(ACT, lambda e, bk=bk, ch=ch, w=w: e.activation(out=uT[:, ch, 0:w], in_=bk[:, (ch % 2) * 256:(ch % 2) * 256 + w], func=AF.Gelu_apprx_tanh),
                          reads=[bkb], writes=[uT_b[ch]])
                for si, s in enumerate(subs):
                    cs = sub_col0(s)
                    vp = vcnt % 2
                    vcnt += 1
                    par = cnt["ln"] % 2
                    cnt["ln"] += 1
                    bk, bkb = ps_alloc()

                    def mm(e, bk=bk, cs=cs):
                        ins = None
                        for k in range(8):
                            ins = e.matmul(bk[:, :], lhsT=xT[:, k, cs:cs + 128], rhs=win[:, k, 2048:2560], start=(k == 0), stop=(k == 7))
                        return ins
                    S.add(PE, mm, reads=[win_b, xT_b[s]], writes=[bkb])
                    S.add(ACT, lambda e, bk=bk: e.activation(out=vg[:, :], in_=bk[:, :], func=AF.Gelu_apprx_tanh), reads=[bkb], writes=[vg_b])
                    S.add(DVE, lambda e, par=par: e.bn_stats(out=stats[:, par, 0, :], in_=vg[:, :]), reads=[vg_b], writes=[st_b[par]])
                    S.add(DVE, lambda e, par=par: e.bn_aggr(out=mv[:, par, :], in_=stats[:, par, 0:1, :]), reads=[st_b[par]], writes=[mv_b[par]])
                    S.add(ACT, lambda e, par=par: e.activation(out=sd[:, par, :], in_=mv[:, par, 1:2], func=AF.Sqrt, bias=epst[:, 0:1], scale=1.0),
                          reads=[mv_b[par], const_b], writes=[sd_b[par]])
                    S.add(DVE, lambda e, par=par: e.reciprocal(out=rstd[:, par, :], in_=sd[:, par, :]), reads=[sd_b[par]], writes=[rstd_b[par]])
                    S.add(DVE, lambda e, par=par: e.tensor_scalar(out=vg[:, :], in0=vg[:, :], scalar1=mv[:, par, 0:1], scalar2=rstd[:, par, :], op0=ALU.subtract, op1=ALU.mult),
                          reads=[vg_b, mv_b[par], rstd_b[par]], writes=[vg_b])
                    S.add(DVE, lambda e: e.tensor_tensor(out=vg[:, :], in0=vg[:, :], in1=sgg[:, 0, :], op=ALU.mult), reads=[vg_b, const_b], writes=[vg_b])
                    S.add(DVE, lambda e, vp=vp: e.tensor_tensor(out=vln[vp][:, :], in0=vg[:, :], in1=sgg[:, 1, :], op=ALU.add), reads=[vg_b, const_b], writes=[vln_b[vp]])
                    bk2, bk2b = ps_alloc()

                    def mm2(e, bk2=bk2, vp=vp):
                        ins = None
                        for h in range(4):
                            e.matmul(bk2[:, h * 128:(h + 1) * 128], lhsT=vln[vp][:, h * 128:(h + 1) * 128], rhs=wmT[:, h, :], start=True, stop=False)
                            e.matmul(bk2[:, h * 128:(h + 1) * 128], lhsT=onesb[0:1, :], rhs=bshl[0:1, 0, h * 128:(h + 1) * 128], start=False, stop=False)
                            ins = e.matmul(bk2[:, h * 128:(h + 1) * 128], lhsT=onesb[0:1, :], rhs=bshl[0:1, 1, h * 128:(h + 1) * 128], start=False, stop=True)
                        return ins
                    S.add(PE, mm2, reads=[vln_b[vp], wmT_b, const_b], writes=[bk2b])
                    loc = 128 * si
                    S.add(DVE, lambda e, bk2=bk2, loc=loc: e.tensor_tensor(out=ycat[:, 4:8, loc:loc + 128], in0=bk2[:, :].rearrange("p (h i) -> p h i", i=128),
                                                                           in1=uT[:, :, loc:loc + 128], op=ALU.mult), reads=[bk2b] + uT_b, writes=[ycat_b])
                out_proj(ycat, ycat_b, wout, wout_b, subs, 0)
                for s in subs:
                    ln_update(s, final)
                    if not final:
                        gating(s)

        def gating(s):
            cs = sub_col0(s)
            gp = s % 2
            g = gsm[:, gp, :]
            lg, mx, msk, ex, nmx, ssum = g[:, 0:8], g[:, 8:16], g[:, 16:24], g[:, 24:32], g[:, 32:33], g[:, 33:34]
            bk, bkb = ps_alloc()

            def mm(e):
                ins = None
                for k in range(8):
                    ins = e.matmul(bk[:, 0:8], lhsT=xT[:, k, cs:cs + 128], rhs=rtr[:, k, :], start=(k == 0), stop=(k == 7))
                return ins
            S.add(PE, mm, reads=[xT_b[s], const_b], writes=[bkb])
            gb_ = gsm_b[gp]
            S.add(DVE, lambda e: e.tensor_copy(out=lg, in_=bk[:, 0:8]), reads=[bkb], writes=[gb_])
            S.add(DVE, lambda e: e.max(out=mx, in_=lg), reads=[gb_], writes=[gb_])
            S.add(DVE, lambda e: e.tensor_scalar(out=msk, in0=lg, scalar1=mx[:, 1:2], scalar2=None, op0=ALU.is_ge), reads=[gb_], writes=[gb_])
            S.add(DVE, lambda e: e.tensor_scalar(out=nmx, in0=mx[:, 0:1], scalar1=-1.0, scalar2=None, op0=ALU.mult), reads=[gb_], writes=[gb_])
            S.add(ACT, lambda e: e.activation(out=ex, in_=lg, func=AF.Exp, bias=nmx, scale=1.0), reads=[gb_], writes=[gb_])
            S.add(DVE, lambda e: e.tensor_tensor(out=ex, in0=ex, in1=msk, op=ALU.mult), reads=[gb_], writes=[gb_])
            S.add(DVE, lambda e: e.reduce_sum(out=ssum, in_=ex, axis=AX.X), reads=[gb_], writes=[gb_])
            S.add(DVE, lambda e: e.reciprocal(out=ssum, in_=ssum), reads=[gb_], writes=[gb_])
            S.add(DVE, lambda e: e.tensor_scalar(out=gates[:, s - 1, :], in0=ex, scalar1=ssum, scalar2=None, op0=ALU.mult), reads=[gb_], writes=[gates_b[s - 1]])

        mixer_even(final=(n_sub == 1))
        if n_sub >= 2:
            load_gb(1, n_sub == 2)
            ffn([(e_wg, e_wu, e_wd, FF_DENSE, None)], with_halo=True, final=(n_sub == 2))
            bar_ffn0 = S.last_ops()
            for s in range(17):
                ln_update(s, n_sub == 2)
        if n_sub >= 3:
            mixer_odd(final=(n_sub == 3), bar=bar_ffn0)
        if n_sub >= 4:
            load_gb(3, True)
            ffn([(o_wg[e], o_wu[e], o_wd[e], FF_EXP, e) for e in range(NEXP)], with_halo=False, final=True)
            for s in range(1, 17):
                ln_update(s, True)
        S.emit(final_wait_ops=out_ops)
    return nc


def _make_in_maps(inp):
    f = lambda a: np.ascontiguousarray(np.asarray(a, dtype=np.float32))
    x = f(inp["x"])

    def pc(v):
        v = f(v)
        return v.reshape(-1, 128).T

    conv_a_w = f(inp["even_conv_a_w"])[0]
    cw = conv_a_w.T.reshape(4, 128, 31).transpose(1, 0, 2).reshape(128, 124)
    conv_c_w = f(inp["odd_conv_c_w"])[0]
    cc = conv_c_w.T.reshape(4, 128, 3).transpose(1, 0, 2).reshape(128, 12)
    cvec = np.concatenate([cw, pc(inp["even_conv_a_b"][0]), pc(inp["even_norm_a_g"][0]), pc(inp["even_norm_a_b"][0]),
                           pc(inp["even_pool_scale"][0]), cc], axis=1)
    cvec = np.ascontiguousarray(cvec, dtype=np.float32)
    assert cvec.shape == (128, 152)
    lnrows = np.stack([f(inp[k])[0] for k in ("even_ln1_g", "even_ln1_b", "even_ln2_g", "even_ln2_b", "odd_ln1_g", "odd_ln1_b", "odd_ln2_g", "odd_ln2_b")])
    sgrows = np.stack([f(inp["odd_sgu_norm_g"])[0], f(inp["odd_sgu_norm_b"])[0], f(inp["odd_sgu_b"])[0].reshape(512)])
    ident = np.eye(128, dtype=np.float32)
    tril = np.triu(np.ones((128, 128), dtype=np.float32))
    common = {
        "e_w_in": f(inp["even_w_in"])[0], "e_w_out": f(inp["even_w_out"])[0],
        "e_wg": f(inp["even_ffn_w_gate"])[0], "e_wu": f(inp["even_ffn_w_up"])[0], "e_wd": f(inp["even_ffn_w_down"])[0],
        "o_w_in": f(inp["odd_w_in"])[0], "o_w_out": f(inp["odd_w_out"])[0], "o_router": f(inp["odd_router"])[0],
        "o_wg": f(inp["odd_moe_w_gate"])[0], "o_wu": f(inp["odd_moe_w_up"])[0], "o_wd": f(inp["odd_moe_w_down"])[0],
        "lnrows": np.ascontiguousarray(lnrows), "sgrows": np.ascontiguousarray(sgrows), "cvec": cvec,
        "pool_w": f(inp["even_pool_w"])[0], "sgu_w": f(inp["odd_sgu_w"])[0], "ident": ident, "tril": tril,
    }
    corr0 = np.ones((4, 16), dtype=np.float32)
    for g in range(4):
        win = 2 ** (g + 1)
        for t in range(16):
            corr0[g, t] = win / min(t + 1, win)
    maps = []
    for c in range(8):
        b, half = c // 2, c % 2
        xc = np.zeros((NT, D), dtype=np.float32)
        if half == 0:
            xc[HALO:] = x[b, 0:T]
            corr = corr0
            hm = 0.0
        else:
            xc[:] = x[b, T - HALO:2 * T]
            corr = np.ones((4, 16), dtype=np.float32)
            hm = 1.0
        m = dict(common)
        m["x_c"] = xc
        m["poolcorr"] = np.ascontiguousarray(np.broadcast_to(corr.reshape(1, 64), (128, 64)), dtype=np.float32)
        m["halomask"] = np.full((128, 1), hm, dtype=np.float32)
        maps.append(m)
    return maps


_NC_CACHE = {}


N_SUB = 4


def kernel(**inputs):
    maps = _make_in_maps(inputs)
    if "nc" not in _NC_CACHE:
        _NC_CACHE["nc"] = build_nc(N_SUB)
    res = run_bass_kernel_spmd(_NC_CACHE["nc"], maps, core_ids=list(range(8)))
    out = np.empty((4, 2 * T, D), dtype=np.float32)
    for c in range(8):
        b, half = c // 2, c % 2
        out[b, half * T:(half + 1) * T] = res.results[c]["out"]
    return out
```

```python
import contextlib
import numpy as np
import concourse.bass as bass
import concourse.mybir as mybir
from concourse.bass_utils import run_bass_kernel_spmd

F32 = mybir.dt.float32
BF16 = mybir.dt.bfloat16
AF = mybir.ActivationFunctionType
ALU = mybir.AluOpType
AX = mybir.AxisListType

PE, ACT, DVE, POOL, SP = "tensor", "scalar", "vector", "gpsimd", "sync"
ENGS = [PE, ACT, DVE, POOL, SP]

HALO = 32
T = 2048
NT = HALO + T
D = 1024
ALPHA = 4.0 ** 0.25
EPS = 1e-5
FF_DENSE = 2816
FF_EXP = 3584
NEXP = 8
CAP = 640
NSL = CAP // 128
I32 = mybir.dt.int32
MOE_MODE = "both"
FORCE_DENSE = False


class Buf:
    __slots__ = ("name", "writer", "readers")

    def __init__(self, name=""):
        self.name = name
        self.writer = None
        self.readers = []


class Op:
    __slots__ = ("eng", "fn", "deps", "dma", "sem", "val", "signal", "cond")

    def __init__(self, eng, fn, dma):
        self.eng = eng
        self.fn = fn
        self.deps = []
        self.dma = dma
        self.sem = None
        self.val = 0
        self.signal = False
        self.cond = None


class Sched:
    def __init__(self, nc):
        self.nc = nc
        self.ops = {e: [] for e in ENGS}
        self.all_ops = []
        self.cur_cond = None
        self.conds = []

    def begin_cond(self, flag_ap, flag_op, sense):
        flag_op.signal = True
        self.conds.append((flag_ap, flag_op, sense))
        self.cur_cond = len(self.conds) - 1

    def end_cond(self):
        self.cur_cond = None

    def add(self, eng, fn, reads=(), writes=(), dma_key=None):
        op = Op(eng, fn, dma_key)
        op.cond = self.cur_cond
        deps = []
        for r in reads:
            if r.writer is not None:
                deps.append(r.writer)
        for w in writes:
            if w.writer is not None:
                deps.append(w.writer)
            deps.extend(w.readers)
        seen = set()
        for d in deps:
            if d is op or id(d) in seen:
                continue
            seen.add(id(d))
            if d.eng == PE and eng == PE and d.dma is None and dma_key is None:
                continue
            op.deps.append(d)
            d.signal = True
        for r in reads:
            r.readers.append(op)
        for w in writes:
            w.writer = op
            w.readers = []
        self.ops[eng].append(op)
        self.all_ops.append(op)
        return op

    def last_ops(self):
        return [self.ops[e][-1] for e in ENGS if self.ops[e]]

    def emit(self, final_wait_ops=()):
        nc = self.nc
        eng_cnt = {e: 0 for e in ENGS}
        dma_keys = {}
        for op in self.all_ops:
            if op.dma is not None:
                ent = dma_keys.setdefault(op.dma, [len(dma_keys), 0])
                ent[1] += 16
                op.sem = ("dma", ent[0])
                op.val = ent[1]
                op.signal = True
            elif op.signal:
                eng_cnt[op.eng] += 1
                op.sem = ("eng", op.eng)
                op.val = eng_cnt[op.eng]
        with contextlib.ExitStack() as st:
            sems = {}
            for e in ENGS:
                sems[("eng", e)] = st.enter_context(nc.semaphore("s_" + e))
            for i in range(len(dma_keys)):
                sems[("dma", i)] = st.enter_context(nc.semaphore("d_%d" % i))
            block = st.enter_context(nc.Block())
            for e in ENGS:
                ops = self.ops[e]

                def body(engobj, ops=ops, e=e):
                    waited = {}

                    def run(op):
                        for d in op.deps:
                            if waited.get(d.sem, 0) >= d.val:
                                continue
                            waited[d.sem] = d.val
                            engobj.wait_ge(sems[d.sem], d.val)
                        ins = op.fn(engobj)
                        if op.signal:
                            ins.then_inc(sems[op.sem], 16 if op.dma is not None else 1)

                    i = 0
                    own_val = 0
                    dma_val = {}
                    while i < len(ops):
                        op = ops[i]
                        if op.cond is None:
                            run(op)
                            if op.dma is not None:
                                dma_val[op.sem] = op.val
                            elif op.signal:
                                own_val = op.val
                            i += 1
                            continue
                        j = i
                        while j < len(ops) and ops[j].cond == op.cond:
                            j += 1
                        blk = ops[i:j]
                        flag_ap, flag_op, sense = self.conds[op.cond]
                        if waited.get(flag_op.sem, 0) < flag_op.val:
                            waited[flag_op.sem] = flag_op.val
                            engobj.wait_ge(sems[flag_op.sem], flag_op.val)
                        creg = engobj.alloc_register("cflag%d_%d" % (op.cond, i))
                        engobj.reg_load(creg, flag_ap)
                        snap_waited = dict(waited)
                        base_own = own_val
                        base_dma = dict(dma_val)
                        n_own = 0
                        n_dma = {}
                        with (engobj.If(creg) if sense else engobj.If_eq(creg, 0)):
                            for bop in blk:
                                run(bop)
                                if bop.dma is not None:
                                    n_dma[bop.sem] = n_dma.get(bop.sem, 0) + 16
                                    dma_val[bop.sem] = bop.val
                                elif bop.signal:
                                    n_own += 1
                                    own_val = bop.val
                        with engobj.Else():
                            if n_own:
                                engobj.wait_ge(sems[("eng", e)], base_own)
                                engobj.sem_inc(sems[("eng", e)], n_own)
                            for k, n in n_dma.items():
                                engobj.wait_ge(sems[k], base_dma.get(k, 0))
                                engobj.sem_inc(sems[k], n)
                        waited.clear()
                        waited.update(snap_waited)
                        i = j
                    if e == SP:
                        for op in final_wait_ops:
                            if waited.get(op.sem, 0) >= op.val:
                                continue
                            waited[op.sem] = op.val
                            engobj.wait_ge(sems[op.sem], op.val)

                getattr(block, e)(body)


def sub_rows(s):
    return HALO if s == 0 else 128


def sub_col0(s):
    return 0 if s == 0 else HALO + 128 * (s - 1)


def build_nc(n_sub=4, moe_mode=None, force_dense=None):
    nc = bass.Bass("TRN2", target_bir_lowering=False)
    moe_mode = MOE_MODE if moe_mode is None else moe_mode
    force_dense = FORCE_DENSE if force_dense is None else force_dense

    def din(name, shape):
        return nc.dram_tensor(name, list(shape), F32, kind="ExternalInput").ap()

    x_c = din("x_c", [NT, D])
    e_w_in = din("e_w_in", [D, 1536])
    e_w_out = din("e_w_out", [D, D])
    e_wg = din("e_wg", [D, FF_DENSE])
    e_wu = din("e_wu", [D, FF_DENSE])
    e_wd = din("e_wd", [FF_DENSE, D])
    o_w_in = din("o_w_in", [D, 2560])
    o_w_out = din("o_w_out", [D, D])
    o_router = din("o_router", [D, NEXP])
    o_wg = din("o_wg", [NEXP, D, FF_EXP])
    o_wu = din("o_wu", [NEXP, D, FF_EXP])
    o_wd = din("o_wd", [NEXP, FF_EXP, D])
    lnrows = din("lnrows", [8, D])
    sgrows = din("sgrows", [3, 512])
    cvec_d = din("cvec", [128, 152])
    pool_w_d = din("pool_w", [4, 128, 128])
    sgu_w_d = din("sgu_w", [4, 128, 128])
    ident_d = din("ident", [128, 128])
    tril_d = din("tril", [128, 128])
    poolcorr_d = din("poolcorr", [128, 64])
    halomask_d = din("halomask", [128, 1])
    ustrict_d = din("ustrict", [128, 128])
    eoff_d = din("eoff", [128, 128])
    out_d = nc.dram_tensor("out", [T, D], F32, kind="ExternalOutput").ap()
    xc_d = nc.dram_tensor("xc_scr", [NEXP * CAP, D], BF16, kind="Internal").ap()
    yc_d = nc.dram_tensor("yc_scr", [NEXP * CAP, D], F32, kind="Internal").ap()

    st = contextlib.ExitStack()
    with st:
        S = Sched(nc)

        def sb(name, shape, dt):
            return st.enter_context(nc.sbuf_tensor(name, list(shape), dt))

        resid = sb("resid", [128, 17, D], F32)
        xT = sb("xT", [128, 8, NT], BF16)
        gbt = sb("gb", [128, 2 * D], F32)
        gb = gbt[:, :].rearrange("p (i d) -> p i d", d=D)
        xb = sb("xb", [128, 2, D], BF16)
        identf = sb("identf", [128, 128], F32)
        identb = sb("identb", [128, 128], BF16)
        onesf = sb("onesf", [128, 128], F32)
        onesb = sb("onesb", [1, 128], BF16)
        epst = sb("epst", [128, 1], F32)
        cvec = sb("cvec_s", [128, 152], F32)
        poolw = sb("poolw", [128, 4, 128], BF16)
        sguwf = gbt[:, D:D + 512].rearrange("p (h j) -> p h j", j=128)
        trilf = gbt[:, D + 512:D + 640]
        wmT = sb("wmT", [128, 4, 128], BF16)
        sgg = sb("sgg", [128, 2, 512], F32)
        bsrow = gbt[0:1, 0:1024].rearrange("p (a b) -> p a b", b=512)
        bshl = sb("bshl", [1, 2, 512], BF16)
        poolcorr = sb("poolcorr_s", [128, 64], F32)
        halomask = sb("halomask_s", [128, 1], F32)
        rtr = sb("rtr", [128, 8, NEXP], BF16)
        gates = sb("gates", [128, 16, NEXP], F32)
        stats = sb("stats", [128, 2, 2, 6], F32)
        mv = sb("mv", [128, 2, 2], F32)
        sd = sb("sd", [128, 2, 1], F32)
        rstd = sb("rstd", [128, 2, 1], F32)
        nb = sb("nb", [128, 2, 1], F32)
        gsm = sb("gsm", [128, 2, 48], F32)
        maskall = sb("maskall", [128, 16, NEXP], F32)
        m0all = sb("m0all", [128, 16, NEXP], F32)
        maskb = sb("maskb", [128, 128], BF16)
        ustb = sb("ustb", [128, 128], BF16)
        ones128b = sb("ones128b", [128, 128], BF16)
        gsel = sb("gsel", [128, 16, 2], F32)
        desti = sb("desti", [128, 2, 16], I32)
        flagi = sb("flagi", [128, 1], I32)
        M_BYTES = 81984
        Mr = sb("Mr", [128, M_BYTES // 2], BF16)
        Mf = Mr.bitcast(F32)

        def mview(off_bytes, dt, n):
            if dt == F32:
                assert off_bytes % 4 == 0
                return Mf[:, off_bytes // 4: off_bytes // 4 + n]
            assert off_bytes % 2 == 0
            return Mr[:, off_bytes // 2: off_bytes // 2 + n]

        pT = st.enter_context(nc.psum_tensor("pT", [128, 8, 128], BF16))
        banks = [st.enter_context(nc.psum_tensor("bank%d" % i, [128, 512], F32)) for i in range(7)]
        bank_bufs = [Buf("bank%d" % i) for i in range(7)]
        pT_b = Buf("pT")
        ring = [0]

        def ps_alloc():
            i = ring[0] % 7
            ring[0] += 1
            return banks[i], bank_bufs[i]

        resid_b = [Buf("resid%d" % s) for s in range(17)]
        xT_b = [Buf("xT%d" % s) for s in range(17)]
        gb_b = [Buf("gbg"), Buf("gbb")]
        xb_b = [Buf("xb0"), Buf("xb1")]
        st_b = [Buf(), Buf()]
        mv_b = [Buf(), Buf()]
        sd_b = [Buf(), Buf()]
        rstd_b = [Buf(), Buf()]
        nb_b = [Buf(), Buf()]
        gsm_b = [Buf(), Buf()]
        const_b = Buf("consts")
        gates_b = [Buf("gates%d" % s) for s in range(16)]
        route_b = Buf("route")
        wmT_b = Buf("wmT")
        cnt = {"ln": 0, "x": 0}
        out_ops = []

        def cload(dst, src, eng=SP, bufs=None):
            key = ("c", cnt["x"])
            cnt["x"] += 1
            return S.add(eng, lambda e: e.dma_start(out=dst, in_=src), writes=(bufs if bufs is not None else [const_b]), dma_key=key)

        cload(identf[:], ident_d)
        cload(cvec[:], cvec_d)
        cload(trilf, tril_d, bufs=[gb_b[1]])
        cload(poolcorr[:], poolcorr_d)
        cload(halomask[:], halomask_d)
        cload(sguwf, sgu_w_d.rearrange("h i j -> i h j"), bufs=[gb_b[1]])
        cload(sgg[:, 0, :], sgrows[0:1, :].partition_broadcast(128))
        cload(sgg[:, 1, :], sgrows[1:2, :].partition_broadcast(128))
        cload(bsrow[:, 0, :], sgrows[2:3, :], bufs=[gb_b[0]])
        cload(poolw[:], pool_w_d.rearrange("g c d -> c g d"), eng=POOL)
        cload(rtr[:], o_router.rearrange("(k p) e -> p k e", p=128), eng=POOL)
        S.add(DVE, lambda e: e.memset(onesf[:], 1.0), writes=[const_b])
        S.add(DVE, lambda e: e.memset(ones128b[:], 1.0), writes=[const_b])
        cload(ustb[:], ustrict_d, eng=POOL)
        S.add(DVE, lambda e: e.memset(onesb[:], 1.0), writes=[const_b])
        S.add(DVE, lambda e: e.memset(epst[:], EPS), writes=[const_b])
        S.add(DVE, lambda e: e.tensor_copy(out=identb[:], in_=identf[:]), reads=[const_b], writes=[const_b])
        S.add(DVE, lambda e: e.tensor_copy(out=bshl[:, 0, :], in_=bsrow[:, 0, :]), reads=[gb_b[0]], writes=[const_b])
        S.add(DVE, lambda e: e.tensor_copy(out=bsrow[:, 1, :], in_=bshl[:, 0, :]), reads=[const_b], writes=[gb_b[0]])
        S.add(DVE, lambda e: e.tensor_tensor(out=bsrow[:, 0, :], in0=bsrow[:, 0, :], in1=bsrow[:, 1, :], op=ALU.subtract), reads=[gb_b[0]], writes=[gb_b[0]])
        S.add(DVE, lambda e: e.tensor_copy(out=bshl[:, 1, :], in_=bsrow[:, 0, :]), reads=[gb_b[0]], writes=[const_b])
        for h in range(4):
            bk, bkb = ps_alloc()
            S.add(PE, lambda e, h=h, bk=bk: e.transpose(out=bk[:, 0:128], in_=sguwf[:, h, :], identity=identf[:]), reads=[const_b, gb_b[1]], writes=[bkb])
            S.add(DVE, lambda e, h=h, bk=bk: e.tensor_tensor(out=wmT[:, h, :], in0=bk[:, 0:128], in1=trilf, op=ALU.mult), reads=[bkb, gb_b[1]], writes=[wmT_b])

        def load_gb(k, final):
            for i in range(2):
                S.add(SP, lambda e, i=i: e.dma_start(out=gb[:, i, :], in_=lnrows[2 * k + i: 2 * k + i + 1, :].partition_broadcast(128)),
                      writes=[gb_b[i]], dma_key=("gb", i))
                if not final:
                    S.add(POOL, lambda e, i=i: e.tensor_scalar(out=gb[:, i, :], in0=gb[:, i, :], scalar1=ALPHA, scalar2=None, op0=ALU.mult),
                          reads=[gb_b[i]], writes=[gb_b[i]])

        def make_xT(s, par):
            rows, c0 = sub_rows(s), sub_col0(s)

            def tr(e):
                ins = None
                for c in range(8):
                    ins = e.transpose(out=pT[:, c, 0:rows], in_=xb[0:rows, par, c * 128:(c + 1) * 128], identity=identb[0:rows, 0:rows])
                return ins
            S.add(PE, tr, reads=[xb_b[par], const_b], writes=[pT_b])
            S.add(ACT, lambda e: e.copy(out=xT[:, :, c0:c0 + rows], in_=pT[:, :, 0:rows]), reads=[pT_b], writes=[xT_b[s]])

        def ln_update(s, final):
            rows = sub_rows(s)
            par = cnt["ln"] % 2
            cnt["ln"] += 1
            r = resid[0:rows, s, :]

            def bn(e):
                e.bn_stats(out=stats[0:rows, par, 0, :], in_=resid[0:rows, s, 0:512])
                return e.bn_stats(out=stats[0:rows, par, 1, :], in_=resid[0:rows, s, 512:1024])
            S.add(DVE, bn, reads=[resid_b[s]], writes=[st_b[par]])
            S.add(DVE, lambda e: e.bn_aggr(out=mv[0:rows, par, :], in_=stats[0:rows, par, :, :]), reads=[st_b[par]], writes=[mv_b[par]])
            S.add(ACT, lambda e: e.activation(out=sd[0:rows, par, :], in_=mv[0:rows, par, 1:2], func=AF.Sqrt, bias=epst[0:rows, 0:1], scale=1.0),
                  reads=[mv_b[par], const_b], writes=[sd_b[par]])
            S.add(DVE, lambda e: e.reciprocal(out=rstd[0:rows, par, :], in_=sd[0:rows, par, :]), reads=[sd_b[par]], writes=[rstd_b[par]])
            S.add(DVE, lambda e: e.scalar_tensor_tensor(out=nb[0:rows, par, :], in0=mv[0:rows, par, 0:1], scalar=-1.0, in1=rstd[0:rows, par, :],
                                                        op0=ALU.mult, op1=ALU.mult), reads=[mv_b[par], rstd_b[par]], writes=[nb_b[par]])
            S.add(ACT, lambda e: e.activation(out=r, in_=r, func=AF.Identity, bias=nb[0:rows, par, :], scale=rstd[0:rows, par, :]),
                  reads=[resid_b[s], nb_b[par], rstd_b[par]], writes=[resid_b[s]])
            S.add(DVE, lambda e: e.tensor_tensor(out=r, in0=r, in1=gb[0:rows, 0, :], op=ALU.mult), reads=[resid_b[s], gb_b[0]], writes=[resid_b[s]])
            S.add(DVE, lambda e: e.tensor_tensor(out=r, in0=r, in1=gb[0:rows, 1, :], op=ALU.add), reads=[resid_b[s], gb_b[1]], writes=[resid_b[s]])
            if final:
                if s >= 1:
                    op = S.add(SP, lambda e: e.dma_start(out=out_d[128 * (s - 1):128 * s, :], in_=r), reads=[resid_b[s]], dma_key=("out", s))
                    out_ops.append(op)
            else:
                S.add(ACT, lambda e: e.activation(out=xb[0:rows, par, :], in_=r, func=AF.Identity, scale=1.0 / ALPHA), reads=[resid_b[s]], writes=[xb_b[par]])
                make_xT(s, par)

        def wload(dst, src, wbufs, key):
            return S.add(POOL, lambda e: e.dma_start(out=dst, in_=src), writes=wbufs, dma_key=key)

        def fresh_bufs(n, barrier):
            bl = []
            for i in range(n):
                b = Buf()
                b.readers = list(barrier)
                bl.append(b)
            return bl

        for s in range(17):
            rows, c0 = sub_rows(s), sub_col0(s)
            par = s % 2
            S.add(SP, lambda e, s=s, rows=rows, c0=c0: e.dma_start(out=resid[0:rows, s, :], in_=x_c[c0:c0 + rows, :]), writes=[resid_b[s]], dma_key=("x", s))
            S.add(ACT, lambda e, s=s, rows=rows, par=par: e.copy(out=xb[0:rows, par, :], in_=resid[0:rows, s, :]), reads=[resid_b[s]], writes=[xb_b[par]])
            make_xT(s, par)
            S.add(POOL, lambda e, s=s, rows=rows: e.tensor_scalar(out=resid[0:rows, s, :], in0=resid[0:rows, s, :], scalar1=ALPHA, scalar2=None, op0=ALU.mult),
                  reads=[resid_b[s]], writes=[resid_b[s]])

        CW, CB, NG, NB_, PS, CC = 0, 124, 128, 132, 136, 140

        def out_proj(ycat, ycat_b, wout, wout_b, subs, loc0):
            for si, s in enumerate(subs):
                rows = sub_rows(s)
                loc = loc0 + 128 * si
                for half in range(2):
                    bk, bkb = ps_alloc()

                    def mm(e, bk=bk, rows=rows, loc=loc, half=half):
                        ins = None
                        for k in range(8):
                            ins = e.matmul(bk[0:rows, :], lhsT=ycat[:, k, loc:loc + rows], rhs=wout[:, k, half * 512:(half + 1) * 512],
                                           start=(k == 0), stop=(k == 7))
                        return ins
                    S.add(PE, mm, reads=[ycat_b, wout_b], writes=[bkb])
                    S.add(DVE, lambda e, bk=bk, rows=rows, s=s, half=half: e.tensor_tensor(
                        out=resid[0:rows, s, half * 512:(half + 1) * 512], in0=resid[0:rows, s, half * 512:(half + 1) * 512], in1=bk[0:rows, :], op=ALU.add),
                        reads=[bkb, resid_b[s]], writes=[resid_b[s]])

        def mixer_even(final):
            bar = []
            win_b, wout_b = fresh_bufs(2, bar)
            tmp_bar = bar
            win = mview(0, BF16, 8 * 1536).rearrange("p (k f) -> p k f", f=1536)
            wout = mview(40960, BF16, 8 * 1024).rearrange("p (k f) -> p k f", f=1024)
            for k0 in (0, 4):
                wload(win[:, k0:k0 + 4, :], e_w_in[k0 * 128:(k0 + 4) * 128, :].rearrange("(k p) f -> p k f", p=128), [win_b], ("win", k0))
            for k0 in (0, 4):
                wload(wout[:, k0:k0 + 4, :], e_w_out[k0 * 128:(k0 + 4) * 128, :].rearrange("(k p) f -> p k f", p=128), [wout_b], ("wout", k0))
            load_gb(0, final)
            W = 256
            offA = 24576
            aT = [mview(offA + i * 4576, F32, 4 * 286).rearrange("p (c t) -> p c t", t=286) for i in range(2)]
            acc = mview(offA + 9152, F32, 4 * W).rearrange("p (c t) -> p c t", t=W)
            sigt = [mview(offA + 13248 + i * 1024, F32, W) for i in range(2)]
            assert 15296 <= 16384
            offB = 57344
            sqt = mview(offB, F32, 4 * W).rearrange("p (c t) -> p c t", t=W)
            meant = mview(offB + 4096, F32, W)
            vart = mview(offB + 5120, F32, W)
            rstt = mview(offB + 6144, F32, W)
            hbT = [mview(offB + 7168 + i * 4336, F32, 4 * 271).rearrange("p (c t) -> p c t", t=271) for i in range(2)]
            tmpA = mview(offB + 15840, F32, 271)
            tmpB = mview(offB + 16924, F32, 271)
            pooled = mview(offB + 18008, BF16, 4 * W).rearrange("p (c t) -> p c t", t=W)
            ycat = mview(offB + 20056, BF16, 8 * W).rearrange("p (c t) -> p c t", t=W)
            assert 24152 <= 24576
            tb = lambda: fresh_bufs(1, tmp_bar)[0]
            aT_b = [[tb() for _ in range(4)] for _ in range(2)]
            acc_b = [tb() for _ in range(4)]
            sig_b = [tb(), tb()]
            sq_b = [tb() for _ in range(4)]
            mean_b, var_b, rst_b = tb(), tb(), tb()
            hb_b = [[tb() for _ in range(4)] for _ in range(2)]
            tA_b, tB_b = tb(), tb()
            pooled_b = [tb() for _ in range(4)]
            ycat_b = tb()
            S.add(DVE, lambda e: e.memset(aT[0][:, :, 0:30], 0.0), writes=aT_b[0])
            S.add(DVE, lambda e: e.memset(hbT[0][:, :, 0:15], 0.0), writes=hb_b[0])
            tiles = [(0, HALO, [0])] + [(HALO + W * i, W, [1 + 2 * i, 2 + 2 * i]) for i in range(8)]
            for ti, (c0, w, subs) in enumerate(tiles):
                cur, nxt = ti % 2, (ti + 1) % 2
                xr = [xT_b[s] for s in subs]
                for ch in range(4):
                    bk, bkb = ps_alloc()

                    def mm(e, bk=bk, ch=ch, c0=c0, w=w):
                        ins = None
                        for part, oc in ((0, ch), (1, 4 + ch)):
                            for k in range(8):
                                ins = e.matmul(bk[:, part * 256: part * 256 + w], lhsT=win[:, k, oc * 128:(oc + 1) * 128], rhs=xT[:, k, c0:c0 + w],
                                               start=(k == 0), stop=(k == 7))
                        return ins
                    S.add(PE, mm, reads=[win_b] + xr, writes=[bkb])
                    sp = ch % 2
                    S.add(ACT, lambda e, bk=bk, w=w, sp=sp: e.activation(out=sigt[sp][:, 0:w], in_=bk[:, 256:256 + w], func=AF.Sigmoid), reads=[bkb], writes=[sig_b[sp]])
                    S.add(DVE, lambda e, bk=bk, w=w, sp=sp, ch=ch, cur=cur: e.tensor_tensor(out=aT[cur][:, ch, 30:30 + w], in0=bk[:, 0:w], in1=sigt[sp][:, 0:w], op=ALU.mult),
                          reads=[bkb, sig_b[sp]], writes=[aT_b[cur][ch]])
                for gp in range(4):
                    if gp % 2 == 0:
                        bkB, bkBb = ps_alloc()

                    def mm(e, bk=bkB, gp=gp, c0=c0, w=w):
                        ins = None
                        for k in range(8):
                            ins = e.matmul(bk[:, (gp % 2) * 256:(gp % 2) * 256 + w], lhsT=win[:, k, (8 + gp) * 128:(9 + gp) * 128], rhs=xT[:, k, c0:c0 + w],
                                           start=(k == 0), stop=(k == 7))
                        return ins
                    S.add(PE, mm, reads=[win_b] + xr, writes=[bkBb])
                    S.add(ACT, lambda e, bk=bkB, gp=gp, w=w, cur=cur: e.copy(out=hbT[cur][:, gp, 15:15 + w], in_=bk[:, (gp % 2) * 256:(gp % 2) * 256 + w]),
                          reads=[bkBb], writes=[hb_b[cur][gp]])
                for j in range(31):
                    for ch in range(4):
                        if j == 0:
                            S.add(DVE, lambda e, ch=ch, w=w, cur=cur: e.tensor_scalar(
                                out=acc[:, ch, 0:w], in0=aT[cur][:, ch, 0:w], scalar1=cvec[:, CW + ch * 31: CW + ch * 31 + 1], scalar2=cvec[:, CB + ch: CB + ch + 1],
                                op0=ALU.mult, op1=ALU.add), reads=[aT_b[cur][ch], const_b], writes=[acc_b[ch]])
                        else:
                            S.add(DVE, lambda e, ch=ch, w=w, cur=cur, j=j: e.scalar_tensor_tensor(
                                out=acc[:, ch, 0:w], in0=aT[cur][:, ch, j:j + w], scalar=cvec[:, CW + ch * 31 + j: CW + ch * 31 + j + 1], in1=acc[:, ch, 0:w],
                                op0=ALU.mult, op1=ALU.add), reads=[aT_b[cur][ch], acc_b[ch]], writes=[acc_b[ch]])
                if ti + 1 < len(tiles):
                    S.add(POOL, lambda e, w=w, cur=cur, nxt=nxt: e.tensor_copy(out=aT[nxt][:, :, 0:30], in_=aT[cur][:, :, w:w + 30]),
                          reads=aT_b[cur], writes=aT_b[nxt])
                for ch in range(4):
                    S.add(ACT, lambda e, ch=ch, w=w: e.activation(out=sqt[:, ch, 0:w], in_=acc[:, ch, 0:w], func=AF.Square), reads=[acc_b[ch]], writes=[sq_b[ch]])
                bk, bkb = ps_alloc()

                def mmst(e, bk=bk, w=w):
                    ins = None
                    for ch in range(4):
                        ins = e.matmul(bk[:, 0:w], lhsT=onesf[:], rhs=acc[:, ch, 0:w], start=(ch == 0), stop=(ch == 3))
                    for ch in range(4):
                        ins = e.matmul(bk[:, 256:256 + w], lhsT=onesf[:], rhs=sqt[:, ch, 0:w], start=(ch == 0), stop=(ch == 3))
                    return ins
                S.add(PE, mmst, reads=acc_b + sq_b + [const_b], writes=[bkb])
                S.add(DVE, lambda e, bk=bk, w=w: e.tensor_scalar(out=meant[:, 0:w], in0=bk[:, 0:w], scalar1=1.0 / 512, scalar2=None, op0=ALU.mult), reads=[bkb], writes=[mean_b])
                S.add(DVE, lambda e, w=w: e.tensor_tensor(out=vart[:, 0:w], in0=meant[:, 0:w], in1=meant[:, 0:w], op=ALU.mult), reads=[mean_b], writes=[var_b])
                S.add(DVE, lambda e, bk=bk, w=w: e.scalar_tensor_tensor(out=vart[:, 0:w], in0=bk[:, 256:256 + w], scalar=1.0 / 512, in1=vart[:, 0:w], op0=ALU.mult, op1=ALU.subtract),
                      reads=[bkb, var_b], writes=[var_b])
                S.add(ACT, lambda e, w=w: e.activation(out=rstt[:, 0:w], in_=vart[:, 0:w], func=AF.Sqrt, bias=epst[:, 0:1], scale=1.0), reads=[var_b, const_b], writes=[rst_b])
                S.add(DVE, lambda e, w=w: e.reciprocal(out=rstt[:, 0:w], in_=rstt[:, 0:w]), reads=[rst_b], writes=[rst_b])
                for ch in range(4):
                    S.add(DVE, lambda e, ch=ch, w=w: e.tensor_tensor(out=acc[:, ch, 0:w], in0=acc[:, ch, 0:w], in1=meant[:, 0:w], op=ALU.subtract),
                          reads=[acc_b[ch], mean_b], writes=[acc_b[ch]])
                for ch in range(4):
                    S.add(DVE, lambda e, ch=ch, w=w: e.tensor_tensor(out=acc[:, ch, 0:w], in0=acc[:, ch, 0:w], in1=rstt[:, 0:w], op=ALU.mult),
                          reads=[acc_b[ch], rst_b], writes=[acc_b[ch]])
                for ch in range(4):
                    S.add(ACT, lambda e, ch=ch, w=w: e.activation(out=ycat[:, ch, 0:w], in_=acc[:, ch, 0:w], func=AF.Silu, bias=cvec[:, NB_ + ch: NB_ + ch + 1],
                                                                 scale=cvec[:, NG + ch: NG + ch + 1]), reads=[acc_b[ch], const_b], writes=[ycat_b])
                E = 15 + w
                for gp in range(4):
                    hsrc = hbT[cur]
                    S.add(DVE, lambda e, gp=gp, E=E, hsrc=hsrc: e.tensor_tensor(out=tmpA[:, 1:E], in0=hsrc[:, gp, 1:E], in1=hsrc[:, gp, 0:E - 1], op=ALU.add),
                          reads=[hb_b[cur][gp]], writes=[tA_b])
                    wsrc, wsb = tmpA, tA_b
                    if gp >= 1:
                        S.add(DVE, lambda e, E=E: e.tensor_tensor(out=tmpB[:, 3:E], in0=tmpA[:, 3:E], in1=tmpA[:, 1:E - 2], op=ALU.add), reads=[tA_b], writes=[tB_b])
                        wsrc, wsb = tmpB, tB_b
                    if gp >= 2:
                        S.add(DVE, lambda e, E=E: e.tensor_tensor(out=tmpA[:, 7:E], in0=tmpB[:, 7:E], in1=tmpB[:, 3:E - 4], op=ALU.add), reads=[tB_b], writes=[tA_b])
                        wsrc, wsb = tmpA, tA_b
                    if gp >= 3:
                        S.add(DVE, lambda e, E=E: e.tensor_tensor(out=tmpB[:, 15:E], in0=tmpA[:, 15:E], in1=tmpA[:, 7:E - 8], op=ALU.add), reads=[tA_b], writes=[tB_b])
                        wsrc, wsb = tmpB, tB_b
                    if ti == 1:
                        S.add(DVE, lambda e, gp=gp, wsrc=wsrc: e.tensor_tensor(out=wsrc[:, 15:31], in0=wsrc[:, 15:31], in1=poolcorr[:, gp * 16:(gp + 1) * 16], op=ALU.mult),
                              reads=[wsb, const_b], writes=[wsb])
                    S.add(DVE, lambda e, gp=gp, wsrc=wsrc, E=E, w=w, hsrc=hsrc: e.scalar_tensor_tensor(
                        out=pooled[:, gp, 0:w], in0=wsrc[:, 15:E], scalar=1.0 / (2 ** (gp + 1)), in1=hsrc[:, gp, 15:E], op0=ALU.mult, op1=ALU.subtract),
                        reads=[wsb, hb_b[cur][gp]], writes=[pooled_b[gp]])
                    if gp % 2 == 0:
                        bkP, bkPb = ps_alloc()
                    S.add(PE, lambda e, bk=bkP, gp=gp, w=w: e.matmul(bk[:, (gp % 2) * 256:(gp % 2) * 256 + w], lhsT=poolw[:, gp, :], rhs=pooled[:, gp, 0:w], start=True, stop=True),
                          reads=[pooled_b[gp], const_b], writes=[bkPb])
                    S.add(ACT, lambda e, bk=bkP, gp=gp, w=w: e.activation(out=ycat[:, 4 + gp, 0:w], in_=bk[:, (gp % 2) * 256:(gp % 2) * 256 + w], func=AF.Identity,
                                                                          scale=cvec[:, PS + gp: PS + gp + 1]), reads=[bkPb, const_b], writes=[ycat_b])
                if ti + 1 < len(tiles):
                    S.add(POOL, lambda e, w=w, cur=cur, nxt=nxt: e.tensor_copy(out=hbT[nxt][:, :, 0:15], in_=hbT[cur][:, :, w:w + 15]),
                          reads=hb_b[cur], writes=hb_b[nxt])
                out_proj(ycat, ycat_b, wout, wout_b, subs, 0)
                for s in subs:
                    ln_update(s, final)

        def ffn(experts, with_halo, final):
            bar = S.last_ops()
            wg_b = fresh_bufs(2, bar)
            wu_b = fresh_bufs(2, bar)
            wd_b = fresh_bufs(2, bar)
            hT_b = [fresh_bufs(4, bar) for _ in range(2)]
            sg_b = fresh_bufs(2, bar)
            wgv = [mview(i * 24576, BF16, 4096).rearrange("p (k f) -> p k f", f=512) for i in range(2)]
            wuv = [mview(i * 24576 + 8192, BF16, 4096).rearrange("p (k f) -> p k f", f=512) for i in range(2)]
            wdv = [mview(i * 24576 + 16384, BF16, 4096).rearrange("p (c d) -> p c d", d=1024) for i in range(2)]
            hTv = [mview(49152 + i * 4096, BF16, 2048).rearrange("p (c t) -> p c t", t=512) for i in range(2)]
            sgv = [mview(57344 + i * 2048, F32, 512) for i in range(2)]
            tiles = ([(0, HALO, [0])] if with_halo else []) + [(HALO + 512 * i, 512, [1 + 4 * i + q for q in range(4)]) for i in range(4)]
            items = []
            groups = []
            for (wg_ap, wu_ap, wd_ap, F, eidx) in experts:
                nfc = F // 128
                for f0 in range(0, nfc, 4):
                    n = min(4, nfc - f0)
                    gi = len(groups)
                    slot = gi % 2
                    groups.append((wg_ap, wu_ap, wd_ap, f0, n, slot))
                    for ti_, tl in enumerate(tiles):
                        items.append((slot, n, tl, eidx, gi if ti_ == 0 else None))

            def load_group(gi):
                wg_ap, wu_ap, wd_ap, f0, n, slot = groups[gi]
                wload(wgv[slot][:, :, 0:n * 128], wg_ap[:, f0 * 128:(f0 + n) * 128].rearrange("(k p) f -> p k f", p=128), [wg_b[slot]], ("wg", slot))
                wload(wuv[slot][:, :, 0:n * 128], wu_ap[:, f0 * 128:(f0 + n) * 128].rearrange("(k p) f -> p k f", p=128), [wu_b[slot]], ("wu", slot))
                wload(wdv[slot][:, 0:n, :], wd_ap[f0 * 128:(f0 + n) * 128, :].rearrange("(c p) d -> p c d", p=128), [wd_b[slot]], ("wd", slot))

            def step1(it, hp):
                slot, n, (c0, w, subs), eidx, _g = it
                xr = [xT_b[s] for s in subs]
                for fc in range(n):
                    bg, bgb = ps_alloc()
                    bu, bub = ps_alloc()

                    def mm(e, bg=bg, bu=bu, fc=fc, slot=slot, c0=c0, w=w):
                        ins = None
                        for k in range(8):
                            ins = e.matmul(bg[:, 0:w], lhsT=wgv[slot][:, k, fc * 128:(fc + 1) * 128], rhs=xT[:, k, c0:c0 + w], start=(k == 0), stop=(k == 7))
                        for k in range(8):
                            ins = e.matmul(bu[:, 0:w], lhsT=wuv[slot][:, k, fc * 128:(fc + 1) * 128], rhs=xT[:, k, c0:c0 + w], start=(k == 0), stop=(k == 7))
                        return ins
                    S.add(PE, mm, reads=[wg_b[slot], wu_b[slot]] + xr, writes=[bgb, bub])
                    sp = fc % 2
                    S.add(ACT, lambda e, bg=bg, w=w, sp=sp: e.activation(out=sgv[sp][:, 0:w], in_=bg[:, 0:w], func=AF.Silu), reads=[bgb], writes=[sg_b[sp]])
                    S.add(DVE, lambda e, bu=bu, w=w, sp=sp, fc=fc, hp=hp: e.tensor_tensor(out=hTv[hp][:, fc, 0:w], in0=bu[:, 0:w], in1=sgv[sp][:, 0:w], op=ALU.mult),
                          reads=[bub, sg_b[sp]], writes=[hT_b[hp][fc]])

            def step2(it, hp):
                slot, n, (c0, w, subs), eidx, _g = it
                for si, s in enumerate(subs):
                    rows = sub_rows(s)
                    loc = 128 * si
                    for half in range(2):
                        bk, bkb = ps_alloc()

                        def mm(e, bk=bk, rows=rows, loc=loc, half=half, slot=slot, n=n, hp=hp):
                            ins = None
                            for fc in range(n):
                                ins = e.matmul(bk[0:rows, :], lhsT=hTv[hp][:, fc, loc:loc + rows], rhs=wdv[slot][:, fc, half * 512:(half + 1) * 512],
                                               start=(fc == 0), stop=(fc == n - 1))
                            return ins
                        S.add(PE, mm, reads=hT_b[hp][0:n] + [wd_b[slot]], writes=[bkb])
                        rs = resid[0:rows, s, half * 512:(half + 1) * 512]
                        if eidx is None:
                            S.add(DVE, lambda e, bk=bk, rows=rows, rs=rs: e.tensor_tensor(out=rs, in0=rs, in1=bk[0:rows, :], op=ALU.add),
                                  reads=[bkb, resid_b[s]], writes=[resid_b[s]])
                        else:
                            S.add(DVE, lambda e, bk=bk, rows=rows, rs=rs, s=s, eidx=eidx: e.scalar_tensor_tensor(
                                out=rs, in0=bk[0:rows, :], scalar=gates[0:rows, s - 1, eidx:eidx + 1], in1=rs, op0=ALU.mult, op1=ALU.add),
                                reads=[bkb, resid_b[s], gates_b[s - 1]], writes=[resid_b[s]])

            load_group(0)
            for i, it in enumerate(items):
                step1(it, i % 2)
                if i >= 1:
                    step2(items[i - 1], (i - 1) % 2)
                if it[4] is not None and it[4] + 1 < len(groups):
                    load_group(it[4] + 1)
            step2(items[-1], (len(items) - 1) % 2)

        def mixer_odd(final, bar):
            win_b, wout_b = fresh_bufs(2, bar)
            win = mview(0, BF16, 8 * 2560).rearrange("p (k f) -> p k f", f=2560)
            wout = mview(40960, BF16, 8 * 1024).rearrange("p (k f) -> p k f", f=1024)
            for k0 in (0, 4):
                wload(win[:, k0:k0 + 4, :], o_w_in[k0 * 128:(k0 + 4) * 128, :].rearrange("(k p) f -> p k f", p=128), [win_b], ("win", k0))
            for k0 in (0, 4):
                wload(wout[:, k0:k0 + 4, :], o_w_out[k0 * 128:(k0 + 4) * 128, :].rearrange("(k p) f -> p k f", p=128), [wout_b], ("wout", k0))
            load_gb(2, final)
            W = 256
            offB = 57344
            cvT = [mview(offB + i * 4128, F32, 4 * 258).rearrange("p (c t) -> p c t", t=258) for i in range(2)]
            vct = [mview(offB + 8256 + i * 1024, F32, W) for i in range(2)]
            acc = mview(offB + 10304, F32, 4 * W).rearrange("p (c t) -> p c t", t=W)
            uT = mview(offB + 14400, BF16, 4 * W).rearrange("p (c t) -> p c t", t=W)
            vg = mview(offB + 16448, F32, 512)
            vln = [mview(offB + 18496 + i * 1024, BF16, 512) for i in range(2)]
            ycat = mview(offB + 20544, BF16, 8 * W).rearrange("p (c t) -> p c t", t=W)
            assert offB + 24640 <= M_BYTES
            tb = lambda: fresh_bufs(1, bar)[0]
            cv_b = [[tb() for _ in range(4)] for _ in range(2)]
            vct_b = [tb(), tb()]
            acc_b = [tb() for _ in range(4)]
            uT_b = [tb() for _ in range(4)]
            vg_b = tb()
            vln_b = [tb(), tb()]
            ycat_b = tb()
            tiles = [(0, HALO, [0])] + [(HALO + W * i, W, [1 + 2 * i, 2 + 2 * i]) for i in range(8)]
            S.add(DVE, lambda e: e.memset(cvT[0][:, :, 0:2], 0.0), writes=cv_b[0])
            vcnt = 0
            for ti, (c0, w, subs) in enumerate(tiles):
                cur, nxt = ti % 2, (ti + 1) % 2
                xr = [xT_b[s] for s in subs]
                halo = (ti == 0)
                for ch in range(4):
                    bk, bkb = ps_alloc()

                    def mm(e, bk=bk, ch=ch, c0=c0, w=w):
                        ins = None
                        for part, oc in ((0, 4 + ch), (1, 8 + ch)):
                            for k in range(8):
                                ins = e.matmul(bk[:, part * 256: part * 256 + w], lhsT=win[:, k, oc * 128:(oc + 1) * 128], rhs=xT[:, k, c0:c0 + w],
                                               start=(k == 0), stop=(k == 7))
                        return ins
                    S.add(PE, mm, reads=[win_b] + xr, writes=[bkb])
                    sp = ch % 2
                    if halo:
                        S.add(ACT, lambda e, bk=bk, w=w, sp=sp: e.activation(out=vct[sp][:, 0:w], in_=bk[:, 256:256 + w], func=AF.Identity, scale=halomask[:, 0:1]),
                              reads=[bkb, const_b], writes=[vct_b[sp]])
                    else:
                        S.add(ACT, lambda e, bk=bk, w=w, sp=sp: e.copy(out=vct[sp][:, 0:w], in_=bk[:, 256:256 + w]), reads=[bkb], writes=[vct_b[sp]])
                    S.add(DVE, lambda e, bk=bk, w=w, sp=sp, ch=ch, cur=cur: e.tensor_tensor(out=cvT[cur][:, ch, 2:2 + w], in0=bk[:, 0:w], in1=vct[sp][:, 0:w], op=ALU.mult),
                          reads=[bkb, vct_b[sp]], writes=[cv_b[cur][ch]])
                if ti + 1 < len(tiles):
                    S.add(POOL, lambda e, w=w, cur=cur, nxt=nxt: e.tensor_copy(out=cvT[nxt][:, :, 0:2], in_=cvT[cur][:, :, w:w + 2]), reads=cv_b[cur], writes=cv_b[nxt])
                if halo:
                    continue
                for ch in range(4):
                    S.add(DVE, lambda e, ch=ch, w=w, cur=cur: e.tensor_scalar(out=acc[:, ch, 0:w], in0=cvT[cur][:, ch, 0:w], scalar1=cvec[:, CC + ch * 3: CC + ch * 3 + 1],
                                                                             scalar2=None, op0=ALU.mult), reads=[cv_b[cur][ch], const_b], writes=[acc_b[ch]])
                    for j in (1, 2):
                        S.add(DVE, lambda e, ch=ch, w=w, cur=cur, j=j: e.scalar_tensor_tensor(
                            out=acc[:, ch, 0:w], in0=cvT[cur][:, ch, j:j + w], scalar=cvec[:, CC + ch * 3 + j: CC + ch * 3 + j + 1], in1=acc[:, ch, 0:w],
                            op0=ALU.mult, op1=ALU.add), reads=[cv_b[cur][ch], acc_b[ch], const_b], writes=[acc_b[ch]])
                for ch in range(4):
                    if ch % 2 == 0:
                        bk, bkb = ps_alloc()

                    def mm(e, bk=bk, ch=ch, c0=c0, w=w):
                        ins = None
                        for k in range(8):
                            ins = e.matmul(bk[:, (ch % 2) * 256:(ch % 2) * 256 + w], lhsT=win[:, k, ch * 128:(ch + 1) * 128], rhs=xT[:, k, c0:c0 + w],
                                           start=(k == 0), stop=(k == 7))
                        return ins
                    S.add(PE, mm, reads=[win_b] + xr, writes=[bkb])
                    S.add(DVE, lambda e, bk=bk, ch=ch, w=w: e.tensor_tensor(out=ycat[:, ch, 0:w], in0=bk[:, (ch % 2) * 256:(ch % 2) * 256 + w], in1=acc[:, ch, 0:w], op=ALU.mult),
                          reads=[bkb, acc_b[ch]], writes=[ycat_b])
                for ch in range(4):
                    if ch % 2 == 0:
                        bk, bkb = ps_alloc()

                    def mm(e, bk=bk, ch=ch, c0=c0, w=w):
                        ins = None
                        for k in range(8):
                            ins = e.matmul(bk[:, (ch % 2) * 256:(ch % 2) * 256 + w], lhsT=win[:, k, (12 + ch) * 128:(13 + ch) * 128], rhs=xT[:, k, c0:c0 + w],
                                           start=(k == 0), stop=(k == 7))
                        return ins
                    S.add(PE, mm, reads=[win_b] + xr, writes=[bkb])
                    S.add(ACT, lambda e, bk=bk, ch=ch, w=w: e.activation(out=uT[:, ch, 0:w], in_=bk[:, (ch % 2) * 256:(ch % 2) * 256 + w], func=AF.Gelu_apprx_tanh),
                          reads=[bkb], writes=[uT_b[ch]])
                for si, s in enumerate(subs):
                    cs = sub_col0(s)
                    vp = vcnt % 2
                    vcnt += 1
                    par = cnt["ln"] % 2
                    cnt["ln"] += 1
                    bk, bkb = ps_alloc()

                    def mm(e, bk=bk, cs=cs):
                        ins = None
                        for k in range(8):
                            ins = e.matmul(bk[:, :], lhsT=xT[:, k, cs:cs + 128], rhs=win[:, k, 2048:2560], start=(k == 0), stop=(k == 7))
                        return ins
                    S.add(PE, mm, reads=[win_b, xT_b[s]], writes=[bkb])
                    S.add(ACT, lambda e, bk=bk: e.activation(out=vg[:, :], in_=bk[:, :], func=AF.Gelu_apprx_tanh), reads=[bkb], writes=[vg_b])
                    S.add(DVE, lambda e, par=par: e.bn_stats(out=stats[:, par, 0, :], in_=vg[:, :]), reads=[vg_b], writes=[st_b[par]])
                    S.add(DVE, lambda e, par=par: e.bn_aggr(out=mv[:, par, :], in_=stats[:, par, 0:1, :]), reads=[st_b[par]], writes=[mv_b[par]])
                    S.add(ACT, lambda e, par=par: e.activation(out=sd[:, par, :], in_=mv[:, par, 1:2], func=AF.Sqrt, bias=epst[:, 0:1], scale=1.0),
                          reads=[mv_b[par], const_b], writes=[sd_b[par]])
                    S.add(DVE, lambda e, par=par: e.reciprocal(out=rstd[:, par, :], in_=sd[:, par, :]), reads=[sd_b[par]], writes=[rstd_b[par]])
                    S.add(DVE, lambda e, par=par: e.tensor_scalar(out=vg[:, :], in0=vg[:, :], scalar1=mv[:, par, 0:1], scalar2=rstd[:, par, :], op0=ALU.subtract, op1=ALU.mult),
                          reads=[vg_b, mv_b[par], rstd_b[par]], writes=[vg_b])
                    S.add(DVE, lambda e: e.tensor_tensor(out=vg[:, :], in0=vg[:, :], in1=sgg[:, 0, :], op=ALU.mult), reads=[vg_b, const_b], writes=[vg_b])
                    S.add(DVE, lambda e, vp=vp: e.tensor_tensor(out=vln[vp][:, :], in0=vg[:, :], in1=sgg[:, 1, :], op=ALU.add), reads=[vg_b, const_b], writes=[vln_b[vp]])
                    bk2, bk2b = ps_alloc()

                    def mm2(e, bk2=bk2, vp=vp):
                        ins = None
                        for h in range(4):
                            e.matmul(bk2[:, h * 128:(h + 1) * 128], lhsT=vln[vp][:, h * 128:(h + 1) * 128], rhs=wmT[:, h, :], start=True, stop=False)
                            e.matmul(bk2[:, h * 128:(h + 1) * 128], lhsT=onesb[0:1, :], rhs=bshl[0:1, 0, h * 128:(h + 1) * 128], start=False, stop=False)
                            ins = e.matmul(bk2[:, h * 128:(h + 1) * 128], lhsT=onesb[0:1, :], rhs=bshl[0:1, 1, h * 128:(h + 1) * 128], start=False, stop=True)
                        return ins
                    S.add(PE, mm2, reads=[vln_b[vp], wmT_b, const_b], writes=[bk2b])
                    loc = 128 * si
                    S.add(DVE, lambda e, bk2=bk2, loc=loc: e.tensor_tensor(out=ycat[:, 4:8, loc:loc + 128], in0=bk2[:, :].rearrange("p (h i) -> p h i", i=128),
                                                                           in1=uT[:, :, loc:loc + 128], op=ALU.mult), reads=[bk2b] + uT_b, writes=[ycat_b])
                out_proj(ycat, ycat_b, wout, wout_b, subs, 0)
                for s in subs:
                    ln_update(s, final)
                    if not final:
                        gating(s)

        def gating(s):
            cs = sub_col0(s)
            gp = s % 2
            g = gsm[:, gp, :]
            lg, mx, msk, ex, nmx, ssum = g[:, 0:8], g[:, 8:16], g[:, 16:24], g[:, 24:32], g[:, 32:33], g[:, 33:34]
            bk, bkb = ps_alloc()

            def mm(e):
                ins = None
                for k in range(8):
                    ins = e.matmul(bk[:, 0:8], lhsT=xT[:, k, cs:cs + 128], rhs=rtr[:, k, :], start=(k == 0), stop=(k == 7))
                return ins
            S.add(PE, mm, reads=[xT_b[s], const_b], writes=[bkb])
            gb_ = gsm_b[gp]
            S.add(DVE, lambda e: e.tensor_copy(out=lg, in_=bk[:, 0:8]), reads=[bkb], writes=[gb_])
            S.add(DVE, lambda e: e.max(out=mx, in_=lg), reads=[gb_], writes=[gb_])
            S.add(DVE, lambda e: e.tensor_scalar(out=msk, in0=lg, scalar1=mx[:, 1:2], scalar2=None, op0=ALU.is_ge), reads=[gb_], writes=[gb_])
            S.add(DVE, lambda e: e.tensor_scalar(out=nmx, in0=mx[:, 0:1], scalar1=-1.0, scalar2=None, op0=ALU.mult), reads=[gb_], writes=[gb_])
            S.add(ACT, lambda e: e.activation(out=ex, in_=lg, func=AF.Exp, bias=nmx, scale=1.0), reads=[gb_], writes=[gb_])
            S.add(DVE, lambda e: e.tensor_tensor(out=ex, in0=ex, in1=msk, op=ALU.mult), reads=[gb_], writes=[gb_])
            S.add(DVE, lambda e: e.reduce_sum(out=ssum, in_=ex, axis=AX.X), reads=[gb_], writes=[gb_])
            S.add(DVE, lambda e: e.reciprocal(out=ssum, in_=ssum), reads=[gb_], writes=[gb_])
            S.add(DVE, lambda e: e.tensor_scalar(out=gates[:, s - 1, :], in0=ex, scalar1=ssum, scalar2=None, op0=ALU.mult), reads=[gb_], writes=[gates_b[s - 1]])
            m0 = g[:, 34:42]
            t8 = g[:, 8:16]
            S.add(DVE, lambda e: e.tensor_scalar(out=m0, in0=lg, scalar1=mx[:, 0:1], scalar2=None, op0=ALU.is_equal), reads=[gb_], writes=[gb_])
            S.add(DVE, lambda e: e.tensor_copy(out=maskall[:, s - 1, :], in_=msk), reads=[gb_], writes=[route_b])
            S.add(DVE, lambda e: e.tensor_copy(out=m0all[:, s - 1, :], in_=m0), reads=[gb_], writes=[route_b])
            S.add(DVE, lambda e: e.tensor_tensor(out=t8, in0=gates[:, s - 1, :], in1=m0, op=ALU.mult), reads=[gb_, gates_b[s - 1]], writes=[gb_])
            S.add(DVE, lambda e: e.reduce_sum(out=gsel[:, s - 1, 0:1], in_=t8, axis=AX.X), reads=[gb_], writes=[route_b])
            S.add(DVE, lambda e: e.tensor_tensor(out=t8, in0=msk, in1=m0, op=ALU.subtract), reads=[gb_], writes=[gb_])
            S.add(DVE, lambda e: e.tensor_tensor(out=t8, in0=t8, in1=gates[:, s - 1, :], op=ALU.mult), reads=[gb_, gates_b[s - 1]], writes=[gb_])
            S.add(DVE, lambda e: e.reduce_sum(out=gsel[:, s - 1, 1:2], in_=t8, axis=AX.X), reads=[gb_], writes=[route_b])

        def routing_finalize():
            cnts = gbt[:, 0:128].rearrange("p (s e) -> p s e", e=NEXP)
            offs = gbt[:, 128:256].rearrange("p (s e) -> p s e", e=NEXP)
            posc = gbt[:, 256:384]
            eoff = gbt[:, 384:512]
            tmpm = gbt[:, 512:640]
            dstf = gbt[:, 640:672].rearrange("p (r s) -> p r s", s=16)
            nmx = gbt[:, 672:673]
            S.add(SP, lambda e: e.dma_start(out=eoff, in_=eoff_d), writes=[gb_b[0]], dma_key=("c", "eoff"))
            S.add(DVE, lambda e: e.tensor_copy(out=maskb[:], in_=maskall[:, :, :].rearrange("p s e -> p (s e)")), reads=[route_b], writes=[route_b])
            bk, bkb = ps_alloc()

            def mm(e):
                e.matmul(bk[:, 0:128], lhsT=ustb[:], rhs=maskb[:], start=True, stop=True)
                return e.matmul(bk[:, 128:256], lhsT=ones128b[:], rhs=maskb[:], start=True, stop=True)
            S.add(PE, mm, reads=[route_b, const_b], writes=[bkb])
            S.add(DVE, lambda e: e.tensor_copy(out=cnts.rearrange("p s e -> p (s e)"), in_=bk[:, 128:256]), reads=[bkb, gb_b[0]], writes=[gb_b[0]])
            S.add(DVE, lambda e: e.memset(offs[:, 0, :], 0.0), reads=[gb_b[0]], writes=[gb_b[0]])
            for q in range(1, 16):
                S.add(DVE, lambda e, q=q: e.tensor_tensor(out=offs[:, q, :], in0=offs[:, q - 1, :], in1=cnts[:, q - 1, :], op=ALU.add), reads=[gb_b[0]], writes=[gb_b[0]])
            S.add(DVE, lambda e: e.tensor_tensor(out=posc, in0=bk[:, 0:128], in1=offs.rearrange("p s e -> p (s e)"), op=ALU.add), reads=[bkb, gb_b[0]], writes=[gb_b[0]])
            S.add(DVE, lambda e: e.tensor_tensor(out=posc, in0=posc, in1=eoff, op=ALU.add), reads=[gb_b[0]], writes=[gb_b[0]])
            S.add(DVE, lambda e: e.tensor_tensor(out=tmpm, in0=posc, in1=m0all[:, :, :].rearrange("p s e -> p (s e)"), op=ALU.mult), reads=[gb_b[0], route_b], writes=[gb_b[0]])
            S.add(DVE, lambda e: e.tensor_reduce(out=dstf[:, 0, :], in_=tmpm.rearrange("p (s e) -> p s e", e=NEXP), axis=AX.X, op=ALU.add), reads=[gb_b[0]], writes=[gb_b[0]])
            S.add(DVE, lambda e: e.tensor_tensor(out=tmpm, in0=maskall[:, :, :].rearrange("p s e -> p (s e)"), in1=m0all[:, :, :].rearrange("p s e -> p (s e)"), op=ALU.subtract),
                  reads=[gb_b[0], route_b], writes=[gb_b[0]])
            S.add(DVE, lambda e: e.tensor_tensor(out=tmpm, in0=tmpm, in1=posc, op=ALU.mult), reads=[gb_b[0]], writes=[gb_b[0]])
            S.add(DVE, lambda e: e.tensor_reduce(out=dstf[:, 1, :], in_=tmpm.rearrange("p (s e) -> p s e", e=NEXP), axis=AX.X, op=ALU.add), reads=[gb_b[0]], writes=[gb_b[0]])
            S.add(DVE, lambda e: e.tensor_copy(out=desti[:, :, :], in_=dstf), reads=[gb_b[0]], writes=[route_b])
            S.add(DVE, lambda e: e.tensor_tensor(out=tmpm[:, 0:8], in0=offs[:, 15, :], in1=cnts[:, 15, :], op=ALU.add), reads=[gb_b[0]], writes=[gb_b[0]])
            S.add(DVE, lambda e: e.reduce_max(out=nmx, in_=tmpm[:, 0:8], axis=AX.X), reads=[gb_b[0]], writes=[gb_b[0]])
            thr = -1.0 if force_dense else float(CAP)
            S.add(DVE, lambda e: e.tensor_scalar(out=nmx, in0=nmx, scalar1=thr, scalar2=None, op0=ALU.is_gt), reads=[gb_b[0]], writes=[gb_b[0]])
            return S.add(DVE, lambda e: e.tensor_copy(out=flagi[:, :], in_=nmx), reads=[gb_b[0]], writes=[route_b])

        def moe_sparse():
            bar = S.last_ops()
            wg_b = fresh_bufs(2, bar)
            wu_b = fresh_bufs(2, bar)
            wd_b = fresh_bufs(2, bar)
            hT_b = [fresh_bufs(4, bar) for _ in range(2)]
            sg_b = fresh_bufs(2, bar)
            yacc_b = fresh_bufs(NSL, bar)
            xcT_b = fresh_bufs(2, bar)
            xcs_b = fresh_bufs(1, bar)[0]
            wgv = [mview(i * 24576, BF16, 4096).rearrange("p (k f) -> p k f", f=512) for i in range(2)]
            wuv = [mview(i * 24576 + 8192, BF16, 4096).rearrange("p (k f) -> p k f", f=512) for i in range(2)]
            wdv = [mview(i * 24576 + 16384, BF16, 4096).rearrange("p (c d) -> p c d", d=1024) for i in range(2)]
            hTv = [mview(49152 + i * 5120, BF16, 4 * CAP).rearrange("p (c t) -> p c t", t=CAP) for i in range(2)]
            yacc = mview(59392, F32, NSL * D).rearrange("p (j d) -> p j d", d=D)
            assert 79872 <= M_BYTES
            sgv = [sgg[:, i, :] for i in range(2)]
            xTf = xT[:, :, :].rearrange("p k t -> p (k t)")
            xcT = [xTf[:, i * 8 * CAP:(i + 1) * 8 * CAP].rearrange("p (k t) -> p k t", t=CAP) for i in range(2)]
            xcs = xTf[:, 16 * CAP:16 * CAP + NSL * D].rearrange("p (j d) -> p j d", d=D)
            assert 16 * CAP + NSL * D <= 8 * NT
            sc_bufs = []
            for s in range(1, 17):
                par = cnt["ln"] % 2
                cnt["ln"] += 1
                S.add(ACT, lambda e, s=s, par=par: e.activation(out=xb[:, par, :], in_=resid[:, s, :], func=AF.Identity, scale=1.0 / ALPHA),
                      reads=[resid_b[s]], writes=[xb_b[par]])
                for r in range(2):
                    b = fresh_bufs(1, bar)[0]
                    sc_bufs.append(b)
                    S.add(POOL, lambda e, s=s, par=par, r=r: e.indirect_dma_start(
                        out=xc_d, out_offset=bass.IndirectOffsetOnAxis(ap=desti[:, r, s - 1:s], axis=0), in_=xb[:, par, :], in_offset=None),
                        reads=[xb_b[par], route_b], writes=[b], dma_key=("xsc", par, r))
            ftiles = [(0, 512), (512, CAP - 512)]
            groups = []
            items = []
            for ex in range(NEXP):
                for f0 in range(0, FF_EXP // 128, 4):
                    gi = len(groups)
                    groups.append((ex, f0, gi % 2))
                    for ti_, tl in enumerate(ftiles):
                        items.append((gi % 2, tl, ex, f0, gi if ti_ == 0 else None, ti_ == len(ftiles) - 1))

            def load_group(gi):
                ex, f0, slot = groups[gi]
                wload(wgv[slot][:, :, :], o_wg[ex][:, f0 * 128:(f0 + 4) * 128].rearrange("(k p) f -> p k f", p=128), [wg_b[slot]], ("wg", slot))
                wload(wuv[slot][:, :, :], o_wu[ex][:, f0 * 128:(f0 + 4) * 128].rearrange("(k p) f -> p k f", p=128), [wu_b[slot]], ("wu", slot))
                wload(wdv[slot][:, :, :], o_wd[ex][f0 * 128:(f0 + 4) * 128, :].rearrange("(c p) d -> p c d", p=128), [wd_b[slot]], ("wd", slot))

            def load_tokens_dma(ex):
                S.add(SP, lambda e, ex=ex: e.dma_start(out=xcs, in_=xc_d[ex * CAP:(ex + 1) * CAP, :].rearrange("(j p) d -> p j d", p=128)),
                      reads=sc_bufs, writes=[xcs_b], dma_key="xcs")

            def load_tokens_tr(ex):
                xp = ex % 2
                for j in range(NSL):
                    def tr(e, j=j):
                        ins = None
                        for c in range(8):
                            ins = e.transpose(out=pT[:, c, :], in_=xcs[:, j, c * 128:(c + 1) * 128], identity=identb[:])
                        return ins
                    S.add(PE, tr, reads=[xcs_b, const_b], writes=[pT_b])
                    S.add(ACT, lambda e, j=j, xp=xp: e.copy(out=xcT[xp][:, :, j * 128:(j + 1) * 128], in_=pT[:, :, :]), reads=[pT_b], writes=[xcT_b[xp]])

            def step1(it, hp):
                slot, (c0, w), ex, f0, _g, _l = it
                xp = ex % 2
                for fc in range(4):
                    bg, bgb = ps_alloc()
                    bu, bub = ps_alloc()

                    def mm(e, bg=bg, bu=bu, fc=fc, slot=slot, c0=c0, w=w, xp=xp):
                        ins = None
                        for k in range(8):
                            ins = e.matmul(bg[:, 0:w], lhsT=wgv[slot][:, k, fc * 128:(fc + 1) * 128], rhs=xcT[xp][:, k, c0:c0 + w], start=(k == 0), stop=(k == 7))
                        for k in range(8):
                            ins = e.matmul(bu[:, 0:w], lhsT=wuv[slot][:, k, fc * 128:(fc + 1) * 128], rhs=xcT[xp][:, k, c0:c0 + w], start=(k == 0), stop=(k == 7))
                        return ins
                    S.add(PE, mm, reads=[wg_b[slot], wu_b[slot], xcT_b[xp]], writes=[bgb, bub])
                    sp = fc % 2
                    S.add(ACT, lambda e, bg=bg, w=w, sp=sp: e.activation(out=sgv[sp][:, 0:w], in_=bg[:, 0:w], func=AF.Silu), reads=[bgb], writes=[sg_b[sp]])
                    S.add(DVE, lambda e, bu=bu, w=w, sp=sp, fc=fc, hp=hp, c0=c0: e.tensor_tensor(out=hTv[hp][:, fc, c0:c0 + w], in0=bu[:, 0:w], in1=sgv[sp][:, 0:w], op=ALU.mult),
                          reads=[bub, sg_b[sp]], writes=[hT_b[hp][fc]])

            def step2(gi, hp):
                ex, f0, slot = groups[gi]
                first = (f0 == 0)
                last = (f0 + 4 == FF_EXP // 128)
                for j in range(NSL):
                    for half in range(2):
                        bk, bkb = ps_alloc()

                        def mm(e, bk=bk, j=j, half=half, slot=slot, hp=hp):
                            ins = None
                            for fc in range(4):
                                ins = e.matmul(bk[:, :], lhsT=hTv[hp][:, fc, j * 128:(j + 1) * 128], rhs=wdv[slot][:, fc, half * 512:(half + 1) * 512],
                                               start=(fc == 0), stop=(fc == 3))
                            return ins
                        S.add(PE, mm, reads=hT_b[hp] + [wd_b[slot]], writes=[bkb])
                        ya = yacc[:, j, half * 512:(half + 1) * 512]
                        if first:
                            S.add(ACT, lambda e, bk=bk, ya=ya: e.copy(out=ya, in_=bk[:, :]), reads=[bkb], writes=[yacc_b[j]])
                        else:
                            S.add(DVE, lambda e, bk=bk, ya=ya: e.tensor_tensor(out=ya, in0=ya, in1=bk[:, :], op=ALU.add), reads=[bkb, yacc_b[j]], writes=[yacc_b[j]])
                if last:
                    b = fresh_bufs(1, [])[0]
                    yst_bufs.append(b)
                    S.add(SP, lambda e, ex=ex: e.dma_start(out=yc_d[ex * CAP:(ex + 1) * CAP, :].rearrange("(j p) d -> p j d", p=128), in_=yacc[:, :, :]),
                          reads=yacc_b, writes=[b], dma_key="yst")

            yst_bufs = []
            load_group(0)
            load_tokens_dma(0)
            load_tokens_tr(0)
            pend = None
            for i, it in enumerate(items):
                slot, tl, ex, f0, gfirst, glast = it
                gi = ex * (FF_EXP // 512) + f0 // 4
                step1(it, gi % 2)
                if gfirst is not None:
                    if pend is not None:
                        step2(pend, pend % 2)
                        pend = None
                    if gi + 1 < len(groups):
                        load_group(gi + 1)
                    if f0 == 0 and ex + 1 < NEXP:
                        load_tokens_dma(ex + 1)
                    if f0 == 12 and ex + 1 < NEXP:
                        load_tokens_tr(ex + 1)
                if glast:
                    pend = gi
            step2(pend, pend % 2)
            for s in range(1, 17):
                for r in range(2):
                    j = (2 * s + r) % NSL
                    S.add(POOL, lambda e, s=s, r=r, j=j: e.indirect_dma_start(
                        out=yacc[:, j, :], out_offset=None, in_=yc_d, in_offset=bass.IndirectOffsetOnAxis(ap=desti[:, r, s - 1:s], axis=0)),
                        reads=yst_bufs + [route_b], writes=[yacc_b[j]], dma_key=("ygt", j))
                    S.add(DVE, lambda e, s=s, r=r, j=j: e.scalar_tensor_tensor(out=resid[:, s, :], in0=yacc[:, j, :], scalar=gsel[:, s - 1, r:r + 1], in1=resid[:, s, :],
                                                                               op0=ALU.mult, op1=ALU.add), reads=[yacc_b[j], resid_b[s], route_b], writes=[resid_b[s]])

        mixer_even(final=(n_sub == 1))
        if n_sub >= 2:
            load_gb(1, n_sub == 2)
            ffn([(e_wg, e_wu, e_wd, FF_DENSE, None)], with_halo=True, final=(n_sub == 2))
            bar_ffn0 = S.last_ops()
            for s in range(17):
                ln_update(s, n_sub == 2)
        if n_sub >= 3:
            mixer_odd(final=(n_sub == 3), bar=bar_ffn0)
        if n_sub >= 4:
            flag_op = routing_finalize()
            load_gb(3, True)
            if moe_mode in ("both", "dense"):
                if moe_mode == "both":
                    S.begin_cond(flagi[0:1, 0:1], flag_op, True)
                ffn([(o_wg[e], o_wu[e], o_wd[e], FF_EXP, e) for e in range(NEXP)], with_halo=False, final=True)
                S.end_cond()
            if moe_mode in ("both", "sparse"):
                if moe_mode == "both":
                    S.begin_cond(flagi[0:1, 0:1], flag_op, False)
                moe_sparse()
                S.end_cond()
            for s in range(1, 17):
                ln_update(s, True)
        S.emit(final_wait_ops=out_ops)
    return nc


def _make_in_maps(inp):
    f = lambda a: np.ascontiguousarray(np.asarray(a, dtype=np.float32))
    x = f(inp["x"])

    def pc(v):
        v = f(v)
        return v.reshape(-1, 128).T

    conv_a_w = f(inp["even_conv_a_w"])[0]
    cw = conv_a_w.T.reshape(4, 128, 31).transpose(1, 0, 2).reshape(128, 124)
    conv_c_w = f(inp["odd_conv_c_w"])[0]
    cc = conv_c_w.T.reshape(4, 128, 3).transpose(1, 0, 2).reshape(128, 12)
    cvec = np.concatenate([cw, pc(inp["even_conv_a_b"][0]), pc(inp["even_norm_a_g"][0]), pc(inp["even_norm_a_b"][0]),
                           pc(inp["even_pool_scale"][0]), cc], axis=1)
    cvec = np.ascontiguousarray(cvec, dtype=np.float32)
    assert cvec.shape == (128, 152)
    lnrows = np.stack([f(inp[k])[0] for k in ("even_ln1_g", "even_ln1_b", "even_ln2_g", "even_ln2_b", "odd_ln1_g", "odd_ln1_b", "odd_ln2_g", "odd_ln2_b")])
    sgrows = np.stack([f(inp["odd_sgu_norm_g"])[0], f(inp["odd_sgu_norm_b"])[0], f(inp["odd_sgu_b"])[0].reshape(512)])
    ident = np.eye(128, dtype=np.float32)
    tril = np.triu(np.ones((128, 128), dtype=np.float32))
    ustrict = np.triu(np.ones((128, 128), dtype=np.float32), k=1)
    eoff = np.ascontiguousarray(np.broadcast_to(np.tile(np.arange(NEXP, dtype=np.float32) * CAP, 16).reshape(1, 128), (128, 128)))
    common = {
        "e_w_in": f(inp["even_w_in"])[0], "e_w_out": f(inp["even_w_out"])[0],
        "e_wg": f(inp["even_ffn_w_gate"])[0], "e_wu": f(inp["even_ffn_w_up"])[0], "e_wd": f(inp["even_ffn_w_down"])[0],
        "o_w_in": f(inp["odd_w_in"])[0], "o_w_out": f(inp["odd_w_out"])[0], "o_router": f(inp["odd_router"])[0],
        "o_wg": f(inp["odd_moe_w_gate"])[0], "o_wu": f(inp["odd_moe_w_up"])[0], "o_wd": f(inp["odd_moe_w_down"])[0],
        "lnrows": np.ascontiguousarray(lnrows), "sgrows": np.ascontiguousarray(sgrows), "cvec": cvec,
        "pool_w": f(inp["even_pool_w"])[0], "sgu_w": f(inp["odd_sgu_w"])[0], "ident": ident, "tril": tril, "ustrict": ustrict, "eoff": eoff,
    }
    corr0 = np.ones((4, 16), dtype=np.float32)
    for g in range(4):
        win = 2 ** (g + 1)
        for t in range(16):
            corr0[g, t] = win / min(t + 1, win)
    maps = []
    for c in range(8):
        b, half = c // 2, c % 2
        xc = np.zeros((NT, D), dtype=np.float32)
        if half == 0:
            xc[HALO:] = x[b, 0:T]
            corr = corr0
            hm = 0.0
        else:
            xc[:] = x[b, T - HALO:2 * T]
            corr = np.ones((4, 16), dtype=np.float32)
            hm = 1.0
        m = dict(common)
        m["x_c"] = xc
        m["poolcorr"] = np.ascontiguousarray(np.broadcast_to(corr.reshape(1, 64), (128, 64)), dtype=np.float32)
        m["halomask"] = np.full((128, 1), hm, dtype=np.float32)
        maps.append(m)
    return maps


_NC_CACHE = {}


N_SUB = 4


def kernel(**inputs):
    maps = _make_in_maps(inputs)
    if "nc" not in _NC_CACHE:
        _NC_CACHE["nc"] = build_nc(N_SUB)
    res = run_bass_kernel_spmd(_NC_CACHE["nc"], maps, core_ids=list(range(8)))
    out = np.empty((4, 2 * T, D), dtype=np.float32)
    for c in range(8):
        b, half = c // 2, c % 2
        out[b, half * T:(half + 1) * T] = res.results[c]["out"]
    return out
```

```python
import contextlib
import numpy as np
import concourse.bass as bass
import concourse.mybir as mybir
from concourse.bass_utils import run_bass_kernel_spmd

F32 = mybir.dt.float32
BF16 = mybir.dt.bfloat16
AF = mybir.ActivationFunctionType
ALU = mybir.AluOpType
AX = mybir.AxisListType

PE, ACT, DVE, POOL, SP = "tensor", "scalar", "vector", "gpsimd", "sync"
ENGS = [PE, ACT, DVE, POOL, SP]

HALO = 32
T = 2048
NT = HALO + T
D = 1024
ALPHA = 4.0 ** 0.25
EPS = 1e-5
FF_DENSE = 2816
FF_EXP = 3584
NEXP = 8
CAP = 640
NSL = CAP // 128
I32 = mybir.dt.int32
MOE_MODE = "both"
FORCE_DENSE = False
PIPE = True


class Buf:
    __slots__ = ("name", "writer", "readers")

    def __init__(self, name=""):
        self.name = name
        self.writer = None
        self.readers = []


class Op:
    __slots__ = ("eng", "fn", "deps", "dma", "sem", "val", "signal", "cond")

    def __init__(self, eng, fn, dma):
        self.eng = eng
        self.fn = fn
        self.deps = []
        self.dma = dma
        self.sem = None
        self.val = 0
        self.signal = False
        self.cond = None


class Sched:
    def __init__(self, nc):
        self.nc = nc
        self.ops = {e: [] for e in ENGS}
        self.all_ops = []
        self.cur_cond = None
        self.conds = []

    def begin_cond(self, flag_ap, flag_op, sense):
        flag_op.signal = True
        self.conds.append((flag_ap, flag_op, sense))
        self.cur_cond = len(self.conds) - 1

    def end_cond(self):
        self.cur_cond = None

    def add(self, eng, fn, reads=(), writes=(), dma_key=None):
        op = Op(eng, fn, dma_key)
        op.cond = self.cur_cond
        deps = []
        for r in reads:
            if r.writer is not None:
                deps.append(r.writer)
        for w in writes:
            if w.writer is not None:
                deps.append(w.writer)
            deps.extend(w.readers)
        seen = set()
        for d in deps:
            if d is op or id(d) in seen:
                continue
            seen.add(id(d))
            if d.eng == PE and eng == PE and d.dma is None and dma_key is None:
                continue
            op.deps.append(d)
            d.signal = True
        for r in reads:
            r.readers.append(op)
        for w in writes:
            w.writer = op
            w.readers = []
        self.ops[eng].append(op)
        self.all_ops.append(op)
        return op

    def last_ops(self):
        return [self.ops[e][-1] for e in ENGS if self.ops[e]]

    def emit(self, final_wait_ops=()):
        nc = self.nc
        eng_cnt = {e: 0 for e in ENGS}
        dma_keys = {}
        for op in self.all_ops:
            if op.dma is not None:
                ent = dma_keys.setdefault(op.dma, [len(dma_keys), 0])
                ent[1] += 16
                op.sem = ("dma", ent[0])
                op.val = ent[1]
                op.signal = True
            elif op.signal:
                eng_cnt[op.eng] += 1
                op.sem = ("eng", op.eng)
                op.val = eng_cnt[op.eng]
        with contextlib.ExitStack() as st:
            sems = {}
            for e in ENGS:
                sems[("eng", e)] = st.enter_context(nc.semaphore("s_" + e))
            for i in range(len(dma_keys)):
                sems[("dma", i)] = st.enter_context(nc.semaphore("d_%d" % i))
            block = st.enter_context(nc.Block())
            for e in ENGS:
                ops = self.ops[e]

                def body(engobj, ops=ops, e=e):
                    waited = {}

                    def run(op):
                        for d in op.deps:
                            if waited.get(d.sem, 0) >= d.val:
                                continue
                            waited[d.sem] = d.val
                            engobj.wait_ge(sems[d.sem], d.val)
                        ins = op.fn(engobj)
                        if op.signal:
                            ins.then_inc(sems[op.sem], 16 if op.dma is not None else 1)

                    i = 0
                    own_val = 0
                    dma_val = {}
                    while i < len(ops):
                        op = ops[i]
                        if op.cond is None:
                            run(op)
                            if op.dma is not None:
                                dma_val[op.sem] = op.val
                            elif op.signal:
                                own_val = op.val
                            i += 1
                            continue
                        j = i
                        while j < len(ops) and ops[j].cond == op.cond:
                            j += 1
                        blk = ops[i:j]
                        flag_ap, flag_op, sense = self.conds[op.cond]
                        if waited.get(flag_op.sem, 0) < flag_op.val:
                            waited[flag_op.sem] = flag_op.val
                            engobj.wait_ge(sems[flag_op.sem], flag_op.val)
                        creg = engobj.alloc_register("cflag%d_%d" % (op.cond, i))
                        engobj.reg_load(creg, flag_ap)
                        snap_waited = dict(waited)
                        base_own = own_val
                        base_dma = dict(dma_val)
                        n_own = 0
                        n_dma = {}
                        with (engobj.If(creg) if sense else engobj.If_eq(creg, 0)):
                            for bop in blk:
                                run(bop)
                                if bop.dma is not None:
                                    n_dma[bop.sem] = n_dma.get(bop.sem, 0) + 16
                                    dma_val[bop.sem] = bop.val
                                elif bop.signal:
                                    n_own += 1
                                    own_val = bop.val
                        with engobj.Else():
                            if n_own:
                                engobj.wait_ge(sems[("eng", e)], base_own)
                                engobj.sem_inc(sems[("eng", e)], n_own)
                            for k, n in n_dma.items():
                                engobj.wait_ge(sems[k], base_dma.get(k, 0))
                                engobj.sem_inc(sems[k], n)
                        waited.clear()
                        waited.update(snap_waited)
                        i = j
                    if e == SP:
                        for op in final_wait_ops:
                            if waited.get(op.sem, 0) >= op.val:
                                continue
                            waited[op.sem] = op.val
                            engobj.wait_ge(sems[op.sem], op.val)

                getattr(block, e)(body)


def sub_rows(s):
    return HALO if s == 0 else 128


def sub_col0(s):
    return 0 if s == 0 else HALO + 128 * (s - 1)


def build_nc(n_sub=4, moe_mode=None, force_dense=None):
    nc = bass.Bass("TRN2", target_bir_lowering=False)
    moe_mode = MOE_MODE if moe_mode is None else moe_mode
    force_dense = FORCE_DENSE if force_dense is None else force_dense

    def din(name, shape):
        return nc.dram_tensor(name, list(shape), F32, kind="ExternalInput").ap()

    x_c = din("x_c", [NT, D])
    e_w_in = din("e_w_in", [D, 1536])
    e_w_out = din("e_w_out", [D, D])
    e_wg = din("e_wg", [D, FF_DENSE])
    e_wu = din("e_wu", [D, FF_DENSE])
    e_wd = din("e_wd", [FF_DENSE, D])
    o_w_in = din("o_w_in", [D, 2560])
    o_w_out = din("o_w_out", [D, D])
    o_router = din("o_router", [D, NEXP])
    o_wg = din("o_wg", [NEXP, D, FF_EXP])
    o_wu = din("o_wu", [NEXP, D, FF_EXP])
    o_wd = din("o_wd", [NEXP, FF_EXP, D])
    lnrows = din("lnrows", [8, D])
    sgrows = din("sgrows", [3, 512])
    cvec_d = din("cvec", [128, 152])
    pool_w_d = din("pool_w", [4, 128, 128])
    sgu_w_d = din("sgu_w", [4, 128, 128])
    ident_d = din("ident", [128, 128])
    tril_d = din("tril", [128, 128])
    poolcorr_d = din("poolcorr", [128, 64])
    halomask_d = din("halomask", [128, 1])
    ustrict_d = din("ustrict", [128, 128])
    eoff_d = din("eoff", [128, 128])
    out_d = nc.dram_tensor("out", [T, D], F32, kind="ExternalOutput").ap()
    xc_d = nc.dram_tensor("xc_scr", [NEXP * CAP, D], BF16, kind="Internal").ap()
    yc_d = nc.dram_tensor("yc_scr", [NEXP * CAP, D], F32, kind="Internal").ap()

    st = contextlib.ExitStack()
    with st:
        S = Sched(nc)

        def sb(name, shape, dt):
            return st.enter_context(nc.sbuf_tensor(name, list(shape), dt))

        resid = sb("resid", [128, 17, D], F32)
        xT = sb("xT", [128, 8, NT], BF16)
        gbt = sb("gb", [128, 2 * D], F32)
        gb = gbt[:, :].rearrange("p (i d) -> p i d", d=D)
        xb = sb("xb", [128, 2, D], BF16)
        identf = sb("identf", [128, 128], F32)
        identb = sb("identb", [128, 128], BF16)
        onesf = sb("onesf", [128, 128], F32)
        onesb = sb("onesb", [1, 128], BF16)
        epst = sb("epst", [128, 1], F32)
        cvec = sb("cvec_s", [128, 152], F32)
        poolw = sb("poolw", [128, 4, 128], BF16)
        sguwf = gbt[:, D:D + 512].rearrange("p (h j) -> p h j", j=128)
        trilf = gbt[:, D + 512:D + 640]
        wmT = sb("wmT", [128, 4, 128], BF16)
        sgg = sb("sgg", [128, 2, 512], F32)
        bsrow = gbt[0:1, 0:1024].rearrange("p (a b) -> p a b", b=512)
        bshl = sb("bshl", [1, 2, 512], BF16)
        poolcorr = sb("poolcorr_s", [128, 64], F32)
        halomask = sb("halomask_s", [128, 1], F32)
        rtr = sb("rtr", [128, 8, NEXP], BF16)
        gates = sb("gates", [128, 16, NEXP], F32)
        stats = sb("stats", [128, 2, 2, 6], F32)
        mv = sb("mv", [128, 2, 2], F32)
        sd = sb("sd", [128, 2, 1], F32)
        rstd = sb("rstd", [128, 2, 1], F32)
        nb = sb("nb", [128, 2, 1], F32)
        gsm = sb("gsm", [128, 2, 48], F32)
        maskall = sb("maskall", [128, 16, NEXP], F32)
        m0all = sb("m0all", [128, 16, NEXP], F32)
        maskb = sb("maskb", [128, 128], BF16)
        ustb = sb("ustb", [128, 128], BF16)
        ones128b = sb("ones128b", [128, 128], BF16)
        gsel = sb("gsel", [128, 16, 2], F32)
        desti = sb("desti", [128, 2, 16], I32)
        flagi = sb("flagi", [128, 1], I32)
        M_BYTES = 81984
        Mr = sb("Mr", [128, M_BYTES // 2], BF16)
        Mf = Mr.bitcast(F32)

        def mview(off_bytes, dt, n):
            if dt == F32:
                assert off_bytes % 4 == 0
                return Mf[:, off_bytes // 4: off_bytes // 4 + n]
            assert off_bytes % 2 == 0
            return Mr[:, off_bytes // 2: off_bytes // 2 + n]

        pT = st.enter_context(nc.psum_tensor("pT", [128, 8, 128], BF16))
        banks = [st.enter_context(nc.psum_tensor("bank%d" % i, [128, 512], F32)) for i in range(7)]
        bank_bufs = [Buf("bank%d" % i) for i in range(7)]
        pT_b = Buf("pT")
        ring = [0]

        def ps_alloc():
            i = ring[0] % 7
            ring[0] += 1
            return banks[i], bank_bufs[i]

        resid_b = [Buf("resid%d" % s) for s in range(17)]
        xT_b = [Buf("xT%d" % s) for s in range(17)]
        gb_b = [Buf("gbg"), Buf("gbb")]
        xb_b = [Buf("xb0"), Buf("xb1")]
        st_b = [Buf(), Buf()]
        mv_b = [Buf(), Buf()]
        sd_b = [Buf(), Buf()]
        rstd_b = [Buf(), Buf()]
        nb_b = [Buf(), Buf()]
        gsm_b = [Buf(), Buf()]
        const_b = Buf("consts")
        gates_b = [Buf("gates%d" % s) for s in range(16)]
        route_b = Buf("route")
        sgg_b = Buf("sgg")
        wmT_b = Buf("wmT")
        cnt = {"ln": 0, "x": 0}
        out_ops = []

        cparts = []

        def cbuf():
            b = Buf()
            cparts.append(b)
            return b

        def cload(dst, src, eng=SP, bufs=None):
            key = ("c", cnt["x"])
            cnt["x"] += 1
            return S.add(eng, lambda e: e.dma_start(out=dst, in_=src), writes=(bufs if bufs is not None else [cbuf()]), dma_key=key)

        win0_b, wout0_b = Buf("win0"), Buf("wout0")
        win0 = mview(0, BF16, 8 * 1536).rearrange("p (k f) -> p k f", f=1536)
        wout0 = mview(40960, BF16, 8 * 1024).rearrange("p (k f) -> p k f", f=1024)
        for k0 in (0, 4):
            S.add(POOL, lambda e, k0=k0: e.dma_start(out=win0[:, k0:k0 + 4, :], in_=e_w_in[k0 * 128:(k0 + 4) * 128, :].rearrange("(k p) f -> p k f", p=128)),
                  writes=[win0_b], dma_key=("win", k0))
        for k0 in (0, 4):
            S.add(POOL, lambda e, k0=k0: e.dma_start(out=wout0[:, k0:k0 + 4, :], in_=e_w_out[k0 * 128:(k0 + 4) * 128, :].rearrange("(k p) f -> p k f", p=128)),
                  writes=[wout0_b], dma_key=("wout", k0))

        identf_b = cbuf()
        cload(identf[:], ident_d, bufs=[identf_b])
        cload(cvec[:], cvec_d)
        cload(trilf, tril_d, bufs=[gb_b[1]])
        cload(poolcorr[:], poolcorr_d)
        cload(halomask[:], halomask_d)
        cload(sguwf, sgu_w_d.rearrange("h i j -> i h j"), bufs=[gb_b[1]])
        cload(bsrow[:, 0, :], sgrows[2:3, :], bufs=[gb_b[0]])
        cload(poolw[:], pool_w_d.rearrange("g c d -> c g d"), eng=POOL)
        cload(rtr[:], o_router.rearrange("(k p) e -> p k e", p=128), eng=POOL)
        S.add(DVE, lambda e: e.memset(onesf[:], 1.0), writes=[cbuf()])
        S.add(DVE, lambda e: e.memset(ones128b[:], 1.0), writes=[cbuf()])
        cload(ustb[:], ustrict_d, eng=POOL)
        S.add(DVE, lambda e: e.memset(onesb[:], 1.0), writes=[cbuf()])
        S.add(DVE, lambda e: e.memset(epst[:], EPS), writes=[cbuf()])
        identb_b = cbuf()
        S.add(DVE, lambda e: e.tensor_copy(out=identb[:], in_=identf[:]), reads=[identf_b], writes=[identb_b])
        bshl_b = cbuf()
        S.add(DVE, lambda e: e.tensor_copy(out=bshl[:, 0, :], in_=bsrow[:, 0, :]), reads=[gb_b[0]], writes=[bshl_b])
        S.add(DVE, lambda e: e.tensor_copy(out=bsrow[:, 1, :], in_=bshl[:, 0, :]), reads=[bshl_b], writes=[gb_b[0]])
        S.add(DVE, lambda e: e.tensor_tensor(out=bsrow[:, 0, :], in0=bsrow[:, 0, :], in1=bsrow[:, 1, :], op=ALU.subtract), reads=[gb_b[0]], writes=[gb_b[0]])
        S.add(DVE, lambda e: e.tensor_copy(out=bshl[:, 1, :], in_=bsrow[:, 0, :]), reads=[gb_b[0]], writes=[bshl_b])
        for h in range(4):
            bk, bkb = ps_alloc()
            S.add(PE, lambda e, h=h, bk=bk: e.transpose(out=bk[:, 0:128], in_=sguwf[:, h, :], identity=identf[:]), reads=[identf_b, gb_b[1]], writes=[bkb])
            S.add(DVE, lambda e, h=h, bk=bk: e.tensor_tensor(out=wmT[:, h, :], in0=bk[:, 0:128], in1=trilf, op=ALU.mult), reads=[bkb, gb_b[1]], writes=[wmT_b])
        S.add(DVE, lambda e: e.memset(nb[:, 0, :], 0.0), reads=cparts, writes=[const_b, nb_b[0]])

        def load_gb(k, final):
            for i in range(2):
                S.add(SP, lambda e, i=i: e.dma_start(out=gb[:, i, :], in_=lnrows[2 * k + i: 2 * k + i + 1, :].partition_broadcast(128)),
                      writes=[gb_b[i]], dma_key=("gb", i))
                if not final:
                    S.add(DVE, lambda e, i=i: e.tensor_scalar(out=gb[:, i, :], in0=gb[:, i, :], scalar1=ALPHA, scalar2=None, op0=ALU.mult),
                          reads=[gb_b[i]], writes=[gb_b[i]])

        def make_xT(s, par):
            rows, c0 = sub_rows(s), sub_col0(s)

            def tr(e):
                ins = None
                for c in range(8):
                    ins = e.transpose(out=pT[:, c, 0:rows], in_=xb[0:rows, par, c * 128:(c + 1) * 128], identity=identb[0:rows, 0:rows])
                return ins
            S.add(PE, tr, reads=[xb_b[par], identb_b], writes=[pT_b])
            S.add(ACT, lambda e: e.copy(out=xT[:, :, c0:c0 + rows], in_=pT[:, :, 0:rows]), reads=[pT_b], writes=[xT_b[s]])

        def ln_update(s, final):
            rows = sub_rows(s)
            par = cnt["ln"] % 2
            cnt["ln"] += 1
            r = resid[0:rows, s, :]

            def bn(e):
                e.bn_stats(out=stats[0:rows, par, 0, :], in_=resid[0:rows, s, 0:512])
                return e.bn_stats(out=stats[0:rows, par, 1, :], in_=resid[0:rows, s, 512:1024])
            S.add(DVE, bn, reads=[resid_b[s]], writes=[st_b[par]])
            S.add(DVE, lambda e: e.bn_aggr(out=mv[0:rows, par, :], in_=stats[0:rows, par, :, :]), reads=[st_b[par]], writes=[mv_b[par]])
            S.add(ACT, lambda e: e.activation(out=sd[0:rows, par, :], in_=mv[0:rows, par, 1:2], func=AF.Sqrt, bias=epst[0:rows, 0:1], scale=1.0),
                  reads=[mv_b[par], const_b], writes=[sd_b[par]])
            S.add(DVE, lambda e: e.reciprocal(out=rstd[0:rows, par, :], in_=sd[0:rows, par, :]), reads=[sd_b[par]], writes=[rstd_b[par]])
            S.add(DVE, lambda e: e.scalar_tensor_tensor(out=nb[0:rows, par, :], in0=mv[0:rows, par, 0:1], scalar=-1.0, in1=rstd[0:rows, par, :],
                                                        op0=ALU.mult, op1=ALU.mult), reads=[mv_b[par], rstd_b[par]], writes=[nb_b[par]])
            S.add(ACT, lambda e: e.activation(out=r, in_=r, func=AF.Identity, bias=nb[0:rows, par, :], scale=rstd[0:rows, par, :]),
                  reads=[resid_b[s], nb_b[par], rstd_b[par]], writes=[resid_b[s]])
            S.add(DVE, lambda e: e.tensor_tensor(out=r, in0=r, in1=gb[0:rows, 0, :], op=ALU.mult), reads=[resid_b[s], gb_b[0]], writes=[resid_b[s]])
            S.add(DVE, lambda e: e.tensor_tensor(out=r, in0=r, in1=gb[0:rows, 1, :], op=ALU.add), reads=[resid_b[s], gb_b[1]], writes=[resid_b[s]])
            if final:
                if s >= 1:
                    op = S.add(SP, lambda e: e.dma_start(out=out_d[128 * (s - 1):128 * s, :], in_=r), reads=[resid_b[s]], dma_key=("out", s))
                    out_ops.append(op)
            else:
                S.add(ACT, lambda e: e.activation(out=xb[0:rows, par, :], in_=r, func=AF.Identity, scale=1.0 / ALPHA), reads=[resid_b[s]], writes=[xb_b[par]])
                make_xT(s, par)

        def wload(dst, src, wbufs, key):
            return S.add(POOL, lambda e: e.dma_start(out=dst, in_=src), writes=wbufs, dma_key=key)

        def fresh_bufs(n, barrier):
            bl = []
            for i in range(n):
                b = Buf()
                b.readers = list(barrier)
                bl.append(b)
            return bl

        for s in range(17):
            rows, c0 = sub_rows(s), sub_col0(s)
            par = s % 2
            S.add(SP, lambda e, s=s, rows=rows, c0=c0: e.dma_start(out=resid[0:rows, s, :], in_=x_c[c0:c0 + rows, :]), writes=[resid_b[s]], dma_key=("x", s))
            S.add(ACT, lambda e, s=s, rows=rows, par=par: e.copy(out=xb[0:rows, par, :], in_=resid[0:rows, s, :]), reads=[resid_b[s]], writes=[xb_b[par]])
            make_xT(s, par)
            S.add(DVE, lambda e, s=s, rows=rows: e.tensor_scalar(out=resid[0:rows, s, :], in0=resid[0:rows, s, :], scalar1=ALPHA, scalar2=None, op0=ALU.mult),
                  reads=[resid_b[s]], writes=[resid_b[s]])

        CW, CB, NG, NB_, PS, CC = 0, 124, 128, 132, 136, 140

        def out_proj(ycat, ycat_b, wout, wout_b, subs, loc0):
            for si, s in enumerate(subs):
                rows = sub_rows(s)
                loc = loc0 + 128 * si
                for half in range(2):
                    bk, bkb = ps_alloc()

                    def mm(e, bk=bk, rows=rows, loc=loc, half=half):
                        ins = None
                        for k in range(8):
                            ins = e.matmul(bk[0:rows, :], lhsT=ycat[:, k, loc:loc + rows], rhs=wout[:, k, half * 512:(half + 1) * 512],
                                           start=(k == 0), stop=(k == 7))
                        return ins
                    S.add(PE, mm, reads=[ycat_b, wout_b], writes=[bkb])
                    S.add(DVE, lambda e, bk=bk, rows=rows, s=s, half=half: e.tensor_tensor(
                        out=resid[0:rows, s, half * 512:(half + 1) * 512], in0=resid[0:rows, s, half * 512:(half + 1) * 512], in1=bk[0:rows, :], op=ALU.add),
                        reads=[bkb, resid_b[s]], writes=[resid_b[s]])

        def mixer_even(final):
            bar = []
            win_b, wout_b = win0_b, wout0_b
            tmp_bar = bar
            win, wout = win0, wout0
            load_gb(0, final)
            W = 256
            offA = 24576
            aT = [mview(offA + i * 4576, F32, 4 * 286).rearrange("p (c t) -> p c t", t=286) for i in range(2)]
            acc = mview(offA + 9152, F32, 4 * W).rearrange("p (c t) -> p c t", t=W)
            sigt = [mview(offA + 13248 + i * 1024, F32, W) for i in range(2)]
            assert 15296 <= 16384
            offB = 57344
            sqt = mview(offB, F32, 4 * W).rearrange("p (c t) -> p c t", t=W)
            meant = mview(offB + 4096, F32, W)
            vart = mview(offB + 5120, F32, W)
            rstt = mview(offB + 6144, F32, W)
            hbT = [mview(offB + 7168 + i * 4336, F32, 4 * 271).rearrange("p (c t) -> p c t", t=271) for i in range(2)]
            tmpA = mview(offB + 15840, F32, 271)
            tmpB = mview(offB + 16924, F32, 271)
            pooled = mview(offB + 18008, BF16, 4 * W).rearrange("p (c t) -> p c t", t=W)
            ycat = mview(offB + 20056, BF16, 8 * W).rearrange("p (c t) -> p c t", t=W)
            assert 24152 <= 24576
            tb = lambda: fresh_bufs(1, tmp_bar)[0]
            aT_b = [[tb() for _ in range(4)] for _ in range(2)]
            acc_b = [tb() for _ in range(4)]
            sig_b = [tb(), tb()]
            sq_b = [tb() for _ in range(4)]
            mean_b, var_b, rst_b = tb(), tb(), tb()
            hb_b = [[tb() for _ in range(4)] for _ in range(2)]
            tA_b, tB_b = tb(), tb()
            pooled_b = [tb() for _ in range(4)]
            ycat_bs = [tb(), sgg_b]
            ycats = [ycat, sgg.bitcast(BF16)[:, :, :].rearrange("p a b -> p (a b)").rearrange("p (c t) -> p c t", t=W)]
            S.add(DVE, lambda e: e.memset(aT[0][:, :, 0:30], 0.0), writes=aT_b[0])
            S.add(DVE, lambda e: e.memset(hbT[0][:, :, 0:15], 0.0), writes=hb_b[0])
            tiles = [(0, HALO, [0])] + [(HALO + W * i, W, [1 + 2 * i, 2 + 2 * i]) for i in range(8)]
            def Y_even(ti):
                c0, w, subs = tiles[ti]
                out_proj(ycats[ti % 2], ycat_bs[ti % 2], wout, wout_b, subs, 0)
                for s in subs:
                    ln_update(s, final)

            for ti, (c0, w, subs) in enumerate(tiles):
                cur, nxt = ti % 2, (ti + 1) % 2
                xr = [xT_b[s] for s in subs]
                ycat, ycat_b = ycats[ti % 2], ycat_bs[ti % 2]
                for ch in range(4):
                    bk, bkb = ps_alloc()

                    def mm(e, bk=bk, ch=ch, c0=c0, w=w):
                        ins = None
                        for part, oc in ((0, ch), (1, 4 + ch)):
                            for k in range(8):
                                ins = e.matmul(bk[:, part * 256: part * 256 + w], lhsT=win[:, k, oc * 128:(oc + 1) * 128], rhs=xT[:, k, c0:c0 + w],
                                               start=(k == 0), stop=(k == 7))
                        return ins
                    S.add(PE, mm, reads=[win_b] + xr, writes=[bkb])
                    sp = ch % 2
                    S.add(ACT, lambda e, bk=bk, w=w, sp=sp: e.activation(out=sigt[sp][:, 0:w], in_=bk[:, 256:256 + w], func=AF.Sigmoid), reads=[bkb], writes=[sig_b[sp]])
                    S.add(DVE, lambda e, bk=bk, w=w, sp=sp, ch=ch, cur=cur: e.tensor_tensor(out=aT[cur][:, ch, 30:30 + w], in0=bk[:, 0:w], in1=sigt[sp][:, 0:w], op=ALU.mult),
                          reads=[bkb, sig_b[sp]], writes=[aT_b[cur][ch]])
                for gp in range(4):
                    if gp % 2 == 0:
                        bkB, bkBb = ps_alloc()

                    def mm(e, bk=bkB, gp=gp, c0=c0, w=w):
                        ins = None
                        for k in range(8):
                            ins = e.matmul(bk[:, (gp % 2) * 256:(gp % 2) * 256 + w], lhsT=win[:, k, (8 + gp) * 128:(9 + gp) * 128], rhs=xT[:, k, c0:c0 + w],
                                           start=(k == 0), stop=(k == 7))
                        return ins
                    S.add(PE, mm, reads=[win_b] + xr, writes=[bkBb])
                    S.add(ACT, lambda e, bk=bkB, gp=gp, w=w, cur=cur: e.copy(out=hbT[cur][:, gp, 15:15 + w], in_=bk[:, (gp % 2) * 256:(gp % 2) * 256 + w]),
                          reads=[bkBb], writes=[hb_b[cur][gp]])
                for j in range(31):
                    for ch in range(4):
                        if j == 0:
                            S.add(DVE, lambda e, ch=ch, w=w, cur=cur: e.tensor_scalar(
                                out=acc[:, ch, 0:w], in0=aT[cur][:, ch, 0:w], scalar1=cvec[:, CW + ch * 31: CW + ch * 31 + 1], scalar2=cvec[:, CB + ch: CB + ch + 1],
                                op0=ALU.mult, op1=ALU.add), reads=[aT_b[cur][ch], const_b], writes=[acc_b[ch]])
                        else:
                            S.add(DVE, lambda e, ch=ch, w=w, cur=cur, j=j: e.scalar_tensor_tensor(
                                out=acc[:, ch, 0:w], in0=aT[cur][:, ch, j:j + w], scalar=cvec[:, CW + ch * 31 + j: CW + ch * 31 + j + 1], in1=acc[:, ch, 0:w],
                                op0=ALU.mult, op1=ALU.add), reads=[aT_b[cur][ch], acc_b[ch]], writes=[acc_b[ch]])
                if ti + 1 < len(tiles):
                    S.add(ACT, lambda e, w=w, cur=cur, nxt=nxt: e.copy(out=aT[nxt][:, :, 0:30], in_=aT[cur][:, :, w:w + 30]),
                          reads=aT_b[cur], writes=aT_b[nxt])
                for ch in range(4):
                    S.add(ACT, lambda e, ch=ch, w=w: e.activation(out=sqt[:, ch, 0:w], in_=acc[:, ch, 0:w], func=AF.Square), reads=[acc_b[ch]], writes=[sq_b[ch]])
                bk, bkb = ps_alloc()

                def mmst(e, bk=bk, w=w):
                    ins = None
                    for ch in range(4):
                        ins = e.matmul(bk[:, 0:w], lhsT=onesf[:], rhs=acc[:, ch, 0:w], start=(ch == 0), stop=(ch == 3))
                    for ch in range(4):
                        ins = e.matmul(bk[:, 256:256 + w], lhsT=onesf[:], rhs=sqt[:, ch, 0:w], start=(ch == 0), stop=(ch == 3))
                    return ins
                S.add(PE, mmst, reads=acc_b + sq_b + [const_b], writes=[bkb])
                S.add(DVE, lambda e, bk=bk, w=w: e.tensor_scalar(out=meant[:, 0:w], in0=bk[:, 0:w], scalar1=1.0 / 512, scalar2=None, op0=ALU.mult), reads=[bkb], writes=[mean_b])
                S.add(DVE, lambda e, w=w: e.tensor_tensor(out=vart[:, 0:w], in0=meant[:, 0:w], in1=meant[:, 0:w], op=ALU.mult), reads=[mean_b], writes=[var_b])
                S.add(DVE, lambda e, bk=bk, w=w: e.scalar_tensor_tensor(out=vart[:, 0:w], in0=bk[:, 256:256 + w], scalar=1.0 / 512, in1=vart[:, 0:w], op0=ALU.mult, op1=ALU.subtract),
                      reads=[bkb, var_b], writes=[var_b])
                S.add(ACT, lambda e, w=w: e.activation(out=rstt[:, 0:w], in_=vart[:, 0:w], func=AF.Sqrt, bias=epst[:, 0:1], scale=1.0), reads=[var_b, const_b], writes=[rst_b])
                S.add(DVE, lambda e, w=w: e.reciprocal(out=rstt[:, 0:w], in_=rstt[:, 0:w]), reads=[rst_b], writes=[rst_b])
                for ch in range(4):
                    S.add(DVE, lambda e, ch=ch, w=w: e.tensor_tensor(out=acc[:, ch, 0:w], in0=acc[:, ch, 0:w], in1=meant[:, 0:w], op=ALU.subtract),
                          reads=[acc_b[ch], mean_b], writes=[acc_b[ch]])
                for ch in range(4):
                    S.add(DVE, lambda e, ch=ch, w=w: e.tensor_tensor(out=acc[:, ch, 0:w], in0=acc[:, ch, 0:w], in1=rstt[:, 0:w], op=ALU.mult),
                          reads=[acc_b[ch], rst_b], writes=[acc_b[ch]])
                for ch in range(4):
                    S.add(ACT, lambda e, ch=ch, w=w, ycat=ycat: e.activation(out=ycat[:, ch, 0:w], in_=acc[:, ch, 0:w], func=AF.Silu, bias=cvec[:, NB_ + ch: NB_ + ch + 1],
                                                                 scale=cvec[:, NG + ch: NG + ch + 1]), reads=[acc_b[ch], const_b], writes=[ycat_b])
                E = 15 + w
                for gp in range(4):
                    hsrc = hbT[cur]
                    S.add(DVE, lambda e, gp=gp, E=E, hsrc=hsrc: e.tensor_tensor(out=tmpA[:, 1:E], in0=hsrc[:, gp, 1:E], in1=hsrc[:, gp, 0:E - 1], op=ALU.add),
                          reads=[hb_b[cur][gp]], writes=[tA_b])
                    wsrc, wsb = tmpA, tA_b
                    if gp >= 1:
                        S.add(DVE, lambda e, E=E: e.tensor_tensor(out=tmpB[:, 3:E], in0=tmpA[:, 3:E], in1=tmpA[:, 1:E - 2], op=ALU.add), reads=[tA_b], writes=[tB_b])
                        wsrc, wsb = tmpB, tB_b
                    if gp >= 2:
                        S.add(DVE, lambda e, E=E: e.tensor_tensor(out=tmpA[:, 7:E], in0=tmpB[:, 7:E], in1=tmpB[:, 3:E - 4], op=ALU.add), reads=[tB_b], writes=[tA_b])
                        wsrc, wsb = tmpA, tA_b
                    if gp >= 3:
                        S.add(DVE, lambda e, E=E: e.tensor_tensor(out=tmpB[:, 15:E], in0=tmpA[:, 15:E], in1=tmpA[:, 7:E - 8], op=ALU.add), reads=[tA_b], writes=[tB_b])
                        wsrc, wsb = tmpB, tB_b
                    if ti == 1:
                        S.add(DVE, lambda e, gp=gp, wsrc=wsrc: e.tensor_tensor(out=wsrc[:, 15:31], in0=wsrc[:, 15:31], in1=poolcorr[:, gp * 16:(gp + 1) * 16], op=ALU.mult),
                              reads=[wsb, const_b], writes=[wsb])
                    S.add(DVE, lambda e, gp=gp, wsrc=wsrc, E=E, w=w, hsrc=hsrc: e.scalar_tensor_tensor(
                        out=pooled[:, gp, 0:w], in0=wsrc[:, 15:E], scalar=1.0 / (2 ** (gp + 1)), in1=hsrc[:, gp, 15:E], op0=ALU.mult, op1=ALU.subtract),
                        reads=[wsb, hb_b[cur][gp]], writes=[pooled_b[gp]])
                    if gp % 2 == 0:
                        bkP, bkPb = ps_alloc()
                    S.add(PE, lambda e, bk=bkP, gp=gp, w=w: e.matmul(bk[:, (gp % 2) * 256:(gp % 2) * 256 + w], lhsT=poolw[:, gp, :], rhs=pooled[:, gp, 0:w], start=True, stop=True),
                          reads=[pooled_b[gp], const_b], writes=[bkPb])
                    S.add(ACT, lambda e, bk=bkP, gp=gp, w=w, ycat=ycat: e.activation(out=ycat[:, 4 + gp, 0:w], in_=bk[:, (gp % 2) * 256:(gp % 2) * 256 + w], func=AF.Identity,
                                                                          scale=cvec[:, PS + gp: PS + gp + 1]), reads=[bkPb, const_b], writes=[ycat_b])
                if ti + 1 < len(tiles):
                    S.add(ACT, lambda e, w=w, cur=cur, nxt=nxt: e.copy(out=hbT[nxt][:, :, 0:15], in_=hbT[cur][:, :, w:w + 15]),
                          reads=hb_b[cur], writes=hb_b[nxt])
                if not PIPE:
                    Y_even(ti)
                elif ti >= 1:
                    Y_even(ti - 1)
            if PIPE:
                Y_even(len(tiles) - 1)

        def ffn(experts, with_halo, final):
            bar = S.last_ops()
            wg_b = fresh_bufs(2, bar)
            wu_b = fresh_bufs(2, bar)
            wd_b = fresh_bufs(2, bar)
            hT_b = [fresh_bufs(4, bar) for _ in range(2)]
            sg_b = fresh_bufs(2, bar)
            wgv = [mview(i * 24576, BF16, 4096).rearrange("p (k f) -> p k f", f=512) for i in range(2)]
            wuv = [mview(i * 24576 + 8192, BF16, 4096).rearrange("p (k f) -> p k f", f=512) for i in range(2)]
            wdv = [mview(i * 24576 + 16384, BF16, 4096).rearrange("p (c d) -> p c d", d=1024) for i in range(2)]
            hTv = [mview(49152 + i * 4096, BF16, 2048).rearrange("p (c t) -> p c t", t=512) for i in range(2)]
            sgv = [mview(57344 + i * 2048, F32, 512) for i in range(2)]
            tiles = ([(0, HALO, [0])] if with_halo else []) + [(HALO + 512 * i, 512, [1 + 4 * i + q for q in range(4)]) for i in range(4)]
            items = []
            groups = []
            for (wg_ap, wu_ap, wd_ap, F, eidx) in experts:
                nfc = F // 128
                for f0 in range(0, nfc, 4):
                    n = min(4, nfc - f0)
                    gi = len(groups)
                    slot = gi % 2
                    groups.append((wg_ap, wu_ap, wd_ap, f0, n, slot))
                    for ti_, tl in enumerate(tiles):
                        items.append((slot, n, tl, eidx, gi if ti_ == 0 else None))

            def load_group(gi):
                wg_ap, wu_ap, wd_ap, f0, n, slot = groups[gi]
                wload(wgv[slot][:, :, 0:n * 128], wg_ap[:, f0 * 128:(f0 + n) * 128].rearrange("(k p) f -> p k f", p=128), [wg_b[slot]], ("wg", slot))
                wload(wuv[slot][:, :, 0:n * 128], wu_ap[:, f0 * 128:(f0 + n) * 128].rearrange("(k p) f -> p k f", p=128), [wu_b[slot]], ("wu", slot))
                wload(wdv[slot][:, 0:n, :], wd_ap[f0 * 128:(f0 + n) * 128, :].rearrange("(c p) d -> p c d", p=128), [wd_b[slot]], ("wd", slot))

            def step1(it, hp):
                slot, n, (c0, w, subs), eidx, _g = it
                xr = [xT_b[s] for s in subs]
                for fc in range(n):
                    bg, bgb = ps_alloc()
                    bu, bub = ps_alloc()

                    def mm(e, bg=bg, bu=bu, fc=fc, slot=slot, c0=c0, w=w):
                        ins = None
                        for k in range(8):
                            ins = e.matmul(bg[:, 0:w], lhsT=wgv[slot][:, k, fc * 128:(fc + 1) * 128], rhs=xT[:, k, c0:c0 + w], start=(k == 0), stop=(k == 7))
                        for k in range(8):
                            ins = e.matmul(bu[:, 0:w], lhsT=wuv[slot][:, k, fc * 128:(fc + 1) * 128], rhs=xT[:, k, c0:c0 + w], start=(k == 0), stop=(k == 7))
                        return ins
                    S.add(PE, mm, reads=[wg_b[slot], wu_b[slot]] + xr, writes=[bgb, bub])
                    sp = fc % 2
                    S.add(ACT, lambda e, bg=bg, w=w, sp=sp: e.activation(out=sgv[sp][:, 0:w], in_=bg[:, 0:w], func=AF.Silu), reads=[bgb], writes=[sg_b[sp]])
                    S.add(DVE, lambda e, bu=bu, w=w, sp=sp, fc=fc, hp=hp: e.tensor_tensor(out=hTv[hp][:, fc, 0:w], in0=bu[:, 0:w], in1=sgv[sp][:, 0:w], op=ALU.mult),
                          reads=[bub, sg_b[sp]], writes=[hT_b[hp][fc]])

            def step2(it, hp):
                slot, n, (c0, w, subs), eidx, _g = it
                for si, s in enumerate(subs):
                    rows = sub_rows(s)
                    loc = 128 * si
                    for half in range(2):
                        bk, bkb = ps_alloc()

                        def mm(e, bk=bk, rows=rows, loc=loc, half=half, slot=slot, n=n, hp=hp):
                            ins = None
                            for fc in range(n):
                                ins = e.matmul(bk[0:rows, :], lhsT=hTv[hp][:, fc, loc:loc + rows], rhs=wdv[slot][:, fc, half * 512:(half + 1) * 512],
                                               start=(fc == 0), stop=(fc == n - 1))
                            return ins
                        S.add(PE, mm, reads=hT_b[hp][0:n] + [wd_b[slot]], writes=[bkb])
                        rs = resid[0:rows, s, half * 512:(half + 1) * 512]
                        if eidx is None:
                            S.add(DVE, lambda e, bk=bk, rows=rows, rs=rs: e.tensor_tensor(out=rs, in0=rs, in1=bk[0:rows, :], op=ALU.add),
                                  reads=[bkb, resid_b[s]], writes=[resid_b[s]])
                        else:
                            S.add(DVE, lambda e, bk=bk, rows=rows, rs=rs, s=s, eidx=eidx: e.scalar_tensor_tensor(
                                out=rs, in0=bk[0:rows, :], scalar=gates[0:rows, s - 1, eidx:eidx + 1], in1=rs, op0=ALU.mult, op1=ALU.add),
                                reads=[bkb, resid_b[s], gates_b[s - 1]], writes=[resid_b[s]])

            load_group(0)
            for i, it in enumerate(items):
                step1(it, i % 2)
                if i >= 1:
                    step2(items[i - 1], (i - 1) % 2)
                if it[4] is not None and it[4] + 1 < len(groups):
                    load_group(it[4] + 1)
            step2(items[-1], (len(items) - 1) % 2)

        def mixer_odd(final, bar):
            win_b, wout_b = fresh_bufs(2, bar)
            win = mview(0, BF16, 8 * 2560).rearrange("p (k f) -> p k f", f=2560)
            wout = mview(40960, BF16, 8 * 1024).rearrange("p (k f) -> p k f", f=1024)
            for k0 in (0, 4):
                wload(win[:, k0:k0 + 4, :], o_w_in[k0 * 128:(k0 + 4) * 128, :].rearrange("(k p) f -> p k f", p=128), [win_b], ("win", k0))
            for k0 in (0, 4):
                wload(wout[:, k0:k0 + 4, :], o_w_out[k0 * 128:(k0 + 4) * 128, :].rearrange("(k p) f -> p k f", p=128), [wout_b], ("wout", k0))
            load_gb(2, final)
            W = 256
            offB = 57344
            cvT = [mview(offB + i * 4128, F32, 4 * 258).rearrange("p (c t) -> p c t", t=258) for i in range(2)]
            vct = [mview(offB + 8256 + i * 1024, F32, W) for i in range(2)]
            acc = mview(offB + 10304, F32, 4 * W).rearrange("p (c t) -> p c t", t=W)
            uT = mview(offB + 14400, BF16, 4 * W).rearrange("p (c t) -> p c t", t=W)
            vg = mview(offB + 16448, F32, 512)
            vln = [mview(offB + 18496 + i * 1024, BF16, 512) for i in range(2)]
            ycat = mview(offB + 20544, BF16, 8 * W).rearrange("p (c t) -> p c t", t=W)
            assert offB + 24640 <= M_BYTES
            tb = lambda: fresh_bufs(1, bar)[0]
            cv_b = [[tb() for _ in range(4)] for _ in range(2)]
            vct_b = [tb(), tb()]
            acc_b = [tb() for _ in range(4)]
            uT_b = [tb() for _ in range(4)]
            vg_b = tb()
            vln_b = [tb(), tb()]
            ycat_bs = [tb(), resid_b[0]]
            ycats = [ycat, resid.bitcast(BF16)[:, 0, :].rearrange("p (c t) -> p c t", t=W)]
            cload(sgg[:, 0, :], sgrows[0:1, :].partition_broadcast(128), bufs=[sgg_b])
            cload(sgg[:, 1, :], sgrows[1:2, :].partition_broadcast(128), bufs=[sgg_b])
            tiles = [(0, HALO, [0])] + [(HALO + W * i, W, [1 + 2 * i, 2 + 2 * i]) for i in range(8)]
            S.add(DVE, lambda e: e.memset(cvT[0][:, :, 0:2], 0.0), writes=cv_b[0])

            def Y_odd(ti):
                c0, w, subs = tiles[ti]
                out_proj(ycats[ti % 2], ycat_bs[ti % 2], wout, wout_b, subs, 0)
                for s in subs:
                    ln_update(s, final)
                    if not final:
                        gating(s)

            vcnt = 0
            for ti, (c0, w, subs) in enumerate(tiles):
                cur, nxt = ti % 2, (ti + 1) % 2
                xr = [xT_b[s] for s in subs]
                halo = (ti == 0)
                ycat, ycat_b = ycats[ti % 2], ycat_bs[ti % 2]
                for ch in range(4):
                    bk, bkb = ps_alloc()

                    def mm(e, bk=bk, ch=ch, c0=c0, w=w):
                        ins = None
                        for part, oc in ((0, 4 + ch), (1, 8 + ch)):
                            for k in range(8):
                                ins = e.matmul(bk[:, part * 256: part * 256 + w], lhsT=win[:, k, oc * 128:(oc + 1) * 128], rhs=xT[:, k, c0:c0 + w],
                                               start=(k == 0), stop=(k == 7))
                        return ins
                    S.add(PE, mm, reads=[win_b] + xr, writes=[bkb])
                    sp = ch % 2
                    if halo:
                        S.add(ACT, lambda e, bk=bk, w=w, sp=sp: e.activation(out=vct[sp][:, 0:w], in_=bk[:, 256:256 + w], func=AF.Identity, scale=halomask[:, 0:1]),
                              reads=[bkb, const_b], writes=[vct_b[sp]])
                    else:
                        S.add(ACT, lambda e, bk=bk, w=w, sp=sp: e.copy(out=vct[sp][:, 0:w], in_=bk[:, 256:256 + w]), reads=[bkb], writes=[vct_b[sp]])
                    S.add(DVE, lambda e, bk=bk, w=w, sp=sp, ch=ch, cur=cur: e.tensor_tensor(out=cvT[cur][:, ch, 2:2 + w], in0=bk[:, 0:w], in1=vct[sp][:, 0:w], op=ALU.mult),
                          reads=[bkb, vct_b[sp]], writes=[cv_b[cur][ch]])
                if ti + 1 < len(tiles):
                    S.add(ACT, lambda e, w=w, cur=cur, nxt=nxt: e.copy(out=cvT[nxt][:, :, 0:2], in_=cvT[cur][:, :, w:w + 2]), reads=cv_b[cur], writes=cv_b[nxt])
                if halo:
                    continue
                for ch in range(4):
                    S.add(DVE, lambda e, ch=ch, w=w, cur=cur: e.tensor_scalar(out=acc[:, ch, 0:w], in0=cvT[cur][:, ch, 0:w], scalar1=cvec[:, CC + ch * 3: CC + ch * 3 + 1],
                                                                             scalar2=None, op0=ALU.mult), reads=[cv_b[cur][ch], const_b], writes=[acc_b[ch]])
                    for j in (1, 2):
                        S.add(DVE, lambda e, ch=ch, w=w, cur=cur, j=j: e.scalar_tensor_tensor(
                            out=acc[:, ch, 0:w], in0=cvT[cur][:, ch, j:j + w], scalar=cvec[:, CC + ch * 3 + j: CC + ch * 3 + j + 1], in1=acc[:, ch, 0:w],
                            op0=ALU.mult, op1=ALU.add), reads=[cv_b[cur][ch], acc_b[ch], const_b], writes=[acc_b[ch]])
                for ch in range(4):
                    if ch % 2 == 0:
                        bk, bkb = ps_alloc()

                    def mm(e, bk=bk, ch=ch, c0=c0, w=w):
                        ins = None
                        for k in range(8):
                            ins = e.matmul(bk[:, (ch % 2) * 256:(ch % 2) * 256 + w], lhsT=win[:, k, ch * 128:(ch + 1) * 128], rhs=xT[:, k, c0:c0 + w],
                                           start=(k == 0), stop=(k == 7))
                        return ins
                    S.add(PE, mm, reads=[win_b] + xr, writes=[bkb])
                    S.add(DVE, lambda e, bk=bk, ch=ch, w=w, ycat=ycat: e.tensor_tensor(out=ycat[:, ch, 0:w], in0=bk[:, (ch % 2) * 256:(ch % 2) * 256 + w], in1=acc[:, ch, 0:w], op=ALU.mult),
                          reads=[bkb, acc_b[ch]], writes=[ycat_b])
                for ch in range(4):
                    if ch % 2 == 0:
                        bk, bkb = ps_alloc()

                    def mm(e, bk=bk, ch=ch, c0=c0, w=w):
                        ins = None
                        for k in range(8):
                            ins = e.matmul(bk[:, (ch % 2) * 256:(ch % 2) * 256 + w], lhsT=win[:, k, (12 + ch) * 128:(13 + ch) * 128], rhs=xT[:, k, c0:c0 + w],
                                           start=(k == 0), stop=(k == 7))
                        return ins
                    S.add(PE, mm, reads=[win_b] + xr, writes=[bkb])
                    S.add(ACT, lambda e, bk=bk, ch=ch, w=w: e.activation(out=uT[:, ch, 0:w], in_=bk[:, (ch % 2) * 256:(ch % 2) * 256 + w], func=AF.Gelu_apprx_tanh),
                          reads=[bkb], writes=[uT_b[ch]])
                for si, s in enumerate(subs):
                    cs = sub_col0(s)
                    vp = vcnt % 2
                    vcnt += 1
                    par = cnt["ln"] % 2
                    cnt["ln"] += 1
                    bk, bkb = ps_alloc()

                    def mm(e, bk=bk, cs=cs):
                        ins = None
                        for k in range(8):
                            ins = e.matmul(bk[:, :], lhsT=xT[:, k, cs:cs + 128], rhs=win[:, k, 2048:2560], start=(k == 0), stop=(k == 7))
                        return ins
                    S.add(PE, mm, reads=[win_b, xT_b[s]], writes=[bkb])
                    S.add(ACT, lambda e, bk=bk: e.activation(out=vg[:, :], in_=bk[:, :], func=AF.Gelu_apprx_tanh), reads=[bkb], writes=[vg_b])
                    S.add(DVE, lambda e, par=par: e.bn_stats(out=stats[:, par, 0, :], in_=vg[:, :]), reads=[vg_b], writes=[st_b[par]])
                    S.add(DVE, lambda e, par=par: e.bn_aggr(out=mv[:, par, :], in_=stats[:, par, 0:1, :]), reads=[st_b[par]], writes=[mv_b[par]])
                    S.add(ACT, lambda e, par=par: e.activation(out=sd[:, par, :], in_=mv[:, par, 1:2], func=AF.Sqrt, bias=epst[:, 0:1], scale=1.0),
                          reads=[mv_b[par], const_b], writes=[sd_b[par]])
                    S.add(DVE, lambda e, par=par: e.reciprocal(out=rstd[:, par, :], in_=sd[:, par, :]), reads=[sd_b[par]], writes=[rstd_b[par]])
                    S.add(DVE, lambda e, par=par: e.tensor_scalar(out=vg[:, :], in0=vg[:, :], scalar1=mv[:, par, 0:1], scalar2=rstd[:, par, :], op0=ALU.subtract, op1=ALU.mult),
                          reads=[vg_b, mv_b[par], rstd_b[par]], writes=[vg_b])
                    S.add(DVE, lambda e: e.tensor_tensor(out=vg[:, :], in0=vg[:, :], in1=sgg[:, 0, :], op=ALU.mult), reads=[vg_b, sgg_b], writes=[vg_b])
                    S.add(DVE, lambda e, vp=vp: e.tensor_tensor(out=vln[vp][:, :], in0=vg[:, :], in1=sgg[:, 1, :], op=ALU.add), reads=[vg_b, sgg_b], writes=[vln_b[vp]])
                    bk2, bk2b = ps_alloc()

                    def mm2(e, bk2=bk2, vp=vp):
                        ins = None
                        for h in range(4):
                            e.matmul(bk2[:, h * 128:(h + 1) * 128], lhsT=vln[vp][:, h * 128:(h + 1) * 128], rhs=wmT[:, h, :], start=True, stop=False)
                            e.matmul(bk2[:, h * 128:(h + 1) * 128], lhsT=onesb[0:1, :], rhs=bshl[0:1, 0, h * 128:(h + 1) * 128], start=False, stop=False)
                            ins = e.matmul(bk2[:, h * 128:(h + 1) * 128], lhsT=onesb[0:1, :], rhs=bshl[0:1, 1, h * 128:(h + 1) * 128], start=False, stop=True)
                        return ins
                    S.add(PE, mm2, reads=[vln_b[vp], wmT_b, const_b], writes=[bk2b])
                    loc = 128 * si
                    S.add(DVE, lambda e, bk2=bk2, loc=loc, ycat=ycat: e.tensor_tensor(out=ycat[:, 4:8, loc:loc + 128], in0=bk2[:, :].rearrange("p (h i) -> p h i", i=128),
                                                                           in1=uT[:, :, loc:loc + 128], op=ALU.mult), reads=[bk2b] + uT_b, writes=[ycat_b])
                if not PIPE:
                    Y_odd(ti)
                elif ti >= 2:
                    Y_odd(ti - 1)
            if PIPE:
                Y_odd(len(tiles) - 1)

        def gating(s):
            cs = sub_col0(s)
            gp = s % 2
            g = gsm[:, gp, :]
            lg, mx, msk, ex, nmx, ssum = g[:, 0:8], g[:, 8:16], g[:, 16:24], g[:, 24:32], g[:, 32:33], g[:, 33:34]
            bk, bkb = ps_alloc()

            def mm(e):
                ins = None
                for k in range(8):
                    ins = e.matmul(bk[:, 0:8], lhsT=xT[:, k, cs:cs + 128], rhs=rtr[:, k, :], start=(k == 0), stop=(k == 7))
                return ins
            S.add(PE, mm, reads=[xT_b[s], const_b], writes=[bkb])
            gb_ = gsm_b[gp]
            S.add(DVE, lambda e: e.tensor_copy(out=lg, in_=bk[:, 0:8]), reads=[bkb], writes=[gb_])
            S.add(DVE, lambda e: e.max(out=mx, in_=lg), reads=[gb_], writes=[gb_])
            S.add(DVE, lambda e: e.tensor_scalar(out=msk, in0=lg, scalar1=mx[:, 1:2], scalar2=None, op0=ALU.is_ge), reads=[gb_], writes=[gb_])
            S.add(DVE, lambda e: e.tensor_scalar(out=nmx, in0=mx[:, 0:1], scalar1=-1.0, scalar2=None, op0=ALU.mult), reads=[gb_], writes=[gb_])
            S.add(ACT, lambda e: e.activation(out=ex, in_=lg, func=AF.Exp, bias=nmx, scale=1.0), reads=[gb_], writes=[gb_])
            S.add(DVE, lambda e: e.tensor_tensor(out=ex, in0=ex, in1=msk, op=ALU.mult), reads=[gb_], writes=[gb_])
            S.add(DVE, lambda e: e.reduce_sum(out=ssum, in_=ex, axis=AX.X), reads=[gb_], writes=[gb_])
            S.add(DVE, lambda e: e.reciprocal(out=ssum, in_=ssum), reads=[gb_], writes=[gb_])
            S.add(DVE, lambda e: e.tensor_scalar(out=gates[:, s - 1, :], in0=ex, scalar1=ssum, scalar2=None, op0=ALU.mult), reads=[gb_], writes=[gates_b[s - 1]])
            m0 = g[:, 34:42]
            t8 = g[:, 8:16]
            S.add(DVE, lambda e: e.tensor_scalar(out=m0, in0=lg, scalar1=mx[:, 0:1], scalar2=None, op0=ALU.is_equal), reads=[gb_], writes=[gb_])
            S.add(DVE, lambda e: e.tensor_copy(out=maskall[:, s - 1, :], in_=msk), reads=[gb_], writes=[route_b])
            S.add(DVE, lambda e: e.tensor_copy(out=m0all[:, s - 1, :], in_=m0), reads=[gb_], writes=[route_b])
            S.add(DVE, lambda e: e.tensor_tensor(out=t8, in0=gates[:, s - 1, :], in1=m0, op=ALU.mult), reads=[gb_, gates_b[s - 1]], writes=[gb_])
            S.add(DVE, lambda e: e.reduce_sum(out=gsel[:, s - 1, 0:1], in_=t8, axis=AX.X), reads=[gb_], writes=[route_b])
            S.add(DVE, lambda e: e.tensor_tensor(out=t8, in0=msk, in1=m0, op=ALU.subtract), reads=[gb_], writes=[gb_])
            S.add(DVE, lambda e: e.tensor_tensor(out=t8, in0=t8, in1=gates[:, s - 1, :], op=ALU.mult), reads=[gb_, gates_b[s - 1]], writes=[gb_])
            S.add(DVE, lambda e: e.reduce_sum(out=gsel[:, s - 1, 1:2], in_=t8, axis=AX.X), reads=[gb_], writes=[route_b])

        def routing_finalize():
            cnts = gbt[:, 0:128].rearrange("p (s e) -> p s e", e=NEXP)
            offs = gbt[:, 128:256].rearrange("p (s e) -> p s e", e=NEXP)
            posc = gbt[:, 256:384]
            eoff = gbt[:, 384:512]
            tmpm = gbt[:, 512:640]
            dstf = gbt[:, 640:672].rearrange("p (r s) -> p r s", s=16)
            nmx = gbt[:, 672:673]
            S.add(SP, lambda e: e.dma_start(out=eoff, in_=eoff_d), writes=[gb_b[0]], dma_key=("c", "eoff"))
            S.add(DVE, lambda e: e.tensor_copy(out=maskb[:], in_=maskall[:, :, :].rearrange("p s e -> p (s e)")), reads=[route_b], writes=[route_b])
            bk, bkb = ps_alloc()

            def mm(e):
                e.matmul(bk[:, 0:128], lhsT=ustb[:], rhs=maskb[:], start=True, stop=True)
                return e.matmul(bk[:, 128:256], lhsT=ones128b[:], rhs=maskb[:], start=True, stop=True)
            S.add(PE, mm, reads=[route_b, const_b], writes=[bkb])
            S.add(DVE, lambda e: e.tensor_copy(out=cnts.rearrange("p s e -> p (s e)"), in_=bk[:, 128:256]), reads=[bkb, gb_b[0]], writes=[gb_b[0]])
            S.add(DVE, lambda e: e.memset(offs[:, 0, :], 0.0), reads=[gb_b[0]], writes=[gb_b[0]])
            for q in range(1, 16):
                S.add(DVE, lambda e, q=q: e.tensor_tensor(out=offs[:, q, :], in0=offs[:, q - 1, :], in1=cnts[:, q - 1, :], op=ALU.add), reads=[gb_b[0]], writes=[gb_b[0]])
            S.add(DVE, lambda e: e.tensor_tensor(out=posc, in0=bk[:, 0:128], in1=offs.rearrange("p s e -> p (s e)"), op=ALU.add), reads=[bkb, gb_b[0]], writes=[gb_b[0]])
            S.add(DVE, lambda e: e.tensor_tensor(out=posc, in0=posc, in1=eoff, op=ALU.add), reads=[gb_b[0]], writes=[gb_b[0]])
            S.add(DVE, lambda e: e.tensor_tensor(out=tmpm, in0=posc, in1=m0all[:, :, :].rearrange("p s e -> p (s e)"), op=ALU.mult), reads=[gb_b[0], route_b], writes=[gb_b[0]])
            S.add(DVE, lambda e: e.tensor_reduce(out=dstf[:, 0, :], in_=tmpm.rearrange("p (s e) -> p s e", e=NEXP), axis=AX.X, op=ALU.add), reads=[gb_b[0]], writes=[gb_b[0]])
            S.add(DVE, lambda e: e.tensor_tensor(out=tmpm, in0=maskall[:, :, :].rearrange("p s e -> p (s e)"), in1=m0all[:, :, :].rearrange("p s e -> p (s e)"), op=ALU.subtract),
                  reads=[gb_b[0], route_b], writes=[gb_b[0]])
            S.add(DVE, lambda e: e.tensor_tensor(out=tmpm, in0=tmpm, in1=posc, op=ALU.mult), reads=[gb_b[0]], writes=[gb_b[0]])
            S.add(DVE, lambda e: e.tensor_reduce(out=dstf[:, 1, :], in_=tmpm.rearrange("p (s e) -> p s e", e=NEXP), axis=AX.X, op=ALU.add), reads=[gb_b[0]], writes=[gb_b[0]])
            S.add(DVE, lambda e: e.tensor_copy(out=desti[:, :, :], in_=dstf), reads=[gb_b[0]], writes=[route_b])
            S.add(DVE, lambda e: e.tensor_tensor(out=tmpm[:, 0:8], in0=offs[:, 15, :], in1=cnts[:, 15, :], op=ALU.add), reads=[gb_b[0]], writes=[gb_b[0]])
            S.add(DVE, lambda e: e.reduce_max(out=nmx, in_=tmpm[:, 0:8], axis=AX.X), reads=[gb_b[0]], writes=[gb_b[0]])
            thr = -1.0 if force_dense else float(CAP)
            S.add(DVE, lambda e: e.tensor_scalar(out=nmx, in0=nmx, scalar1=thr, scalar2=None, op0=ALU.is_gt), reads=[gb_b[0]], writes=[gb_b[0]])
            return S.add(DVE, lambda e: e.tensor_copy(out=flagi[:, :], in_=nmx), reads=[gb_b[0]], writes=[route_b])

        def moe_sparse():
            bar = S.last_ops()
            wg_b = fresh_bufs(2, bar)
            wu_b = fresh_bufs(2, bar)
            wd_b = fresh_bufs(2, bar)
            hT_b = [fresh_bufs(4, bar) for _ in range(2)]
            sg_b = fresh_bufs(2, bar)
            yacc_b = fresh_bufs(NSL, bar)
            xcT_b = fresh_bufs(2, bar)
            xcs_b = fresh_bufs(1, bar)[0]
            wgv = [mview(i * 24576, BF16, 4096).rearrange("p (k f) -> p k f", f=512) for i in range(2)]
            wuv = [mview(i * 24576 + 8192, BF16, 4096).rearrange("p (k f) -> p k f", f=512) for i in range(2)]
            wdv = [mview(i * 24576 + 16384, BF16, 4096).rearrange("p (c d) -> p c d", d=1024) for i in range(2)]
            hTv = [mview(49152 + i * 5120, BF16, 4 * CAP).rearrange("p (c t) -> p c t", t=CAP) for i in range(2)]
            yacc = mview(59392, F32, NSL * D).rearrange("p (j d) -> p j d", d=D)
            assert 79872 <= M_BYTES
            sgv = [sgg[:, i, :] for i in range(2)]
            xTf = xT[:, :, :].rearrange("p k t -> p (k t)")
            xcT = [xTf[:, i * 8 * CAP:(i + 1) * 8 * CAP].rearrange("p (k t) -> p k t", t=CAP) for i in range(2)]
            xcs = xTf[:, 16 * CAP:16 * CAP + NSL * D].rearrange("p (j d) -> p j d", d=D)
            assert 16 * CAP + NSL * D <= 8 * NT
            sc_bufs = []
            for s in range(1, 17):
                par = cnt["ln"] % 2
                cnt["ln"] += 1
                S.add(ACT, lambda e, s=s, par=par: e.activation(out=xb[:, par, :], in_=resid[:, s, :], func=AF.Identity, scale=1.0 / ALPHA),
                      reads=[resid_b[s]], writes=[xb_b[par]])
                for r in range(2):
                    b = fresh_bufs(1, bar)[0]
                    sc_bufs.append(b)
                    S.add(POOL, lambda e, s=s, par=par, r=r: e.indirect_dma_start(
                        out=xc_d, out_offset=bass.IndirectOffsetOnAxis(ap=desti[:, r, s - 1:s], axis=0), in_=xb[:, par, :], in_offset=None),
                        reads=[xb_b[par], route_b], writes=[b], dma_key=("xsc", par, r))
            ftiles = [(0, 512), (512, CAP - 512)]
            groups = []
            items = []
            for ex in range(NEXP):
                for f0 in range(0, FF_EXP // 128, 4):
                    gi = len(groups)
                    groups.append((ex, f0, gi % 2))
                    for ti_, tl in enumerate(ftiles):
                        items.append((gi % 2, tl, ex, f0, gi if ti_ == 0 else None, ti_ == len(ftiles) - 1))

            def load_group(gi):
                ex, f0, slot = groups[gi]
                wload(wgv[slot][:, :, :], o_wg[ex][:, f0 * 128:(f0 + 4) * 128].rearrange("(k p) f -> p k f", p=128), [wg_b[slot]], ("wg", slot))
                wload(wuv[slot][:, :, :], o_wu[ex][:, f0 * 128:(f0 + 4) * 128].rearrange("(k p) f -> p k f", p=128), [wu_b[slot]], ("wu", slot))
                wload(wdv[slot][:, :, :], o_wd[ex][f0 * 128:(f0 + 4) * 128, :].rearrange("(c p) d -> p c d", p=128), [wd_b[slot]], ("wd", slot))

            def load_tokens_dma(ex):
                S.add(SP, lambda e, ex=ex: e.dma_start(out=xcs, in_=xc_d[ex * CAP:(ex + 1) * CAP, :].rearrange("(j p) d -> p j d", p=128)),
                      reads=sc_bufs, writes=[xcs_b], dma_key="xcs")

            def load_tokens_tr(ex):
                xp = ex % 2
                for j in range(NSL):
                    def tr(e, j=j):
                        ins = None
                        for c in range(8):
                            ins = e.transpose(out=pT[:, c, :], in_=xcs[:, j, c * 128:(c + 1) * 128], identity=identb[:])
                        return ins
                    S.add(PE, tr, reads=[xcs_b, const_b], writes=[pT_b])
                    S.add(ACT, lambda e, j=j, xp=xp: e.copy(out=xcT[xp][:, :, j * 128:(j + 1) * 128], in_=pT[:, :, :]), reads=[pT_b], writes=[xcT_b[xp]])

            def step1(it, hp):
                slot, (c0, w), ex, f0, _g, _l = it
                xp = ex % 2
                for fc in range(4):
                    bg, bgb = ps_alloc()
                    bu, bub = ps_alloc()

                    def mm(e, bg=bg, bu=bu, fc=fc, slot=slot, c0=c0, w=w, xp=xp):
                        ins = None
                        for k in range(8):
                            ins = e.matmul(bg[:, 0:w], lhsT=wgv[slot][:, k, fc * 128:(fc + 1) * 128], rhs=xcT[xp][:, k, c0:c0 + w], start=(k == 0), stop=(k == 7))
                        for k in range(8):
                            ins = e.matmul(bu[:, 0:w], lhsT=wuv[slot][:, k, fc * 128:(fc + 1) * 128], rhs=xcT[xp][:, k, c0:c0 + w], start=(k == 0), stop=(k == 7))
                        return ins
                    S.add(PE, mm, reads=[wg_b[slot], wu_b[slot], xcT_b[xp]], writes=[bgb, bub])
                    sp = fc % 2
                    S.add(ACT, lambda e, bg=bg, w=w, sp=sp: e.activation(out=sgv[sp][:, 0:w], in_=bg[:, 0:w], func=AF.Silu), reads=[bgb], writes=[sg_b[sp]])
                    S.add(DVE, lambda e, bu=bu, w=w, sp=sp, fc=fc, hp=hp, c0=c0: e.tensor_tensor(out=hTv[hp][:, fc, c0:c0 + w], in0=bu[:, 0:w], in1=sgv[sp][:, 0:w], op=ALU.mult),
                          reads=[bub, sg_b[sp]], writes=[hT_b[hp][fc]])

            def step2(gi, hp):
                ex, f0, slot = groups[gi]
                first = (f0 == 0)
                last = (f0 + 4 == FF_EXP // 128)
                for j in range(NSL):
                    for half in range(2):
                        bk, bkb = ps_alloc()

                        def mm(e, bk=bk, j=j, half=half, slot=slot, hp=hp):
                            ins = None
                            for fc in range(4):
                                ins = e.matmul(bk[:, :], lhsT=hTv[hp][:, fc, j * 128:(j + 1) * 128], rhs=wdv[slot][:, fc, half * 512:(half + 1) * 512],
                                               start=(fc == 0), stop=(fc == 3))
                            return ins
                        S.add(PE, mm, reads=hT_b[hp] + [wd_b[slot]], writes=[bkb])
                        ya = yacc[:, j, half * 512:(half + 1) * 512]
                        if first:
                            S.add(ACT, lambda e, bk=bk, ya=ya: e.copy(out=ya, in_=bk[:, :]), reads=[bkb], writes=[yacc_b[j]])
                        else:
                            S.add(DVE, lambda e, bk=bk, ya=ya: e.tensor_tensor(out=ya, in0=ya, in1=bk[:, :], op=ALU.add), reads=[bkb, yacc_b[j]], writes=[yacc_b[j]])
                if last:
                    b = fresh_bufs(1, [])[0]
                    yst_bufs.append(b)
                    S.add(SP, lambda e, ex=ex: e.dma_start(out=yc_d[ex * CAP:(ex + 1) * CAP, :].rearrange("(j p) d -> p j d", p=128), in_=yacc[:, :, :]),
                          reads=yacc_b, writes=[b], dma_key="yst")

            yst_bufs = []
            load_group(0)
            load_tokens_dma(0)
            load_tokens_tr(0)
            pend = None
            for i, it in enumerate(items):
                slot, tl, ex, f0, gfirst, glast = it
                gi = ex * (FF_EXP // 512) + f0 // 4
                step1(it, gi % 2)
                if gfirst is not None:
                    if pend is not None:
                        step2(pend, pend % 2)
                        pend = None
                    if gi + 1 < len(groups):
                        load_group(gi + 1)
                    if f0 == 0 and ex + 1 < NEXP:
                        load_tokens_dma(ex + 1)
                    if f0 == 12 and ex + 1 < NEXP:
                        load_tokens_tr(ex + 1)
                if glast:
                    pend = gi
            step2(pend, pend % 2)
            for s in range(1, 17):
                for r in range(2):
                    j = (2 * s + r) % NSL
                    S.add(POOL, lambda e, s=s, r=r, j=j: e.indirect_dma_start(
                        out=yacc[:, j, :], out_offset=None, in_=yc_d, in_offset=bass.IndirectOffsetOnAxis(ap=desti[:, r, s - 1:s], axis=0)),
                        reads=yst_bufs + [route_b], writes=[yacc_b[j]], dma_key=("ygt", j))
                    S.add(DVE, lambda e, s=s, r=r, j=j: e.scalar_tensor_tensor(out=resid[:, s, :], in0=yacc[:, j, :], scalar=gsel[:, s - 1, r:r + 1], in1=resid[:, s, :],
                                                                               op0=ALU.mult, op1=ALU.add), reads=[yacc_b[j], resid_b[s], route_b], writes=[resid_b[s]])

        mixer_even(final=(n_sub == 1))
        if n_sub >= 2:
            load_gb(1, n_sub == 2)
            ffn([(e_wg, e_wu, e_wd, FF_DENSE, None)], with_halo=True, final=(n_sub == 2))
            bar_ffn0 = S.last_ops()
            for s in range(17):
                ln_update(s, n_sub == 2)
        if n_sub >= 3:
            mixer_odd(final=(n_sub == 3), bar=bar_ffn0)
        if n_sub >= 4:
            flag_op = routing_finalize()
            load_gb(3, True)
            if moe_mode in ("both", "dense"):
                if moe_mode == "both":
                    S.begin_cond(flagi[0:1, 0:1], flag_op, True)
                ffn([(o_wg[e], o_wu[e], o_wd[e], FF_EXP, e) for e in range(NEXP)], with_halo=False, final=True)
                S.end_cond()
            if moe_mode in ("both", "sparse"):
                if moe_mode == "both":
                    S.begin_cond(flagi[0:1, 0:1], flag_op, False)
                moe_sparse()
                S.end_cond()
            for s in range(1, 17):
                ln_update(s, True)
        S.emit(final_wait_ops=out_ops)
    return nc


def _make_in_maps(inp):
    f = lambda a: np.ascontiguousarray(np.asarray(a, dtype=np.float32))
    x = f(inp["x"])

    def pc(v):
        v = f(v)
        return v.reshape(-1, 128).T

    conv_a_w = f(inp["even_conv_a_w"])[0]
    cw = conv_a_w.T.reshape(4, 128, 31).transpose(1, 0, 2).reshape(128, 124)
    conv_c_w = f(inp["odd_conv_c_w"])[0]
    cc = conv_c_w.T.reshape(4, 128, 3).transpose(1, 0, 2).reshape(128, 12)
    cvec = np.concatenate([cw, pc(inp["even_conv_a_b"][0]), pc(inp["even_norm_a_g"][0]), pc(inp["even_norm_a_b"][0]),
                           pc(inp["even_pool_scale"][0]), cc], axis=1)
    cvec = np.ascontiguousarray(cvec, dtype=np.float32)
    assert cvec.shape == (128, 152)
    lnrows = np.stack([f(inp[k])[0] for k in ("even_ln1_g", "even_ln1_b", "even_ln2_g", "even_ln2_b", "odd_ln1_g", "odd_ln1_b", "odd_ln2_g", "odd_ln2_b")])
    sgrows = np.stack([f(inp["odd_sgu_norm_g"])[0], f(inp["odd_sgu_norm_b"])[0], f(inp["odd_sgu_b"])[0].reshape(512)])
    ident = np.eye(128, dtype=np.float32)
    tril = np.triu(np.ones((128, 128), dtype=np.float32))
    ustrict = np.triu(np.ones((128, 128), dtype=np.float32), k=1)
    eoff = np.ascontiguousarray(np.broadcast_to(np.tile(np.arange(NEXP, dtype=np.float32) * CAP, 16).reshape(1, 128), (128, 128)))
    common = {
        "e_w_in": f(inp["even_w_in"])[0], "e_w_out": f(inp["even_w_out"])[0],
        "e_wg": f(inp["even_ffn_w_gate"])[0], "e_wu": f(inp["even_ffn_w_up"])[0], "e_wd": f(inp["even_ffn_w_down"])[0],
        "o_w_in": f(inp["odd_w_in"])[0], "o_w_out": f(inp["odd_w_out"])[0], "o_router": f(inp["odd_router"])[0],
        "o_wg": f(inp["odd_moe_w_gate"])[0], "o_wu": f(inp["odd_moe_w_up"])[0], "o_wd": f(inp["odd_moe_w_down"])[0],
        "lnrows": np.ascontiguousarray(lnrows), "sgrows": np.ascontiguousarray(sgrows), "cvec": cvec,
        "pool_w": f(inp["even_pool_w"])[0], "sgu_w": f(inp["odd_sgu_w"])[0], "ident": ident, "tril": tril, "ustrict": ustrict, "eoff": eoff,
    }
    corr0 = np.ones((4, 16), dtype=np.float32)
    for g in range(4):
        win = 2 ** (g + 1)
        for t in range(16):
            corr0[g, t] = win / min(t + 1, win)
    maps = []
    for c in range(8):
        b, half = c // 2, c % 2
        xc = np.zeros((NT, D), dtype=np.float32)
        if half == 0:
            xc[HALO:] = x[b, 0:T]
            corr = corr0
            hm = 0.0
        else:
            xc[:] = x[b, T - HALO:2 * T]
            corr = np.ones((4, 16), dtype=np.float32)
            hm = 1.0
        m = dict(common)
        m["x_c"] = xc
        m["poolcorr"] = np.ascontiguousarray(np.broadcast_to(corr.reshape(1, 64), (128, 64)), dtype=np.float32)
        m["halomask"] = np.full((128, 1), hm, dtype=np.float32)
        maps.append(m)
    return maps


_NC_CACHE = {}


N_SUB = 4


def kernel(**inputs):
    maps = _make_in_maps(inputs)
    if "nc" not in _NC_CACHE:
        _NC_CACHE["nc"] = build_nc(N_SUB)
    res = run_bass_kernel_spmd(_NC_CACHE["nc"], maps, core_ids=list(range(8)))
    out = np.empty((4, 2 * T, D), dtype=np.float32)
    for c in range(8):
        b, half = c // 2, c % 2
        out[b, half * T:(half + 1) * T] = res.results[c]["out"]
    return out
```

```python
import contextlib
import numpy as np
import concourse.bass as bass
import concourse.mybir as mybir
from concourse.bass_utils import run_bass_kernel_spmd

F32 = mybir.dt.float32
BF16 = mybir.dt.bfloat16
AF = mybir.ActivationFunctionType
ALU = mybir.AluOpType
AX = mybir.AxisListType

PE, ACT, DVE, POOL, SP = "tensor", "scalar", "vector", "gpsimd", "sync"
ENGS = [PE, ACT, DVE, POOL, SP]

HALO = 32
T = 2048
NT = HALO + T
D = 1024
ALPHA = 4.0 ** 0.25
EPS = 1e-5
FF_DENSE = 2816
FF_EXP = 3584
NEXP = 8
CAP = 640
NSL = CAP // 128
I32 = mybir.dt.int32
MOE_MODE = "both"
FORCE_DENSE = False
PIPE = True


class Buf:
    __slots__ = ("name", "writer", "readers")

    def __init__(self, name=""):
        self.name = name
        self.writer = None
        self.readers = []


class Op:
    __slots__ = ("eng", "fn", "deps", "dma", "sem", "val", "signal", "cond")

    def __init__(self, eng, fn, dma):
        self.eng = eng
        self.fn = fn
        self.deps = []
        self.dma = dma
        self.sem = None
        self.val = 0
        self.signal = False
        self.cond = None


class Sched:
    def __init__(self, nc):
        self.nc = nc
        self.ops = {e: [] for e in ENGS}
        self.all_ops = []
        self.cur_cond = None
        self.conds = []

    def begin_cond(self, flag_ap, flag_op, sense):
        flag_op.signal = True
        self.conds.append((flag_ap, flag_op, sense))
        self.cur_cond = len(self.conds) - 1

    def end_cond(self):
        self.cur_cond = None

    def add(self, eng, fn, reads=(), writes=(), dma_key=None):
        op = Op(eng, fn, dma_key)
        op.cond = self.cur_cond
        deps = []
        for r in reads:
            if r.writer is not None:
                deps.append(r.writer)
        for w in writes:
            if w.writer is not None:
                deps.append(w.writer)
            deps.extend(w.readers)
        seen = set()
        for d in deps:
            if d is op or id(d) in seen:
                continue
            seen.add(id(d))
            if d.eng == PE and eng == PE and d.dma is None and dma_key is None:
                continue
            op.deps.append(d)
            d.signal = True
        for r in reads:
            r.readers.append(op)
        for w in writes:
            w.writer = op
            w.readers = []
        self.ops[eng].append(op)
        self.all_ops.append(op)
        return op

    def last_ops(self):
        return [self.ops[e][-1] for e in ENGS if self.ops[e]]

    def emit(self, final_wait_ops=()):
        nc = self.nc
        eng_cnt = {e: 0 for e in ENGS}
        dma_keys = {}
        for op in self.all_ops:
            if op.dma is not None:
                ent = dma_keys.setdefault(op.dma, [len(dma_keys), 0])
                ent[1] += 16
                op.sem = ("dma", ent[0])
                op.val = ent[1]
                op.signal = True
            elif op.signal:
                eng_cnt[op.eng] += 1
                op.sem = ("eng", op.eng)
                op.val = eng_cnt[op.eng]
        with contextlib.ExitStack() as st:
            sems = {}
            for e in ENGS:
                sems[("eng", e)] = st.enter_context(nc.semaphore("s_" + e))
            for i in range(len(dma_keys)):
                sems[("dma", i)] = st.enter_context(nc.semaphore("d_%d" % i))
            block = st.enter_context(nc.Block())
            for e in ENGS:
                ops = self.ops[e]

                def body(engobj, ops=ops, e=e):
                    waited = {}

                    def run(op):
                        for d in op.deps:
                            if waited.get(d.sem, 0) >= d.val:
                                continue
                            waited[d.sem] = d.val
                            engobj.wait_ge(sems[d.sem], d.val)
                        ins = op.fn(engobj)
                        if op.signal:
                            ins.then_inc(sems[op.sem], 16 if op.dma is not None else 1)

                    i = 0
                    own_val = 0
                    dma_val = {}
                    while i < len(ops):
                        op = ops[i]
                        if op.cond is None:
                            run(op)
                            if op.dma is not None:
                                dma_val[op.sem] = op.val
                            elif op.signal:
                                own_val = op.val
                            i += 1
                            continue
                        j = i
                        while j < len(ops) and ops[j].cond == op.cond:
                            j += 1
                        blk = ops[i:j]
                        flag_ap, flag_op, sense = self.conds[op.cond]
                        if waited.get(flag_op.sem, 0) < flag_op.val:
                            waited[flag_op.sem] = flag_op.val
                            engobj.wait_ge(sems[flag_op.sem], flag_op.val)
                        creg = engobj.alloc_register("cflag%d_%d" % (op.cond, i))
                        engobj.reg_load(creg, flag_ap)
                        snap_waited = dict(waited)
                        base_own = own_val
                        base_dma = dict(dma_val)
                        n_own = 0
                        n_dma = {}
                        with (engobj.If(creg) if sense else engobj.If_eq(creg, 0)):
                            for bop in blk:
                                run(bop)
                                if bop.dma is not None:
                                    n_dma[bop.sem] = n_dma.get(bop.sem, 0) + 16
                                    dma_val[bop.sem] = bop.val
                                elif bop.signal:
                                    n_own += 1
                                    own_val = bop.val
                        with engobj.Else():
                            if n_own:
                                engobj.wait_ge(sems[("eng", e)], base_own)
                                engobj.sem_inc(sems[("eng", e)], n_own)
                            for k, n in n_dma.items():
                                engobj.wait_ge(sems[k], base_dma.get(k, 0))
                                engobj.sem_inc(sems[k], n)
                        waited.clear()
                        waited.update(snap_waited)
                        i = j
                    if e == SP:
                        for op in final_wait_ops:
                            if waited.get(op.sem, 0) >= op.val:
                                continue
                            waited[op.sem] = op.val
                            engobj.wait_ge(sems[op.sem], op.val)

                getattr(block, e)(body)


def sub_rows(s):
    return HALO if s == 0 else 128


def sub_col0(s):
    return 0 if s == 0 else HALO + 128 * (s - 1)


def build_nc(n_sub=4, moe_mode=None, force_dense=None):
    nc = bass.Bass("TRN2", target_bir_lowering=False)
    moe_mode = MOE_MODE if moe_mode is None else moe_mode
    force_dense = FORCE_DENSE if force_dense is None else force_dense

    def din(name, shape):
        return nc.dram_tensor(name, list(shape), F32, kind="ExternalInput").ap()

    x_c = din("x_c", [NT, D])
    e_w_in = din("e_w_in", [D, 1536])
    e_w_out = din("e_w_out", [D, D])
    e_wg = din("e_wg", [D, FF_DENSE])
    e_wu = din("e_wu", [D, FF_DENSE])
    e_wd = din("e_wd", [FF_DENSE, D])
    o_w_in = din("o_w_in", [D, 2560])
    o_w_out = din("o_w_out", [D, D])
    o_router = din("o_router", [D, NEXP])
    o_wg = din("o_wg", [NEXP, D, FF_EXP])
    o_wu = din("o_wu", [NEXP, D, FF_EXP])
    o_wd = din("o_wd", [NEXP, FF_EXP, D])
    lnrows = din("lnrows", [8, D])
    sgrows = din("sgrows", [3, 512])
    cvec_d = din("cvec", [128, 152])
    pool_w_d = din("pool_w", [4, 128, 128])
    sgu_w_d = din("sgu_w", [4, 128, 128])
    ident_d = din("ident", [128, 128])
    tril_d = din("tril", [128, 128])
    poolcorr_d = din("poolcorr", [128, 64])
    halomask_d = din("halomask", [128, 1])
    ustrict_d = din("ustrict", [128, 128])
    eoff_d = din("eoff", [128, 128])
    out_d = nc.dram_tensor("out", [T, D], F32, kind="ExternalOutput").ap()
    xc_d = nc.dram_tensor("xc_scr", [NEXP * CAP, D], BF16, kind="Internal").ap()
    yc_d = nc.dram_tensor("yc_scr", [NEXP * CAP, D], F32, kind="Internal").ap()

    st = contextlib.ExitStack()
    with st:
        S = Sched(nc)

        def sb(name, shape, dt):
            return st.enter_context(nc.sbuf_tensor(name, list(shape), dt))

        resid = sb("resid", [128, 17, D], F32)
        xT = sb("xT", [128, 8, NT], BF16)
        gbt = sb("gb", [128, 2 * D], F32)
        gb = gbt[:, :].rearrange("p (i d) -> p i d", d=D)
        xb = sb("xb", [128, 2, D], BF16)
        identf = sb("identf", [128, 128], F32)
        identb = sb("identb", [128, 128], BF16)
        onesf = sb("onesf", [128, 128], F32)
        onesb = sb("onesb", [1, 128], BF16)
        epst = sb("epst", [128, 1], F32)
        cvec = sb("cvec_s", [128, 152], F32)
        poolw = sb("poolw", [128, 4, 128], BF16)
        sguwf = gbt[:, D:D + 512].rearrange("p (h j) -> p h j", j=128)
        trilf = gbt[:, D + 512:D + 640]
        wmT = sb("wmT", [128, 4, 128], BF16)
        sgg = sb("sgg", [128, 2, 512], F32)
        bsrow = gbt[0:1, 0:1024].rearrange("p (a b) -> p a b", b=512)
        bshl = sb("bshl", [1, 2, 512], BF16)
        poolcorr = sb("poolcorr_s", [128, 64], F32)
        halomask = sb("halomask_s", [128, 1], F32)
        rtr = sb("rtr", [128, 8, NEXP], BF16)
        gates = sb("gates", [128, 16, NEXP], F32)
        stats = sb("stats", [128, 2, 2, 6], F32)
        mv = sb("mv", [128, 2, 2], F32)
        sd = sb("sd", [128, 2, 1], F32)
        rstd = sb("rstd", [128, 2, 1], F32)
        nb = sb("nb", [128, 2, 1], F32)
        gsm = sb("gsm", [128, 2, 48], F32)
        maskall = sb("maskall", [128, 16, NEXP], F32)
        m0all = sb("m0all", [128, 16, NEXP], F32)
        maskb = sb("maskb", [128, 128], BF16)
        ustb = sb("ustb", [128, 128], BF16)
        ones128b = sb("ones128b", [128, 128], BF16)
        gsel = sb("gsel", [128, 16, 2], F32)
        desti = sb("desti", [128, 2, 16], I32)
        flagi = sb("flagi", [128, 1], I32)
        M_BYTES = 81984
        Mr = sb("Mr", [128, M_BYTES // 2], BF16)
        Mf = Mr.bitcast(F32)

        def mview(off_bytes, dt, n):
            if dt == F32:
                assert off_bytes % 4 == 0
                return Mf[:, off_bytes // 4: off_bytes // 4 + n]
            assert off_bytes % 2 == 0
            return Mr[:, off_bytes // 2: off_bytes // 2 + n]

        pT = st.enter_context(nc.psum_tensor("pT", [128, 8, 128], BF16))
        banks = [st.enter_context(nc.psum_tensor("bank%d" % i, [128, 512], F32)) for i in range(7)]
        bank_bufs = [Buf("bank%d" % i) for i in range(7)]
        pT_b = Buf("pT")
        ring = [0]

        def ps_alloc():
            i = ring[0] % 7
            ring[0] += 1
            return banks[i], bank_bufs[i]

        resid_b = [Buf("resid%d" % s) for s in range(17)]
        xT_b = [Buf("xT%d" % s) for s in range(17)]
        gb_b = [Buf("gbg"), Buf("gbb")]
        xb_b = [Buf("xb0"), Buf("xb1")]
        st_b = [Buf(), Buf()]
        mv_b = [Buf(), Buf()]
        sd_b = [Buf(), Buf()]
        rstd_b = [Buf(), Buf()]
        nb_b = [Buf(), Buf()]
        gsm_b = [Buf(), Buf()]
        const_b = Buf("consts")
        gates_b = [Buf("gates%d" % s) for s in range(16)]
        route_b = Buf("route")
        sgg_b = Buf("sgg")
        wmT_b = Buf("wmT")
        cnt = {"ln": 0, "x": 0}
        out_ops = []

        cparts = []

        def cbuf():
            b = Buf()
            cparts.append(b)
            return b

        def cload(dst, src, eng=SP, bufs=None):
            key = ("c", cnt["x"])
            cnt["x"] += 1
            return S.add(eng, lambda e: e.dma_start(out=dst, in_=src), writes=(bufs if bufs is not None else [cbuf()]), dma_key=key)

        win0_b, wout0_b = Buf("win0"), Buf("wout0")
        win0 = mview(0, BF16, 8 * 1536).rearrange("p (k f) -> p k f", f=1536)
        wout0 = mview(40960, BF16, 8 * 1024).rearrange("p (k f) -> p k f", f=1024)
        for k0 in (0, 4):
            S.add(POOL, lambda e, k0=k0: e.dma_start(out=win0[:, k0:k0 + 4, :], in_=e_w_in[k0 * 128:(k0 + 4) * 128, :].rearrange("(k p) f -> p k f", p=128)),
                  writes=[win0_b], dma_key=("win", k0))
        for k0 in (0, 4):
            S.add(POOL, lambda e, k0=k0: e.dma_start(out=wout0[:, k0:k0 + 4, :], in_=e_w_out[k0 * 128:(k0 + 4) * 128, :].rearrange("(k p) f -> p k f", p=128)),
                  writes=[wout0_b], dma_key=("wout", k0))

        identf_b = cbuf()
        cload(identf[:], ident_d, bufs=[identf_b])
        cload(cvec[:], cvec_d)
        cload(trilf, tril_d, bufs=[gb_b[1]])
        cload(poolcorr[:], poolcorr_d)
        cload(halomask[:], halomask_d)
        cload(sguwf, sgu_w_d.rearrange("h i j -> i h j"), bufs=[gb_b[1]])
        cload(bsrow[:, 0, :], sgrows[2:3, :], bufs=[gb_b[0]])
        cload(poolw[:], pool_w_d.rearrange("g c d -> c g d"), eng=POOL)
        cload(rtr[:], o_router.rearrange("(k p) e -> p k e", p=128), eng=POOL)
        S.add(DVE, lambda e: e.memset(onesf[:], 1.0), writes=[cbuf()])
        S.add(DVE, lambda e: e.memset(ones128b[:], 1.0), writes=[cbuf()])
        cload(ustb[:], ustrict_d, eng=POOL)
        S.add(DVE, lambda e: e.memset(onesb[:], 1.0), writes=[cbuf()])
        S.add(DVE, lambda e: e.memset(epst[:], EPS), writes=[cbuf()])
        identb_b = cbuf()
        S.add(DVE, lambda e: e.tensor_copy(out=identb[:], in_=identf[:]), reads=[identf_b], writes=[identb_b])
        bshl_b = cbuf()
        S.add(DVE, lambda e: e.tensor_copy(out=bshl[:, 0, :], in_=bsrow[:, 0, :]), reads=[gb_b[0]], writes=[bshl_b])
        S.add(DVE, lambda e: e.tensor_copy(out=bsrow[:, 1, :], in_=bshl[:, 0, :]), reads=[bshl_b], writes=[gb_b[0]])
        S.add(DVE, lambda e: e.tensor_tensor(out=bsrow[:, 0, :], in0=bsrow[:, 0, :], in1=bsrow[:, 1, :], op=ALU.subtract), reads=[gb_b[0]], writes=[gb_b[0]])
        S.add(DVE, lambda e: e.tensor_copy(out=bshl[:, 1, :], in_=bsrow[:, 0, :]), reads=[gb_b[0]], writes=[bshl_b])
        for h in range(4):
            bk, bkb = ps_alloc()
            S.add(PE, lambda e, h=h, bk=bk: e.transpose(out=bk[:, 0:128], in_=sguwf[:, h, :], identity=identf[:]), reads=[identf_b, gb_b[1]], writes=[bkb])
            S.add(DVE, lambda e, h=h, bk=bk: e.tensor_tensor(out=wmT[:, h, :], in0=bk[:, 0:128], in1=trilf, op=ALU.mult), reads=[bkb, gb_b[1]], writes=[wmT_b])
        S.add(DVE, lambda e: e.memset(nb[:, 0, :], 0.0), reads=cparts, writes=[const_b, nb_b[0]])

        def load_gb(k, final):
            for i in range(2):
                S.add(SP, lambda e, i=i: e.dma_start(out=gb[:, i, :], in_=lnrows[2 * k + i: 2 * k + i + 1, :].partition_broadcast(128)),
                      writes=[gb_b[i]], dma_key=("gb", i))
                if not final:
                    S.add(DVE, lambda e, i=i: e.tensor_scalar(out=gb[:, i, :], in0=gb[:, i, :], scalar1=ALPHA, scalar2=None, op0=ALU.mult),
                          reads=[gb_b[i]], writes=[gb_b[i]])

        def make_xT(s, par):
            rows, c0 = sub_rows(s), sub_col0(s)

            def tr(e):
                ins = None
                for c in range(8):
                    ins = e.transpose(out=pT[:, c, 0:rows], in_=xb[0:rows, par, c * 128:(c + 1) * 128], identity=identb[0:rows, 0:rows])
                return ins
            S.add(PE, tr, reads=[xb_b[par], identb_b], writes=[pT_b])
            S.add(ACT, lambda e: e.copy(out=xT[:, :, c0:c0 + rows], in_=pT[:, :, 0:rows]), reads=[pT_b], writes=[xT_b[s]])

        def ln_update_multi(subs, final):
            assert len(subs) <= 2
            info = [(s, sub_rows(s), i % 2) for i, s in enumerate(subs)]
            for s, rows, par in info:
                def bn(e, s=s, rows=rows, par=par):
                    e.bn_stats(out=stats[0:rows, par, 0, :], in_=resid[0:rows, s, 0:512])
                    return e.bn_stats(out=stats[0:rows, par, 1, :], in_=resid[0:rows, s, 512:1024])
                S.add(DVE, bn, reads=[resid_b[s]], writes=[st_b[par]])
            for s, rows, par in info:
                S.add(DVE, lambda e, rows=rows, par=par: e.bn_aggr(out=mv[0:rows, par, :], in_=stats[0:rows, par, :, :]), reads=[st_b[par]], writes=[mv_b[par]])
            for s, rows, par in info:
                S.add(ACT, lambda e, rows=rows, par=par: e.activation(out=sd[0:rows, par, :], in_=mv[0:rows, par, 1:2], func=AF.Sqrt, bias=epst[0:rows, 0:1], scale=1.0),
                      reads=[mv_b[par], const_b], writes=[sd_b[par]])
            for s, rows, par in info:
                S.add(DVE, lambda e, s=s, rows=rows, par=par: e.scalar_tensor_tensor(
                    out=resid[0:rows, s, :], in0=resid[0:rows, s, :], scalar=mv[0:rows, par, 0:1], in1=gb[0:rows, 0, :], op0=ALU.subtract, op1=ALU.mult),
                    reads=[resid_b[s], mv_b[par], gb_b[0]], writes=[resid_b[s]])
            for s, rows, par in info:
                S.add(DVE, lambda e, rows=rows, par=par: e.reciprocal(out=rstd[0:rows, par, :], in_=sd[0:rows, par, :]), reads=[sd_b[par]], writes=[rstd_b[par]])
            for s, rows, par in info:
                S.add(DVE, lambda e, s=s, rows=rows, par=par: e.scalar_tensor_tensor(
                    out=resid[0:rows, s, :], in0=resid[0:rows, s, :], scalar=rstd[0:rows, par, :], in1=gb[0:rows, 1, :], op0=ALU.mult, op1=ALU.add),
                    reads=[resid_b[s], rstd_b[par], gb_b[1]], writes=[resid_b[s]])
            for s, rows, par in info:
                if final:
                    if s >= 1:
                        op = S.add(SP, lambda e, s=s: e.dma_start(out=out_d[128 * (s - 1):128 * s, :], in_=resid[:, s, :]), reads=[resid_b[s]], dma_key=("out", s))
                        out_ops.append(op)
                else:
                    S.add(ACT, lambda e, s=s, rows=rows, par=par: e.activation(out=xb[0:rows, par, :], in_=resid[0:rows, s, :], func=AF.Identity, scale=1.0 / ALPHA),
                          reads=[resid_b[s]], writes=[xb_b[par]])
            if not final:
                for s, rows, par in info:
                    make_xT(s, par)

        def ln_update(s, final):
            ln_update_multi([s], final)

        def wload(dst, src, wbufs, key):
            return S.add(POOL, lambda e: e.dma_start(out=dst, in_=src), writes=wbufs, dma_key=key)

        def fresh_bufs(n, barrier):
            bl = []
            for i in range(n):
                b = Buf()
                b.readers = list(barrier)
                bl.append(b)
            return bl

        for s in range(17):
            rows, c0 = sub_rows(s), sub_col0(s)
            par = s % 2
            S.add(SP, lambda e, s=s, rows=rows, c0=c0: e.dma_start(out=resid[0:rows, s, :], in_=x_c[c0:c0 + rows, :]), writes=[resid_b[s]], dma_key=("x", s))
            S.add(ACT, lambda e, s=s, rows=rows, par=par: e.copy(out=xb[0:rows, par, :], in_=resid[0:rows, s, :]), reads=[resid_b[s]], writes=[xb_b[par]])
            make_xT(s, par)
            S.add(DVE, lambda e, s=s, rows=rows: e.tensor_scalar(out=resid[0:rows, s, :], in0=resid[0:rows, s, :], scalar1=ALPHA, scalar2=None, op0=ALU.mult),
                  reads=[resid_b[s]], writes=[resid_b[s]])

        CW, CB, NG, NB_, PS, CC = 0, 124, 128, 132, 136, 140

        def out_proj(ycat, ycat_b, wout, wout_b, subs, loc0):
            for si, s in enumerate(subs):
                rows = sub_rows(s)
                loc = loc0 + 128 * si
                for half in range(2):
                    bk, bkb = ps_alloc()

                    def mm(e, bk=bk, rows=rows, loc=loc, half=half):
                        ins = None
                        for k in range(8):
                            ins = e.matmul(bk[0:rows, :], lhsT=ycat[:, k, loc:loc + rows], rhs=wout[:, k, half * 512:(half + 1) * 512],
                                           start=(k == 0), stop=(k == 7))
                        return ins
                    S.add(PE, mm, reads=[ycat_b, wout_b], writes=[bkb])
                    S.add(DVE, lambda e, bk=bk, rows=rows, s=s, half=half: e.tensor_tensor(
                        out=resid[0:rows, s, half * 512:(half + 1) * 512], in0=resid[0:rows, s, half * 512:(half + 1) * 512], in1=bk[0:rows, :], op=ALU.add),
                        reads=[bkb, resid_b[s]], writes=[resid_b[s]])

        def mixer_even(final):
            bar = []
            win_b, wout_b = win0_b, wout0_b
            tmp_bar = bar
            win, wout = win0, wout0
            load_gb(0, final)
            W = 256
            offA = 24576
            aT = [mview(offA + i * 4576, F32, 4 * 286).rearrange("p (c t) -> p c t", t=286) for i in range(2)]
            acc = mview(offA + 9152, F32, 4 * W).rearrange("p (c t) -> p c t", t=W)
            sigt = [mview(offA + 13248 + i * 1024, F32, W) for i in range(2)]
            assert 15296 <= 16384
            offB = 57344
            sqt = mview(offB, F32, 4 * W).rearrange("p (c t) -> p c t", t=W)
            meant = mview(offB + 4096, F32, W)
            vart = mview(offB + 5120, F32, W)
            rstt = mview(offB + 6144, F32, W)
            hbT = [mview(offB + 7168 + i * 4336, F32, 4 * 271).rearrange("p (c t) -> p c t", t=271) for i in range(2)]
            tmpA = mview(offB + 15840, F32, 271)
            tmpB = mview(offB + 16924, F32, 271)
            pooled = mview(offB + 18008, BF16, 4 * W).rearrange("p (c t) -> p c t", t=W)
            ycat = mview(offB + 20056, BF16, 8 * W).rearrange("p (c t) -> p c t", t=W)
            assert 24152 <= 24576
            tb = lambda: fresh_bufs(1, tmp_bar)[0]
            aT_b = [[tb() for _ in range(4)] for _ in range(2)]
            acc_b = [tb() for _ in range(4)]
            sig_b = [tb(), tb()]
            sq_b = [tb() for _ in range(4)]
            mean_b, var_b, rst_b = tb(), tb(), tb()
            hb_b = [[tb() for _ in range(4)] for _ in range(2)]
            tA_b, tB_b = tb(), tb()
            pooled_b = [tb() for _ in range(4)]
            ycat_bs = [tb(), sgg_b]
            ycats = [ycat, sgg.bitcast(BF16)[:, :, :].rearrange("p a b -> p (a b)").rearrange("p (c t) -> p c t", t=W)]
            S.add(DVE, lambda e: e.memset(aT[0][:, :, 0:30], 0.0), writes=aT_b[0])
            S.add(DVE, lambda e: e.memset(hbT[0][:, :, 0:15], 0.0), writes=hb_b[0])
            tiles = [(0, HALO, [0])] + [(HALO + W * i, W, [1 + 2 * i, 2 + 2 * i]) for i in range(8)]
            def Y_even(ti):
                c0, w, subs = tiles[ti]
                out_proj(ycats[ti % 2], ycat_bs[ti % 2], wout, wout_b, subs, 0)
                ln_update_multi(subs, final)

            for ti, (c0, w, subs) in enumerate(tiles):
                cur, nxt = ti % 2, (ti + 1) % 2
                xr = [xT_b[s] for s in subs]
                ycat, ycat_b = ycats[ti % 2], ycat_bs[ti % 2]
                for ch in range(4):
                    bk, bkb = ps_alloc()

                    def mm(e, bk=bk, ch=ch, c0=c0, w=w):
                        ins = None
                        for part, oc in ((0, ch), (1, 4 + ch)):
                            for k in range(8):
                                ins = e.matmul(bk[:, part * 256: part * 256 + w], lhsT=win[:, k, oc * 128:(oc + 1) * 128], rhs=xT[:, k, c0:c0 + w],
                                               start=(k == 0), stop=(k == 7))
                        return ins
                    S.add(PE, mm, reads=[win_b] + xr, writes=[bkb])
                    sp = ch % 2
                    S.add(ACT, lambda e, bk=bk, w=w, sp=sp: e.activation(out=sigt[sp][:, 0:w], in_=bk[:, 256:256 + w], func=AF.Sigmoid), reads=[bkb], writes=[sig_b[sp]])
                    S.add(DVE, lambda e, bk=bk, w=w, sp=sp, ch=ch, cur=cur: e.tensor_tensor(out=aT[cur][:, ch, 30:30 + w], in0=bk[:, 0:w], in1=sigt[sp][:, 0:w], op=ALU.mult),
                          reads=[bkb, sig_b[sp]], writes=[aT_b[cur][ch]])
                for gp in range(4):
                    if gp % 2 == 0:
                        bkB, bkBb = ps_alloc()

                    def mm(e, bk=bkB, gp=gp, c0=c0, w=w):
                        ins = None
                        for k in range(8):
                            ins = e.matmul(bk[:, (gp % 2) * 256:(gp % 2) * 256 + w], lhsT=win[:, k, (8 + gp) * 128:(9 + gp) * 128], rhs=xT[:, k, c0:c0 + w],
                                           start=(k == 0), stop=(k == 7))
                        return ins
                    S.add(PE, mm, reads=[win_b] + xr, writes=[bkBb])
                    S.add(ACT, lambda e, bk=bkB, gp=gp, w=w, cur=cur: e.copy(out=hbT[cur][:, gp, 15:15 + w], in_=bk[:, (gp % 2) * 256:(gp % 2) * 256 + w]),
                          reads=[bkBb], writes=[hb_b[cur][gp]])
                for j in range(31):
                    for ch in range(4):
                        if j == 0:
                            S.add(DVE, lambda e, ch=ch, w=w, cur=cur: e.tensor_scalar(
                                out=acc[:, ch, 0:w], in0=aT[cur][:, ch, 0:w], scalar1=cvec[:, CW + ch * 31: CW + ch * 31 + 1], scalar2=cvec[:, CB + ch: CB + ch + 1],
                                op0=ALU.mult, op1=ALU.add), reads=[aT_b[cur][ch], const_b], writes=[acc_b[ch]])
                        else:
                            S.add(DVE, lambda e, ch=ch, w=w, cur=cur, j=j: e.scalar_tensor_tensor(
                                out=acc[:, ch, 0:w], in0=aT[cur][:, ch, j:j + w], scalar=cvec[:, CW + ch * 31 + j: CW + ch * 31 + j + 1], in1=acc[:, ch, 0:w],
                                op0=ALU.mult, op1=ALU.add), reads=[aT_b[cur][ch], acc_b[ch]], writes=[acc_b[ch]])
                if ti + 1 < len(tiles):
                    S.add(ACT, lambda e, w=w, cur=cur, nxt=nxt: e.copy(out=aT[nxt][:, :, 0:30], in_=aT[cur][:, :, w:w + 30]),
                          reads=aT_b[cur], writes=aT_b[nxt])
                for ch in range(4):
                    S.add(ACT, lambda e, ch=ch, w=w: e.activation(out=sqt[:, ch, 0:w], in_=acc[:, ch, 0:w], func=AF.Square), reads=[acc_b[ch]], writes=[sq_b[ch]])
                bk, bkb = ps_alloc()

                def mmst(e, bk=bk, w=w):
                    ins = None
                    for ch in range(4):
                        ins = e.matmul(bk[:, 0:w], lhsT=onesf[:], rhs=acc[:, ch, 0:w], start=(ch == 0), stop=(ch == 3))
                    for ch in range(4):
                        ins = e.matmul(bk[:, 256:256 + w], lhsT=onesf[:], rhs=sqt[:, ch, 0:w], start=(ch == 0), stop=(ch == 3))
                    return ins
                S.add(PE, mmst, reads=acc_b + sq_b + [const_b], writes=[bkb])
                S.add(DVE, lambda e, bk=bk, w=w: e.tensor_scalar(out=meant[:, 0:w], in0=bk[:, 0:w], scalar1=1.0 / 512, scalar2=None, op0=ALU.mult), reads=[bkb], writes=[mean_b])
                S.add(DVE, lambda e, w=w: e.tensor_tensor(out=vart[:, 0:w], in0=meant[:, 0:w], in1=meant[:, 0:w], op=ALU.mult), reads=[mean_b], writes=[var_b])
                S.add(DVE, lambda e, bk=bk, w=w: e.scalar_tensor_tensor(out=vart[:, 0:w], in0=bk[:, 256:256 + w], scalar=1.0 / 512, in1=vart[:, 0:w], op0=ALU.mult, op1=ALU.subtract),
                      reads=[bkb, var_b], writes=[var_b])
                S.add(ACT, lambda e, w=w: e.activation(out=rstt[:, 0:w], in_=vart[:, 0:w], func=AF.Sqrt, bias=epst[:, 0:1], scale=1.0), reads=[var_b, const_b], writes=[rst_b])
                S.add(DVE, lambda e, w=w: e.reciprocal(out=rstt[:, 0:w], in_=rstt[:, 0:w]), reads=[rst_b], writes=[rst_b])
                for ch in range(4):
                    S.add(DVE, lambda e, ch=ch, w=w: e.tensor_tensor(out=acc[:, ch, 0:w], in0=acc[:, ch, 0:w], in1=meant[:, 0:w], op=ALU.subtract),
                          reads=[acc_b[ch], mean_b], writes=[acc_b[ch]])
                for ch in range(4):
                    S.add(DVE, lambda e, ch=ch, w=w: e.tensor_tensor(out=acc[:, ch, 0:w], in0=acc[:, ch, 0:w], in1=rstt[:, 0:w], op=ALU.mult),
                          reads=[acc_b[ch], rst_b], writes=[acc_b[ch]])
                for ch in range(4):
                    S.add(ACT, lambda e, ch=ch, w=w, ycat=ycat: e.activation(out=ycat[:, ch, 0:w], in_=acc[:, ch, 0:w], func=AF.Silu, bias=cvec[:, NB_ + ch: NB_ + ch + 1],
                                                                 scale=cvec[:, NG + ch: NG + ch + 1]), reads=[acc_b[ch], const_b], writes=[ycat_b])
                E = 15 + w
                for gp in range(4):
                    hsrc = hbT[cur]
                    S.add(DVE, lambda e, gp=gp, E=E, hsrc=hsrc: e.tensor_tensor(out=tmpA[:, 1:E], in0=hsrc[:, gp, 1:E], in1=hsrc[:, gp, 0:E - 1], op=ALU.add),
                          reads=[hb_b[cur][gp]], writes=[tA_b])
                    wsrc, wsb = tmpA, tA_b
                    if gp >= 1:
                        S.add(DVE, lambda e, E=E: e.tensor_tensor(out=tmpB[:, 3:E], in0=tmpA[:, 3:E], in1=tmpA[:, 1:E - 2], op=ALU.add), reads=[tA_b], writes=[tB_b])
                        wsrc, wsb = tmpB, tB_b
                    if gp >= 2:
                        S.add(DVE, lambda e, E=E: e.tensor_tensor(out=tmpA[:, 7:E], in0=tmpB[:, 7:E], in1=tmpB[:, 3:E - 4], op=ALU.add), reads=[tB_b], writes=[tA_b])
                        wsrc, wsb = tmpA, tA_b
                    if gp >= 3:
                        S.add(DVE, lambda e, E=E: e.tensor_tensor(out=tmpB[:, 15:E], in0=tmpA[:, 15:E], in1=tmpA[:, 7:E - 8], op=ALU.add), reads=[tA_b], writes=[tB_b])
                        wsrc, wsb = tmpB, tB_b
                    if ti == 1:
                        S.add(DVE, lambda e, gp=gp, wsrc=wsrc: e.tensor_tensor(out=wsrc[:, 15:31], in0=wsrc[:, 15:31], in1=poolcorr[:, gp * 16:(gp + 1) * 16], op=ALU.mult),
                              reads=[wsb, const_b], writes=[wsb])
                    S.add(DVE, lambda e, gp=gp, wsrc=wsrc, E=E, w=w, hsrc=hsrc: e.scalar_tensor_tensor(
                        out=pooled[:, gp, 0:w], in0=wsrc[:, 15:E], scalar=1.0 / (2 ** (gp + 1)), in1=hsrc[:, gp, 15:E], op0=ALU.mult, op1=ALU.subtract),
                        reads=[wsb, hb_b[cur][gp]], writes=[pooled_b[gp]])
                    if gp % 2 == 0:
                        bkP, bkPb = ps_alloc()
                    S.add(PE, lambda e, bk=bkP, gp=gp, w=w: e.matmul(bk[:, (gp % 2) * 256:(gp % 2) * 256 + w], lhsT=poolw[:, gp, :], rhs=pooled[:, gp, 0:w], start=True, stop=True),
                          reads=[pooled_b[gp], const_b], writes=[bkPb])
                    S.add(ACT, lambda e, bk=bkP, gp=gp, w=w, ycat=ycat: e.activation(out=ycat[:, 4 + gp, 0:w], in_=bk[:, (gp % 2) * 256:(gp % 2) * 256 + w], func=AF.Identity,
                                                                          scale=cvec[:, PS + gp: PS + gp + 1]), reads=[bkPb, const_b], writes=[ycat_b])
                if ti + 1 < len(tiles):
                    S.add(ACT, lambda e, w=w, cur=cur, nxt=nxt: e.copy(out=hbT[nxt][:, :, 0:15], in_=hbT[cur][:, :, w:w + 15]),
                          reads=hb_b[cur], writes=hb_b[nxt])
                if not PIPE:
                    Y_even(ti)
                elif ti >= 1:
                    Y_even(ti - 1)
            if PIPE:
                Y_even(len(tiles) - 1)

        def ffn(experts, with_halo, final):
            bar = S.last_ops()
            wg_b = fresh_bufs(2, bar)
            wu_b = fresh_bufs(2, bar)
            wd_b = fresh_bufs(2, bar)
            hT_b = [fresh_bufs(4, bar) for _ in range(2)]
            sg_b = fresh_bufs(2, bar)
            wgv = [mview(i * 24576, BF16, 4096).rearrange("p (k f) -> p k f", f=512) for i in range(2)]
            wuv = [mview(i * 24576 + 8192, BF16, 4096).rearrange("p (k f) -> p k f", f=512) for i in range(2)]
            wdv = [mview(i * 24576 + 16384, BF16, 4096).rearrange("p (c d) -> p c d", d=1024) for i in range(2)]
            hTv = [mview(49152 + i * 4096, BF16, 2048).rearrange("p (c t) -> p c t", t=512) for i in range(2)]
            sgv = [mview(57344 + i * 2048, F32, 512) for i in range(2)]
            tiles = ([(0, HALO, [0])] if with_halo else []) + [(HALO + 512 * i, 512, [1 + 4 * i + q for q in range(4)]) for i in range(4)]
            items = []
            groups = []
            for (wg_ap, wu_ap, wd_ap, F, eidx) in experts:
                nfc = F // 128
                for f0 in range(0, nfc, 4):
                    n = min(4, nfc - f0)
                    gi = len(groups)
                    slot = gi % 2
                    groups.append((wg_ap, wu_ap, wd_ap, f0, n, slot))
                    for ti_, tl in enumerate(tiles):
                        items.append((slot, n, tl, eidx, gi if ti_ == 0 else None))

            def load_group(gi):
                wg_ap, wu_ap, wd_ap, f0, n, slot = groups[gi]
                wload(wgv[slot][:, :, 0:n * 128], wg_ap[:, f0 * 128:(f0 + n) * 128].rearrange("(k p) f -> p k f", p=128), [wg_b[slot]], ("wg", slot))
                wload(wuv[slot][:, :, 0:n * 128], wu_ap[:, f0 * 128:(f0 + n) * 128].rearrange("(k p) f -> p k f", p=128), [wu_b[slot]], ("wu", slot))
                wload(wdv[slot][:, 0:n, :], wd_ap[f0 * 128:(f0 + n) * 128, :].rearrange("(c p) d -> p c d", p=128), [wd_b[slot]], ("wd", slot))

            def step1(it, hp):
                slot, n, (c0, w, subs), eidx, _g = it
                xr = [xT_b[s] for s in subs]
                for fc in range(n):
                    bg, bgb = ps_alloc()
                    bu, bub = ps_alloc()

                    def mm(e, bg=bg, bu=bu, fc=fc, slot=slot, c0=c0, w=w):
                        ins = None
                        for k in range(8):
                            ins = e.matmul(bg[:, 0:w], lhsT=wgv[slot][:, k, fc * 128:(fc + 1) * 128], rhs=xT[:, k, c0:c0 + w], start=(k == 0), stop=(k == 7))
                        for k in range(8):
                            ins = e.matmul(bu[:, 0:w], lhsT=wuv[slot][:, k, fc * 128:(fc + 1) * 128], rhs=xT[:, k, c0:c0 + w], start=(k == 0), stop=(k == 7))
                        return ins
                    S.add(PE, mm, reads=[wg_b[slot], wu_b[slot]] + xr, writes=[bgb, bub])
                    sp = fc % 2
                    S.add(ACT, lambda e, bg=bg, w=w, sp=sp: e.activation(out=sgv[sp][:, 0:w], in_=bg[:, 0:w], func=AF.Silu), reads=[bgb], writes=[sg_b[sp]])
                    S.add(DVE, lambda e, bu=bu, w=w, sp=sp, fc=fc, hp=hp: e.tensor_tensor(out=hTv[hp][:, fc, 0:w], in0=bu[:, 0:w], in1=sgv[sp][:, 0:w], op=ALU.mult),
                          reads=[bub, sg_b[sp]], writes=[hT_b[hp][fc]])

            def step2(it, hp):
                slot, n, (c0, w, subs), eidx, _g = it
                for si, s in enumerate(subs):
                    rows = sub_rows(s)
                    loc = 128 * si
                    for half in range(2):
                        bk, bkb = ps_alloc()

                        def mm(e, bk=bk, rows=rows, loc=loc, half=half, slot=slot, n=n, hp=hp):
                            ins = None
                            for fc in range(n):
                                ins = e.matmul(bk[0:rows, :], lhsT=hTv[hp][:, fc, loc:loc + rows], rhs=wdv[slot][:, fc, half * 512:(half + 1) * 512],
                                               start=(fc == 0), stop=(fc == n - 1))
                            return ins
                        S.add(PE, mm, reads=hT_b[hp][0:n] + [wd_b[slot]], writes=[bkb])
                        rs = resid[0:rows, s, half * 512:(half + 1) * 512]
                        if eidx is None:
                            S.add(DVE, lambda e, bk=bk, rows=rows, rs=rs: e.tensor_tensor(out=rs, in0=rs, in1=bk[0:rows, :], op=ALU.add),
                                  reads=[bkb, resid_b[s]], writes=[resid_b[s]])
                        else:
                            S.add(DVE, lambda e, bk=bk, rows=rows, rs=rs, s=s, eidx=eidx: e.scalar_tensor_tensor(
                                out=rs, in0=bk[0:rows, :], scalar=gates[0:rows, s - 1, eidx:eidx + 1], in1=rs, op0=ALU.mult, op1=ALU.add),
                                reads=[bkb, resid_b[s], gates_b[s - 1]], writes=[resid_b[s]])

            load_group(0)
            for i, it in enumerate(items):
                step1(it, i % 2)
                if i >= 1:
                    step2(items[i - 1], (i - 1) % 2)
                if it[4] is not None and it[4] + 1 < len(groups):
                    load_group(it[4] + 1)
            step2(items[-1], (len(items) - 1) % 2)

        def mixer_odd(final, bar):
            win_b, wout_b = fresh_bufs(2, bar)
            win = mview(0, BF16, 8 * 2560).rearrange("p (k f) -> p k f", f=2560)
            wout = mview(40960, BF16, 8 * 1024).rearrange("p (k f) -> p k f", f=1024)
            for k0 in (0, 4):
                wload(win[:, k0:k0 + 4, :], o_w_in[k0 * 128:(k0 + 4) * 128, :].rearrange("(k p) f -> p k f", p=128), [win_b], ("win", k0))
            for k0 in (0, 4):
                wload(wout[:, k0:k0 + 4, :], o_w_out[k0 * 128:(k0 + 4) * 128, :].rearrange("(k p) f -> p k f", p=128), [wout_b], ("wout", k0))
            load_gb(2, final)
            W = 256
            offB = 57344
            cvT = [mview(offB + i * 4128, F32, 4 * 258).rearrange("p (c t) -> p c t", t=258) for i in range(2)]
            vct = [mview(offB + 8256 + i * 1024, F32, W) for i in range(2)]
            acc = mview(offB + 10304, F32, 4 * W).rearrange("p (c t) -> p c t", t=W)
            uT = mview(offB + 14400, BF16, 4 * W).rearrange("p (c t) -> p c t", t=W)
            vg = mview(offB + 16448, F32, 512)
            vln = [mview(offB + 18496 + i * 1024, BF16, 512) for i in range(2)]
            ycat = mview(offB + 20544, BF16, 8 * W).rearrange("p (c t) -> p c t", t=W)
            assert offB + 24640 <= M_BYTES
            tb = lambda: fresh_bufs(1, bar)[0]
            cv_b = [[tb() for _ in range(4)] for _ in range(2)]
            vct_b = [tb(), tb()]
            acc_b = [tb() for _ in range(4)]
            uT_b = [tb() for _ in range(4)]
            vg_b = tb()
            vln_b = [tb(), tb()]
            ycat_bs = [tb(), resid_b[0]]
            ycats = [ycat, resid.bitcast(BF16)[:, 0, :].rearrange("p (c t) -> p c t", t=W)]
            cload(sgg[:, 0, :], sgrows[0:1, :].partition_broadcast(128), bufs=[sgg_b])
            cload(sgg[:, 1, :], sgrows[1:2, :].partition_broadcast(128), bufs=[sgg_b])
            tiles = [(0, HALO, [0])] + [(HALO + W * i, W, [1 + 2 * i, 2 + 2 * i]) for i in range(8)]
            S.add(DVE, lambda e: e.memset(cvT[0][:, :, 0:2], 0.0), writes=cv_b[0])

            def Y_odd(ti):
                c0, w, subs = tiles[ti]
                out_proj(ycats[ti % 2], ycat_bs[ti % 2], wout, wout_b, subs, 0)
                ln_update_multi(subs, final)

            vcnt = 0
            for ti, (c0, w, subs) in enumerate(tiles):
                cur, nxt = ti % 2, (ti + 1) % 2
                xr = [xT_b[s] for s in subs]
                halo = (ti == 0)
                ycat, ycat_b = ycats[ti % 2], ycat_bs[ti % 2]
                for ch in range(4):
                    bk, bkb = ps_alloc()

                    def mm(e, bk=bk, ch=ch, c0=c0, w=w):
                        ins = None
                        for part, oc in ((0, 4 + ch), (1, 8 + ch)):
                            for k in range(8):
                                ins = e.matmul(bk[:, part * 256: part * 256 + w], lhsT=win[:, k, oc * 128:(oc + 1) * 128], rhs=xT[:, k, c0:c0 + w],
                                               start=(k == 0), stop=(k == 7))
                        return ins
                    S.add(PE, mm, reads=[win_b] + xr, writes=[bkb])
                    sp = ch % 2
                    if halo:
                        S.add(ACT, lambda e, bk=bk, w=w, sp=sp: e.activation(out=vct[sp][:, 0:w], in_=bk[:, 256:256 + w], func=AF.Identity, scale=halomask[:, 0:1]),
                              reads=[bkb, const_b], writes=[vct_b[sp]])
                    else:
                        S.add(ACT, lambda e, bk=bk, w=w, sp=sp: e.copy(out=vct[sp][:, 0:w], in_=bk[:, 256:256 + w]), reads=[bkb], writes=[vct_b[sp]])
                    S.add(DVE, lambda e, bk=bk, w=w, sp=sp, ch=ch, cur=cur: e.tensor_tensor(out=cvT[cur][:, ch, 2:2 + w], in0=bk[:, 0:w], in1=vct[sp][:, 0:w], op=ALU.mult),
                          reads=[bkb, vct_b[sp]], writes=[cv_b[cur][ch]])
                if ti + 1 < len(tiles):
                    S.add(ACT, lambda e, w=w, cur=cur, nxt=nxt: e.copy(out=cvT[nxt][:, :, 0:2], in_=cvT[cur][:, :, w:w + 2]), reads=cv_b[cur], writes=cv_b[nxt])
                if halo:
                    continue
                for ch in range(4):
                    S.add(DVE, lambda e, ch=ch, w=w, cur=cur: e.tensor_scalar(out=acc[:, ch, 0:w], in0=cvT[cur][:, ch, 0:w], scalar1=cvec[:, CC + ch * 3: CC + ch * 3 + 1],
                                                                             scalar2=None, op0=ALU.mult), reads=[cv_b[cur][ch], const_b], writes=[acc_b[ch]])
                    for j in (1, 2):
                        S.add(DVE, lambda e, ch=ch, w=w, cur=cur, j=j: e.scalar_tensor_tensor(
                            out=acc[:, ch, 0:w], in0=cvT[cur][:, ch, j:j + w], scalar=cvec[:, CC + ch * 3 + j: CC + ch * 3 + j + 1], in1=acc[:, ch, 0:w],
                            op0=ALU.mult, op1=ALU.add), reads=[cv_b[cur][ch], acc_b[ch], const_b], writes=[acc_b[ch]])
                for ch in range(4):
                    if ch % 2 == 0:
                        bk, bkb = ps_alloc()

                    def mm(e, bk=bk, ch=ch, c0=c0, w=w):
                        ins = None
                        for k in range(8):
                            ins = e.matmul(bk[:, (ch % 2) * 256:(ch % 2) * 256 + w], lhsT=win[:, k, ch * 128:(ch + 1) * 128], rhs=xT[:, k, c0:c0 + w],
                                           start=(k == 0), stop=(k == 7))
                        return ins
                    S.add(PE, mm, reads=[win_b] + xr, writes=[bkb])
                    S.add(DVE, lambda e, bk=bk, ch=ch, w=w, ycat=ycat: e.tensor_tensor(out=ycat[:, ch, 0:w], in0=bk[:, (ch % 2) * 256:(ch % 2) * 256 + w], in1=acc[:, ch, 0:w], op=ALU.mult),
                          reads=[bkb, acc_b[ch]], writes=[ycat_b])
                for ch in range(4):
                    if ch % 2 == 0:
                        bk, bkb = ps_alloc()

                    def mm(e, bk=bk, ch=ch, c0=c0, w=w):
                        ins = None
                        for k in range(8):
                            ins = e.matmul(bk[:, (ch % 2) * 256:(ch % 2) * 256 + w], lhsT=win[:, k, (12 + ch) * 128:(13 + ch) * 128], rhs=xT[:, k, c0:c0 + w],
                                           start=(k == 0), stop=(k == 7))
                        return ins
                    S.add(PE, mm, reads=[win_b] + xr, writes=[bkb])
                    S.add(ACT, lambda e, bk=bk, ch=ch, w=w: e.activation(out=uT[:, ch, 0:w], in_=bk[:, (ch % 2) * 256:(ch % 2) * 256 + w], func=AF.Gelu_apprx_tanh),
                          reads=[bkb], writes=[uT_b[ch]])
                for si, s in enumerate(subs):
                    cs = sub_col0(s)
                    vp = vcnt % 2
                    vcnt += 1
                    par = cnt["ln"] % 2
                    cnt["ln"] += 1
                    bk, bkb = ps_alloc()

                    def mm(e, bk=bk, cs=cs):
                        ins = None
                        for k in range(8):
                            ins = e.matmul(bk[:, :], lhsT=xT[:, k, cs:cs + 128], rhs=win[:, k, 2048:2560], start=(k == 0), stop=(k == 7))
                        return ins
                    S.add(PE, mm, reads=[win_b, xT_b[s]], writes=[bkb])
                    S.add(ACT, lambda e, bk=bk: e.activation(out=vg[:, :], in_=bk[:, :], func=AF.Gelu_apprx_tanh), reads=[bkb], writes=[vg_b])
                    S.add(DVE, lambda e, par=par: e.bn_stats(out=stats[:, par, 0, :], in_=vg[:, :]), reads=[vg_b], writes=[st_b[par]])
                    S.add(DVE, lambda e, par=par: e.bn_aggr(out=mv[:, par, :], in_=stats[:, par, 0:1, :]), reads=[st_b[par]], writes=[mv_b[par]])
                    S.add(ACT, lambda e, par=par: e.activation(out=sd[:, par, :], in_=mv[:, par, 1:2], func=AF.Sqrt, bias=epst[:, 0:1], scale=1.0),
                          reads=[mv_b[par], const_b], writes=[sd_b[par]])
                    S.add(DVE, lambda e, par=par: e.scalar_tensor_tensor(out=vg[:, :], in0=vg[:, :], scalar=mv[:, par, 0:1], in1=sgg[:, 0, :], op0=ALU.subtract, op1=ALU.mult),
                          reads=[vg_b, mv_b[par], sgg_b], writes=[vg_b])
                    S.add(DVE, lambda e, par=par: e.reciprocal(out=rstd[:, par, :], in_=sd[:, par, :]), reads=[sd_b[par]], writes=[rstd_b[par]])
                    S.add(DVE, lambda e, vp=vp, par=par: e.scalar_tensor_tensor(out=vln[vp][:, :], in0=vg[:, :], scalar=rstd[:, par, :], in1=sgg[:, 1, :], op0=ALU.mult, op1=ALU.add),
                          reads=[vg_b, rstd_b[par], sgg_b], writes=[vln_b[vp]])
                    bk2, bk2b = ps_alloc()

                    def mm2(e, bk2=bk2, vp=vp):
                        ins = None
                        for h in range(4):
                            e.matmul(bk2[:, h * 128:(h + 1) * 128], lhsT=vln[vp][:, h * 128:(h + 1) * 128], rhs=wmT[:, h, :], start=True, stop=False)
                            e.matmul(bk2[:, h * 128:(h + 1) * 128], lhsT=onesb[0:1, :], rhs=bshl[0:1, 0, h * 128:(h + 1) * 128], start=False, stop=False)
                            ins = e.matmul(bk2[:, h * 128:(h + 1) * 128], lhsT=onesb[0:1, :], rhs=bshl[0:1, 1, h * 128:(h + 1) * 128], start=False, stop=True)
                        return ins
                    S.add(PE, mm2, reads=[vln_b[vp], wmT_b, const_b], writes=[bk2b])
                    loc = 128 * si
                    S.add(DVE, lambda e, bk2=bk2, loc=loc, ycat=ycat: e.tensor_tensor(out=ycat[:, 4:8, loc:loc + 128], in0=bk2[:, :].rearrange("p (h i) -> p h i", i=128),
                                                                           in1=uT[:, :, loc:loc + 128], op=ALU.mult), reads=[bk2b] + uT_b, writes=[ycat_b])
                if not PIPE:
                    Y_odd(ti)
                elif ti >= 2:
                    Y_odd(ti - 1)
            if PIPE:
                Y_odd(len(tiles) - 1)

        def gating_all():
            lgall = gbt[:, 0:128].rearrange("p (s e) -> p s e", e=NEXP)
            lg2 = gbt[:, 128:256].rearrange("p (s e) -> p s e", e=NEXP)
            exa = gbt[:, 256:384].rearrange("p (s e) -> p s e", e=NEXP)
            mx1 = gbt[:, 384:400].rearrange("p (s o) -> p s o", o=1)
            mx2 = gbt[:, 400:416].rearrange("p (s o) -> p s o", o=1)
            ssm = gbt[:, 416:432].rearrange("p (s o) -> p s o", o=1)
            G = gb_b[0]
            bk, bkb = ps_alloc()

            def mm(e):
                ins = None
                for s in range(1, 17):
                    cs = sub_col0(s)
                    for k in range(8):
                        ins = e.matmul(bk[:, (s - 1) * 8:s * 8], lhsT=xT[:, k, cs:cs + 128], rhs=rtr[:, k, :], start=(k == 0), stop=(k == 7))
                return ins
            S.add(PE, mm, reads=xT_b[1:] + [const_b], writes=[bkb])
            bc = lambda t: t.to_broadcast([128, 16, NEXP])
            S.add(DVE, lambda e: e.tensor_copy(out=lgall.rearrange("p s e -> p (s e)"), in_=bk[:, 0:128]), reads=[bkb, G], writes=[G])
            S.add(DVE, lambda e: e.tensor_reduce(out=mx1, in_=lgall, axis=AX.X, op=ALU.max), reads=[G], writes=[G])
            S.add(DVE, lambda e: e.tensor_tensor(out=m0all[:, :, :], in0=lgall, in1=bc(mx1), op=ALU.is_equal), reads=[G], writes=[route_b])
            S.add(DVE, lambda e: e.scalar_tensor_tensor(out=lg2, in0=m0all[:, :, :], scalar=-1e30, in1=lgall, op0=ALU.mult, op1=ALU.add), reads=[G, route_b], writes=[G])
            S.add(DVE, lambda e: e.tensor_reduce(out=mx2, in_=lg2, axis=AX.X, op=ALU.max), reads=[G], writes=[G])
            S.add(DVE, lambda e: e.tensor_tensor(out=maskall[:, :, :], in0=lgall, in1=bc(mx2), op=ALU.is_ge), reads=[G, route_b], writes=[route_b])
            S.add(DVE, lambda e: e.tensor_tensor(out=exa, in0=lgall, in1=bc(mx1), op=ALU.subtract), reads=[G], writes=[G])
            S.add(ACT, lambda e: e.activation(out=exa, in_=exa, func=AF.Exp), reads=[G], writes=[G])
            S.add(DVE, lambda e: e.tensor_tensor(out=exa, in0=exa, in1=maskall[:, :, :], op=ALU.mult), reads=[G, route_b], writes=[G])
            S.add(DVE, lambda e: e.tensor_reduce(out=ssm, in_=exa, axis=AX.X, op=ALU.add), reads=[G], writes=[G])
            S.add(DVE, lambda e: e.reciprocal(out=ssm, in_=ssm), reads=[G], writes=[G])
            S.add(DVE, lambda e: e.tensor_tensor(out=gates[:, :, :], in0=exa, in1=bc(ssm), op=ALU.mult), reads=[G], writes=gates_b)
            S.add(DVE, lambda e: e.tensor_tensor(out=lg2, in0=gates[:, :, :], in1=m0all[:, :, :], op=ALU.mult), reads=gates_b + [route_b, G], writes=[G])
            S.add(DVE, lambda e: e.tensor_reduce(out=gsel[:, :, 0:1], in_=lg2, axis=AX.X, op=ALU.add), reads=[G], writes=[route_b])
            S.add(DVE, lambda e: e.tensor_tensor(out=lg2, in0=maskall[:, :, :], in1=m0all[:, :, :], op=ALU.subtract), reads=[G, route_b], writes=[G])
            S.add(DVE, lambda e: e.tensor_tensor(out=lg2, in0=lg2, in1=gates[:, :, :], op=ALU.mult), reads=gates_b + [G], writes=[G])
            S.add(DVE, lambda e: e.tensor_reduce(out=gsel[:, :, 1:2], in_=lg2, axis=AX.X, op=ALU.add), reads=[G], writes=[route_b])

        def routing_finalize():
            cnts = gbt[:, 0:128].rearrange("p (s e) -> p s e", e=NEXP)
            offs = gbt[:, 128:256].rearrange("p (s e) -> p s e", e=NEXP)
            posc = gbt[:, 256:384]
            eoff = gbt[:, 384:512]
            tmpm = gbt[:, 512:640]
            dstf = gbt[:, 640:672].rearrange("p (r s) -> p r s", s=16)
            nmx = gbt[:, 672:673]
            S.add(SP, lambda e: e.dma_start(out=eoff, in_=eoff_d), writes=[gb_b[0]], dma_key=("c", "eoff"))
            S.add(DVE, lambda e: e.tensor_copy(out=maskb[:], in_=maskall[:, :, :].rearrange("p s e -> p (s e)")), reads=[route_b], writes=[route_b])
            bk, bkb = ps_alloc()

            def mm(e):
                e.matmul(bk[:, 0:128], lhsT=ustb[:], rhs=maskb[:], start=True, stop=True)
                return e.matmul(bk[:, 128:256], lhsT=ones128b[:], rhs=maskb[:], start=True, stop=True)
            S.add(PE, mm, reads=[route_b, const_b], writes=[bkb])
            S.add(DVE, lambda e: e.tensor_copy(out=cnts.rearrange("p s e -> p (s e)"), in_=bk[:, 128:256]), reads=[bkb, gb_b[0]], writes=[gb_b[0]])
            S.add(DVE, lambda e: e.memset(offs[:, 0, :], 0.0), reads=[gb_b[0]], writes=[gb_b[0]])
            for q in range(1, 16):
                S.add(DVE, lambda e, q=q: e.tensor_tensor(out=offs[:, q, :], in0=offs[:, q - 1, :], in1=cnts[:, q - 1, :], op=ALU.add), reads=[gb_b[0]], writes=[gb_b[0]])
            S.add(DVE, lambda e: e.tensor_tensor(out=posc, in0=bk[:, 0:128], in1=offs.rearrange("p s e -> p (s e)"), op=ALU.add), reads=[bkb, gb_b[0]], writes=[gb_b[0]])
            S.add(DVE, lambda e: e.tensor_tensor(out=posc, in0=posc, in1=eoff, op=ALU.add), reads=[gb_b[0]], writes=[gb_b[0]])
            S.add(DVE, lambda e: e.tensor_tensor(out=tmpm, in0=posc, in1=m0all[:, :, :].rearrange("p s e -> p (s e)"), op=ALU.mult), reads=[gb_b[0], route_b], writes=[gb_b[0]])
            S.add(DVE, lambda e: e.tensor_reduce(out=dstf[:, 0, :], in_=tmpm.rearrange("p (s e) -> p s e", e=NEXP), axis=AX.X, op=ALU.add), reads=[gb_b[0]], writes=[gb_b[0]])
            S.add(DVE, lambda e: e.tensor_tensor(out=tmpm, in0=maskall[:, :, :].rearrange("p s e -> p (s e)"), in1=m0all[:, :, :].rearrange("p s e -> p (s e)"), op=ALU.subtract),
                  reads=[gb_b[0], route_b], writes=[gb_b[0]])
            S.add(DVE, lambda e: e.tensor_tensor(out=tmpm, in0=tmpm, in1=posc, op=ALU.mult), reads=[gb_b[0]], writes=[gb_b[0]])
            S.add(DVE, lambda e: e.tensor_reduce(out=dstf[:, 1, :], in_=tmpm.rearrange("p (s e) -> p s e", e=NEXP), axis=AX.X, op=ALU.add), reads=[gb_b[0]], writes=[gb_b[0]])
            S.add(DVE, lambda e: e.tensor_copy(out=desti[:, :, :], in_=dstf), reads=[gb_b[0]], writes=[route_b])
            S.add(DVE, lambda e: e.tensor_tensor(out=tmpm[:, 0:8], in0=offs[:, 15, :], in1=cnts[:, 15, :], op=ALU.add), reads=[gb_b[0]], writes=[gb_b[0]])
            S.add(DVE, lambda e: e.reduce_max(out=nmx, in_=tmpm[:, 0:8], axis=AX.X), reads=[gb_b[0]], writes=[gb_b[0]])
            thr = -1.0 if force_dense else float(CAP)
            S.add(DVE, lambda e: e.tensor_scalar(out=nmx, in0=nmx, scalar1=thr, scalar2=None, op0=ALU.is_gt), reads=[gb_b[0]], writes=[gb_b[0]])
            return S.add(DVE, lambda e: e.tensor_copy(out=flagi[:, :], in_=nmx), reads=[gb_b[0]], writes=[route_b])

        def moe_sparse():
            bar = S.last_ops()
            wg_b = fresh_bufs(2, bar)
            wu_b = fresh_bufs(2, bar)
            wd_b = fresh_bufs(2, bar)
            hT_b = [fresh_bufs(4, bar) for _ in range(2)]
            sg_b = fresh_bufs(2, bar)
            yacc_b = fresh_bufs(NSL, bar)
            xcT_b = fresh_bufs(2, bar)
            xcs_b = fresh_bufs(1, bar)[0]
            wgv = [mview(i * 24576, BF16, 4096).rearrange("p (k f) -> p k f", f=512) for i in range(2)]
            wuv = [mview(i * 24576 + 8192, BF16, 4096).rearrange("p (k f) -> p k f", f=512) for i in range(2)]
            wdv = [mview(i * 24576 + 16384, BF16, 4096).rearrange("p (c d) -> p c d", d=1024) for i in range(2)]
            hTv = [mview(49152 + i * 5120, BF16, 4 * CAP).rearrange("p (c t) -> p c t", t=CAP) for i in range(2)]
            yacc = mview(59392, F32, NSL * D).rearrange("p (j d) -> p j d", d=D)
            assert 79872 <= M_BYTES
            sgv = [sgg[:, i, :] for i in range(2)]
            xTf = xT[:, :, :].rearrange("p k t -> p (k t)")
            xcT = [xTf[:, i * 8 * CAP:(i + 1) * 8 * CAP].rearrange("p (k t) -> p k t", t=CAP) for i in range(2)]
            xcs = xTf[:, 16 * CAP:16 * CAP + NSL * D].rearrange("p (j d) -> p j d", d=D)
            assert 16 * CAP + NSL * D <= 8 * NT
            sc_bufs = []
            for s in range(1, 17):
                par = cnt["ln"] % 2
                cnt["ln"] += 1
                S.add(ACT, lambda e, s=s, par=par: e.activation(out=xb[:, par, :], in_=resid[:, s, :], func=AF.Identity, scale=1.0 / ALPHA),
                      reads=[resid_b[s]], writes=[xb_b[par]])
                for r in range(2):
                    b = fresh_bufs(1, bar)[0]
                    sc_bufs.append(b)
                    S.add(POOL, lambda e, s=s, par=par, r=r: e.indirect_dma_start(
                        out=xc_d, out_offset=bass.IndirectOffsetOnAxis(ap=desti[:, r, s - 1:s], axis=0), in_=xb[:, par, :], in_offset=None),
                        reads=[xb_b[par], route_b], writes=[b], dma_key=("xsc", par, r))
            ftiles = [(0, 512), (512, CAP - 512)]
            groups = []
            items = []
            for ex in range(NEXP):
                for f0 in range(0, FF_EXP // 128, 4):
                    gi = len(groups)
                    groups.append((ex, f0, gi % 2))
                    for ti_, tl in enumerate(ftiles):
                        items.append((gi % 2, tl, ex, f0, gi if ti_ == 0 else None, ti_ == len(ftiles) - 1))

            def load_group(gi):
                ex, f0, slot = groups[gi]
                wload(wgv[slot][:, :, :], o_wg[ex][:, f0 * 128:(f0 + 4) * 128].rearrange("(k p) f -> p k f", p=128), [wg_b[slot]], ("wg", slot))
                wload(wuv[slot][:, :, :], o_wu[ex][:, f0 * 128:(f0 + 4) * 128].rearrange("(k p) f -> p k f", p=128), [wu_b[slot]], ("wu", slot))
                wload(wdv[slot][:, :, :], o_wd[ex][f0 * 128:(f0 + 4) * 128, :].rearrange("(c p) d -> p c d", p=128), [wd_b[slot]], ("wd", slot))

            def load_tokens_dma(ex):
                S.add(SP, lambda e, ex=ex: e.dma_start(out=xcs, in_=xc_d[ex * CAP:(ex + 1) * CAP, :].rearrange("(j p) d -> p j d", p=128)),
                      reads=sc_bufs, writes=[xcs_b], dma_key="xcs")

            def load_tokens_tr(ex):
                xp = ex % 2
                for j in range(NSL):
                    def tr(e, j=j):
                        ins = None
                        for c in range(8):
                            ins = e.transpose(out=pT[:, c, :], in_=xcs[:, j, c * 128:(c + 1) * 128], identity=identb[:])
                        return ins
                    S.add(PE, tr, reads=[xcs_b, const_b], writes=[pT_b])
                    S.add(ACT, lambda e, j=j, xp=xp: e.copy(out=xcT[xp][:, :, j * 128:(j + 1) * 128], in_=pT[:, :, :]), reads=[pT_b], writes=[xcT_b[xp]])

            def step1(it, hp):
                slot, (c0, w), ex, f0, _g, _l = it
                xp = ex % 2
                for fc in range(4):
                    bg, bgb = ps_alloc()
                    bu, bub = ps_alloc()

                    def mm(e, bg=bg, bu=bu, fc=fc, slot=slot, c0=c0, w=w, xp=xp):
                        ins = None
                        for k in range(8):
                            ins = e.matmul(bg[:, 0:w], lhsT=wgv[slot][:, k, fc * 128:(fc + 1) * 128], rhs=xcT[xp][:, k, c0:c0 + w], start=(k == 0), stop=(k == 7))
                        for k in range(8):
                            ins = e.matmul(bu[:, 0:w], lhsT=wuv[slot][:, k, fc * 128:(fc + 1) * 128], rhs=xcT[xp][:, k, c0:c0 + w], start=(k == 0), stop=(k == 7))
                        return ins
                    S.add(PE, mm, reads=[wg_b[slot], wu_b[slot], xcT_b[xp]], writes=[bgb, bub])
                    sp = fc % 2
                    S.add(ACT, lambda e, bg=bg, w=w, sp=sp: e.activation(out=sgv[sp][:, 0:w], in_=bg[:, 0:w], func=AF.Silu), reads=[bgb], writes=[sg_b[sp]])
                    S.add(DVE, lambda e, bu=bu, w=w, sp=sp, fc=fc, hp=hp, c0=c0: e.tensor_tensor(out=hTv[hp][:, fc, c0:c0 + w], in0=bu[:, 0:w], in1=sgv[sp][:, 0:w], op=ALU.mult),
                          reads=[bub, sg_b[sp]], writes=[hT_b[hp][fc]])

            def step2(gi, hp):
                ex, f0, slot = groups[gi]
                first = (f0 == 0)
                last = (f0 + 4 == FF_EXP // 128)
                for j in range(NSL):
                    for half in range(2):
                        bk, bkb = ps_alloc()

                        def mm(e, bk=bk, j=j, half=half, slot=slot, hp=hp):
                            ins = None
                            for fc in range(4):
                                ins = e.matmul(bk[:, :], lhsT=hTv[hp][:, fc, j * 128:(j + 1) * 128], rhs=wdv[slot][:, fc, half * 512:(half + 1) * 512],
                                               start=(fc == 0), stop=(fc == 3))
                            return ins
                        S.add(PE, mm, reads=hT_b[hp] + [wd_b[slot]], writes=[bkb])
                        ya = yacc[:, j, half * 512:(half + 1) * 512]
                        if first:
                            S.add(ACT, lambda e, bk=bk, ya=ya: e.copy(out=ya, in_=bk[:, :]), reads=[bkb], writes=[yacc_b[j]])
                        else:
                            S.add(DVE, lambda e, bk=bk, ya=ya: e.tensor_tensor(out=ya, in0=ya, in1=bk[:, :], op=ALU.add), reads=[bkb, yacc_b[j]], writes=[yacc_b[j]])
                if last:
                    b = fresh_bufs(1, [])[0]
                    yst_bufs.append(b)
                    S.add(SP, lambda e, ex=ex: e.dma_start(out=yc_d[ex * CAP:(ex + 1) * CAP, :].rearrange("(j p) d -> p j d", p=128), in_=yacc[:, :, :]),
                          reads=yacc_b, writes=[b], dma_key="yst")

            yst_bufs = []
            load_group(0)
            load_tokens_dma(0)
            load_tokens_tr(0)
            pend = None
            for i, it in enumerate(items):
                slot, tl, ex, f0, gfirst, glast = it
                gi = ex * (FF_EXP // 512) + f0 // 4
                step1(it, gi % 2)
                if gfirst is not None:
                    if pend is not None:
                        step2(pend, pend % 2)
                        pend = None
                    if gi + 1 < len(groups):
                        load_group(gi + 1)
                    if f0 == 0 and ex + 1 < NEXP:
                        load_tokens_dma(ex + 1)
                    if f0 == 12 and ex + 1 < NEXP:
                        load_tokens_tr(ex + 1)
                if glast:
                    pend = gi
            step2(pend, pend % 2)
            for s in range(1, 17):
                for r in range(2):
                    j = (2 * s + r) % NSL
                    S.add(POOL, lambda e, s=s, r=r, j=j: e.indirect_dma_start(
                        out=yacc[:, j, :], out_offset=None, in_=yc_d, in_offset=bass.IndirectOffsetOnAxis(ap=desti[:, r, s - 1:s], axis=0)),
                        reads=yst_bufs + [route_b], writes=[yacc_b[j]], dma_key=("ygt", j))
                    S.add(DVE, lambda e, s=s, r=r, j=j: e.scalar_tensor_tensor(out=resid[:, s, :], in0=yacc[:, j, :], scalar=gsel[:, s - 1, r:r + 1], in1=resid[:, s, :],
                                                                               op0=ALU.mult, op1=ALU.add), reads=[yacc_b[j], resid_b[s], route_b], writes=[resid_b[s]])

        mixer_even(final=(n_sub == 1))
        if n_sub >= 2:
            load_gb(1, n_sub == 2)
            ffn([(e_wg, e_wu, e_wd, FF_DENSE, None)], with_halo=True, final=(n_sub == 2))
            bar_ffn0 = S.last_ops()
            ln_update_multi([0], n_sub == 2)
            for s in range(1, 17, 2):
                ln_update_multi([s, s + 1], n_sub == 2)
        if n_sub >= 3:
            mixer_odd(final=(n_sub == 3), bar=bar_ffn0)
        if n_sub >= 4:
            gating_all()
            flag_op = routing_finalize()
            load_gb(3, True)
            if moe_mode in ("both", "dense"):
                if moe_mode == "both":
                    S.begin_cond(flagi[0:1, 0:1], flag_op, True)
                ffn([(o_wg[e], o_wu[e], o_wd[e], FF_EXP, e) for e in range(NEXP)], with_halo=False, final=True)
                S.end_cond()
            if moe_mode in ("both", "sparse"):
                if moe_mode == "both":
                    S.begin_cond(flagi[0:1, 0:1], flag_op, False)
                moe_sparse()
                S.end_cond()
            for s in range(1, 17, 2):
                ln_update_multi([s, s + 1], True)
        S.emit(final_wait_ops=out_ops)
    return nc


def _make_in_maps(inp):
    f = lambda a: np.ascontiguousarray(np.asarray(a, dtype=np.float32))
    x = f(inp["x"])

    def pc(v):
        v = f(v)
        return v.reshape(-1, 128).T

    conv_a_w = f(inp["even_conv_a_w"])[0]
    cw = conv_a_w.T.reshape(4, 128, 31).transpose(1, 0, 2).reshape(128, 124)
    conv_c_w = f(inp["odd_conv_c_w"])[0]
    cc = conv_c_w.T.reshape(4, 128, 3).transpose(1, 0, 2).reshape(128, 12)
    cvec = np.concatenate([cw, pc(inp["even_conv_a_b"][0]), pc(inp["even_norm_a_g"][0]), pc(inp["even_norm_a_b"][0]),
                           pc(inp["even_pool_scale"][0]), cc], axis=1)
    cvec = np.ascontiguousarray(cvec, dtype=np.float32)
    assert cvec.shape == (128, 152)
    lnrows = np.stack([f(inp[k])[0] for k in ("even_ln1_g", "even_ln1_b", "even_ln2_g", "even_ln2_b", "odd_ln1_g", "odd_ln1_b", "odd_ln2_g", "odd_ln2_b")])
    sgrows = np.stack([f(inp["odd_sgu_norm_g"])[0], f(inp["odd_sgu_norm_b"])[0], f(inp["odd_sgu_b"])[0].reshape(512)])
    ident = np.eye(128, dtype=np.float32)
    tril = np.triu(np.ones((128, 128), dtype=np.float32))
    ustrict = np.triu(np.ones((128, 128), dtype=np.float32), k=1)
    eoff = np.ascontiguousarray(np.broadcast_to(np.tile(np.arange(NEXP, dtype=np.float32) * CAP, 16).reshape(1, 128), (128, 128)))
    common = {
        "e_w_in": f(inp["even_w_in"])[0], "e_w_out": f(inp["even_w_out"])[0],
        "e_wg": f(inp["even_ffn_w_gate"])[0], "e_wu": f(inp["even_ffn_w_up"])[0], "e_wd": f(inp["even_ffn_w_down"])[0],
        "o_w_in": f(inp["odd_w_in"])[0], "o_w_out": f(inp["odd_w_out"])[0], "o_router": f(inp["odd_router"])[0],
        "o_wg": f(inp["odd_moe_w_gate"])[0], "o_wu": f(inp["odd_moe_w_up"])[0], "o_wd": f(inp["odd_moe_w_down"])[0],
        "lnrows": np.ascontiguousarray(lnrows), "sgrows": np.ascontiguousarray(sgrows), "cvec": cvec,
        "pool_w": f(inp["even_pool_w"])[0], "sgu_w": f(inp["odd_sgu_w"])[0], "ident": ident, "tril": tril, "ustrict": ustrict, "eoff": eoff,
    }
    corr0 = np.ones((4, 16), dtype=np.float32)
    for g in range(4):
        win = 2 ** (g + 1)
        for t in range(16):
            corr0[g, t] = win / min(t + 1, win)
    maps = []
    for c in range(8):
        b, half = c // 2, c % 2
        xc = np.zeros((NT, D), dtype=np.float32)
        if half == 0:
            xc[HALO:] = x[b, 0:T]
            corr = corr0
            hm = 0.0
        else:
            xc[:] = x[b, T - HALO:2 * T]
            corr = np.ones((4, 16), dtype=np.float32)
            hm = 1.0
        m = dict(common)
        m["x_c"] = xc
        m["poolcorr"] = np.ascontiguousarray(np.broadcast_to(corr.reshape(1, 64), (128, 64)), dtype=np.float32)
        m["halomask"] = np.full((128, 1), hm, dtype=np.float32)
        maps.append(m)
    return maps


_NC_CACHE = {}


N_SUB = 4


def kernel(**inputs):
    maps = _make_in_maps(inputs)
    if "nc" not in _NC_CACHE:
        _NC_CACHE["nc"] = build_nc(N_SUB)
    res = run_bass_kernel_spmd(_NC_CACHE["nc"], maps, core_ids=list(range(8)))
    out = np.empty((4, 2 * T, D), dtype=np.float32)
    for c in range(8):
        b, half = c // 2, c % 2
        out[b, half * T:(half + 1) * T] = res.results[c]["out"]
    return out
```

```python
import contextlib
import numpy as np
import concourse.bass as bass
import concourse.mybir as mybir
from concourse.bass_utils import run_bass_kernel_spmd

F32 = mybir.dt.float32
BF16 = mybir.dt.bfloat16
AF = mybir.ActivationFunctionType
ALU = mybir.AluOpType
AX = mybir.AxisListType

PE, ACT, DVE, POOL, SP = "tensor", "scalar", "vector", "gpsimd", "sync"
ENGS = [PE, ACT, DVE, POOL, SP]

HALO = 32
T = 2048
NT = HALO + T
D = 1024
ALPHA = 4.0 ** 0.25
EPS = 1e-5
FF_DENSE = 2816
FF_EXP = 3584
NEXP = 8
CAP = 640
NSL = CAP // 128
I32 = mybir.dt.int32
MOE_MODE = "both"
FORCE_DENSE = False
PIPE = True


class Buf:
    __slots__ = ("name", "writer", "readers")

    def __init__(self, name=""):
        self.name = name
        self.writer = None
        self.readers = []


class Op:
    __slots__ = ("eng", "fn", "deps", "dma", "sem", "val", "signal", "cond")

    def __init__(self, eng, fn, dma):
        self.eng = eng
        self.fn = fn
        self.deps = []
        self.dma = dma
        self.sem = None
        self.val = 0
        self.signal = False
        self.cond = None


class Sched:
    def __init__(self, nc):
        self.nc = nc
        self.ops = {e: [] for e in ENGS}
        self.all_ops = []
        self.cur_cond = None
        self.conds = []

    def begin_cond(self, flag_ap, flag_op, sense):
        flag_op.signal = True
        self.conds.append((flag_ap, flag_op, sense))
        self.cur_cond = len(self.conds) - 1

    def end_cond(self):
        self.cur_cond = None

    def add(self, eng, fn, reads=(), writes=(), dma_key=None):
        op = Op(eng, fn, dma_key)
        op.cond = self.cur_cond
        deps = []
        for r in reads:
            if r.writer is not None:
                deps.append(r.writer)
        for w in writes:
            if w.writer is not None:
                deps.append(w.writer)
            deps.extend(w.readers)
        seen = set()
        for d in deps:
            if d is op or id(d) in seen:
                continue
            seen.add(id(d))
            if d.eng == PE and eng == PE and d.dma is None and dma_key is None:
                continue
            op.deps.append(d)
            d.signal = True
        for r in reads:
            r.readers.append(op)
        for w in writes:
            w.writer = op
            w.readers = []
        self.ops[eng].append(op)
        self.all_ops.append(op)
        return op

    def last_ops(self):
        return [self.ops[e][-1] for e in ENGS if self.ops[e]]

    def emit(self, final_wait_ops=()):
        nc = self.nc
        eng_cnt = {e: 0 for e in ENGS}
        dma_keys = {}
        for op in self.all_ops:
            if op.dma is not None:
                ent = dma_keys.setdefault(op.dma, [len(dma_keys), 0])
                ent[1] += 16
                op.sem = ("dma", ent[0])
                op.val = ent[1]
                op.signal = True
            elif op.signal:
                eng_cnt[op.eng] += 1
                op.sem = ("eng", op.eng)
                op.val = eng_cnt[op.eng]
        with contextlib.ExitStack() as st:
            sems = {}
            for e in ENGS:
                sems[("eng", e)] = st.enter_context(nc.semaphore("s_" + e))
            for i in range(len(dma_keys)):
                sems[("dma", i)] = st.enter_context(nc.semaphore("d_%d" % i))
            block = st.enter_context(nc.Block())
            for e in ENGS:
                ops = self.ops[e]

                def body(engobj, ops=ops, e=e):
                    waited = {}

                    def run(op):
                        for d in op.deps:
                            if waited.get(d.sem, 0) >= d.val:
                                continue
                            waited[d.sem] = d.val
                            engobj.wait_ge(sems[d.sem], d.val)
                        ins = op.fn(engobj)
                        if op.signal:
                            ins.then_inc(sems[op.sem], 16 if op.dma is not None else 1)

                    i = 0
                    own_val = 0
                    dma_val = {}
                    while i < len(ops):
                        op = ops[i]
                        if op.cond is None:
                            run(op)
                            if op.dma is not None:
                                dma_val[op.sem] = op.val
                            elif op.signal:
                                own_val = op.val
                            i += 1
                            continue
                        j = i
                        while j < len(ops) and ops[j].cond == op.cond:
                            j += 1
                        blk = ops[i:j]
                        flag_ap, flag_op, sense = self.conds[op.cond]
                        if waited.get(flag_op.sem, 0) < flag_op.val:
                            waited[flag_op.sem] = flag_op.val
                            engobj.wait_ge(sems[flag_op.sem], flag_op.val)
                        creg = engobj.alloc_register("cflag%d_%d" % (op.cond, i))
                        engobj.reg_load(creg, flag_ap)
                        snap_waited = dict(waited)
                        base_own = own_val
                        base_dma = dict(dma_val)
                        n_own = 0
                        n_dma = {}
                        with (engobj.If(creg) if sense else engobj.If_eq(creg, 0)):
                            for bop in blk:
                                run(bop)
                                if bop.dma is not None:
                                    n_dma[bop.sem] = n_dma.get(bop.sem, 0) + 16
                                    dma_val[bop.sem] = bop.val
                                elif bop.signal:
                                    n_own += 1
                                    own_val = bop.val
                        with engobj.Else():
                            if n_own:
                                engobj.wait_ge(sems[("eng", e)], base_own)
                                engobj.sem_inc(sems[("eng", e)], n_own)
                            for k, n in n_dma.items():
                                engobj.wait_ge(sems[k], base_dma.get(k, 0))
                                engobj.sem_inc(sems[k], n)
                        waited.clear()
                        waited.update(snap_waited)
                        i = j
                    if e == SP:
                        for op in final_wait_ops:
                            if waited.get(op.sem, 0) >= op.val:
                                continue
                            waited[op.sem] = op.val
                            engobj.wait_ge(sems[op.sem], op.val)

                getattr(block, e)(body)


def sub_rows(s):
    return HALO if s == 0 else 128


def sub_col0(s):
    return 0 if s == 0 else HALO + 128 * (s - 1)


def build_nc(n_sub=4, moe_mode=None, force_dense=None):
    nc = bass.Bass("TRN2", target_bir_lowering=False)
    moe_mode = MOE_MODE if moe_mode is None else moe_mode
    force_dense = FORCE_DENSE if force_dense is None else force_dense

    def din(name, shape):
        return nc.dram_tensor(name, list(shape), F32, kind="ExternalInput").ap()

    x_c = din("x_c", [NT, D])
    e_w_in = din("e_w_in", [D, 1536])
    e_w_out = din("e_w_out", [D, D])
    e_wg = din("e_wg", [D, FF_DENSE])
    e_wu = din("e_wu", [D, FF_DENSE])
    e_wd = din("e_wd", [FF_DENSE, D])
    o_w_in = din("o_w_in", [D, 2560])
    o_w_out = din("o_w_out", [D, D])
    o_router = din("o_router", [D, NEXP])
    o_wg = din("o_wg", [NEXP, D, FF_EXP])
    o_wu = din("o_wu", [NEXP, D, FF_EXP])
    o_wd = din("o_wd", [NEXP, FF_EXP, D])
    lnrows = din("lnrows", [8, D])
    sgrows = din("sgrows", [3, 512])
    cvec_d = din("cvec", [128, 152])
    pool_w_d = din("pool_w", [4, 128, 128])
    sgu_w_d = din("sgu_w", [4, 128, 128])
    ident_d = din("ident", [128, 128])
    tril_d = din("tril", [128, 128])
    poolcorr_d = din("poolcorr", [128, 64])
    halomask_d = din("halomask", [128, 1])
    ustrict_d = din("ustrict", [128, 128])
    eoff_d = din("eoff", [128, 128])
    out_d = nc.dram_tensor("out", [T, D], F32, kind="ExternalOutput").ap()
    xc_d = nc.dram_tensor("xc_scr", [NEXP * CAP, D], BF16, kind="Internal").ap()
    yc_d = nc.dram_tensor("yc_scr", [NEXP * CAP, D], F32, kind="Internal").ap()

    st = contextlib.ExitStack()
    with st:
        S = Sched(nc)

        def sb(name, shape, dt):
            return st.enter_context(nc.sbuf_tensor(name, list(shape), dt))

        resid = sb("resid", [128, 17, D], F32)
        xT = sb("xT", [128, 8, NT], BF16)
        gbt = sb("gb", [128, 2 * D], F32)
        gb = gbt[:, :].rearrange("p (i d) -> p i d", d=D)
        xb = sb("xb", [128, 2, D], BF16)
        identf = sb("identf", [128, 128], F32)
        identb = sb("identb", [128, 128], BF16)
        onesf = sb("onesf", [128, 128], F32)
        onesb = sb("onesb", [1, 128], BF16)
        epst = sb("epst", [128, 1], F32)
        cvec = sb("cvec_s", [128, 152], F32)
        poolw = sb("poolw", [128, 4, 128], BF16)
        sguwf = gbt[:, D:D + 512].rearrange("p (h j) -> p h j", j=128)
        trilf = gbt[:, D + 512:D + 640]
        wmT = sb("wmT", [128, 4, 128], BF16)
        sgg = sb("sgg", [128, 2, 512], F32)
        bsrow = gbt[0:1, 0:1024].rearrange("p (a b) -> p a b", b=512)
        bshl = sb("bshl", [1, 2, 512], BF16)
        poolcorr = sb("poolcorr_s", [128, 64], F32)
        halomask = sb("halomask_s", [128, 1], F32)
        rtr = sb("rtr", [128, 8, NEXP], BF16)
        gates = sb("gates", [128, 16, NEXP], F32)
        stats = sb("stats", [128, 2, 2, 6], F32)
        mv = sb("mv", [128, 2, 2], F32)
        sd = sb("sd", [128, 2, 1], F32)
        rstd = sb("rstd", [128, 2, 1], F32)
        nb = sb("nb", [128, 2, 1], F32)
        xstats = sb("xstats", [128, 2, 6], F32)
        xmv = sb("xmv", [128, 2, 2], F32)
        xsd = sb("xsd", [128, 2, 1], F32)
        xrstd = sb("xrstd", [128, 2, 1], F32)
        gsm = sb("gsm", [128, 2, 48], F32)
        maskall = sb("maskall", [128, 16, NEXP], F32)
        m0all = sb("m0all", [128, 16, NEXP], F32)
        maskb = sb("maskb", [128, 128], BF16)
        ustb = sb("ustb", [128, 128], BF16)
        ones128b = sb("ones128b", [128, 128], BF16)
        gsel = sb("gsel", [128, 16, 2], F32)
        desti = sb("desti", [128, 2, 16], I32)
        flagi = sb("flagi", [128, 1], I32)
        M_BYTES = 81984
        Mr = sb("Mr", [128, M_BYTES // 2], BF16)
        Mf = Mr.bitcast(F32)

        def mview(off_bytes, dt, n):
            if dt == F32:
                assert off_bytes % 4 == 0
                return Mf[:, off_bytes // 4: off_bytes // 4 + n]
            assert off_bytes % 2 == 0
            return Mr[:, off_bytes // 2: off_bytes // 2 + n]

        pT = st.enter_context(nc.psum_tensor("pT", [128, 8, 128], BF16))
        banks = [st.enter_context(nc.psum_tensor("bank%d" % i, [128, 512], F32)) for i in range(7)]
        bank_bufs = [Buf("bank%d" % i) for i in range(7)]
        pT_b = Buf("pT")
        ring = [0]
        ring_n = [7]

        def ps_alloc():
            i = ring[0] % ring_n[0]
            ring[0] += 1
            return banks[i], bank_bufs[i]

        resid_b = [Buf("resid%d" % s) for s in range(17)]
        xT_b = [Buf("xT%d" % s) for s in range(17)]
        gb_b = [Buf("gbg"), Buf("gbb")]
        xb_b = [Buf("xb0"), Buf("xb1")]
        st_b = [Buf(), Buf()]
        mv_b = [Buf(), Buf()]
        sd_b = [Buf(), Buf()]
        rstd_b = [Buf(), Buf()]
        nb_b = [Buf(), Buf()]
        gsm_b = [Buf(), Buf()]
        const_b = Buf("consts")
        gates_b = [Buf("gates%d" % s) for s in range(16)]
        route_b = Buf("route")
        sgg_b = Buf("sgg")
        wmT_b = Buf("wmT")
        cnt = {"ln": 0, "x": 0}
        out_ops = []

        cparts = []

        def cbuf():
            b = Buf()
            cparts.append(b)
            return b

        def cload(dst, src, eng=SP, bufs=None):
            key = ("c", cnt["x"])
            cnt["x"] += 1
            return S.add(eng, lambda e: e.dma_start(out=dst, in_=src), writes=(bufs if bufs is not None else [cbuf()]), dma_key=key)

        win0_b, wout0_b = Buf("win0"), Buf("wout0")
        win0 = mview(0, BF16, 8 * 1536).rearrange("p (k f) -> p k f", f=1536)
        wout0 = mview(40960, BF16, 8 * 1024).rearrange("p (k f) -> p k f", f=1024)
        for k0 in (0, 4):
            S.add(POOL, lambda e, k0=k0: e.dma_start(out=win0[:, k0:k0 + 4, :], in_=e_w_in[k0 * 128:(k0 + 4) * 128, :].rearrange("(k p) f -> p k f", p=128)),
                  writes=[win0_b], dma_key=("win", k0))
        for k0 in (0, 4):
            S.add(POOL, lambda e, k0=k0: e.dma_start(out=wout0[:, k0:k0 + 4, :], in_=e_w_out[k0 * 128:(k0 + 4) * 128, :].rearrange("(k p) f -> p k f", p=128)),
                  writes=[wout0_b], dma_key=("wout", k0))

        identf_b = cbuf()
        cload(identf[:], ident_d, bufs=[identf_b])
        cload(cvec[:], cvec_d)
        cload(trilf, tril_d, bufs=[gb_b[1]])
        cload(poolcorr[:], poolcorr_d)
        cload(halomask[:], halomask_d)
        cload(sguwf, sgu_w_d.rearrange("h i j -> i h j"), bufs=[gb_b[1]])
        cload(bsrow[:, 0, :], sgrows[2:3, :], bufs=[gb_b[0]])
        cload(poolw[:], pool_w_d.rearrange("g c d -> c g d"), eng=POOL)
        cload(rtr[:], o_router.rearrange("(k p) e -> p k e", p=128), eng=POOL)
        S.add(DVE, lambda e: e.memset(onesf[:], 1.0), writes=[cbuf()])
        S.add(DVE, lambda e: e.memset(ones128b[:], 1.0), writes=[cbuf()])
        cload(ustb[:], ustrict_d, eng=POOL)
        S.add(DVE, lambda e: e.memset(onesb[:], 1.0), writes=[cbuf()])
        S.add(DVE, lambda e: e.memset(epst[:], EPS), writes=[cbuf()])
        identb_b = cbuf()
        S.add(DVE, lambda e: e.tensor_copy(out=identb[:], in_=identf[:]), reads=[identf_b], writes=[identb_b])
        bshl_b = cbuf()
        S.add(DVE, lambda e: e.tensor_copy(out=bshl[:, 0, :], in_=bsrow[:, 0, :]), reads=[gb_b[0]], writes=[bshl_b])
        S.add(DVE, lambda e: e.tensor_copy(out=bsrow[:, 1, :], in_=bshl[:, 0, :]), reads=[bshl_b], writes=[gb_b[0]])
        S.add(DVE, lambda e: e.tensor_tensor(out=bsrow[:, 0, :], in0=bsrow[:, 0, :], in1=bsrow[:, 1, :], op=ALU.subtract), reads=[gb_b[0]], writes=[gb_b[0]])
        S.add(DVE, lambda e: e.tensor_copy(out=bshl[:, 1, :], in_=bsrow[:, 0, :]), reads=[gb_b[0]], writes=[bshl_b])
        for h in range(4):
            bk, bkb = ps_alloc()
            S.add(PE, lambda e, h=h, bk=bk: e.transpose(out=bk[:, 0:128], in_=sguwf[:, h, :], identity=identf[:]), reads=[identf_b, gb_b[1]], writes=[bkb])
            S.add(DVE, lambda e, h=h, bk=bk: e.tensor_tensor(out=wmT[:, h, :], in0=bk[:, 0:128], in1=trilf, op=ALU.mult), reads=[bkb, gb_b[1]], writes=[wmT_b])
        S.add(DVE, lambda e: e.memset(nb[:, 0, :], 0.0), reads=cparts, writes=[const_b, nb_b[0]])

        def load_gb(k, final):
            for i in range(2):
                S.add(SP, lambda e, i=i: e.dma_start(out=gb[:, i, :], in_=lnrows[2 * k + i: 2 * k + i + 1, :].partition_broadcast(128)),
                      writes=[gb_b[i]], dma_key=("gb", i))
                if not final:
                    S.add(DVE, lambda e, i=i: e.tensor_scalar(out=gb[:, i, :], in0=gb[:, i, :], scalar1=ALPHA, scalar2=None, op0=ALU.mult),
                          reads=[gb_b[i]], writes=[gb_b[i]])

        def make_xT(s, par):
            rows, c0 = sub_rows(s), sub_col0(s)

            def tr(e):
                ins = None
                for c in range(8):
                    ins = e.transpose(out=pT[:, c, 0:rows], in_=xb[0:rows, par, c * 128:(c + 1) * 128], identity=identb[0:rows, 0:rows])
                return ins
            S.add(PE, tr, reads=[xb_b[par], identb_b], writes=[pT_b])
            S.add(ACT, lambda e: e.copy(out=xT[:, :, c0:c0 + rows], in_=pT[:, :, 0:rows]), reads=[pT_b], writes=[xT_b[s]])

        def interleave(gx, gy, rx):
            dx, dy = gx is None, gy is None
            while not (dx and dy):
                for _ in range(rx):
                    if not dx:
                        try:
                            next(gx)
                        except StopIteration:
                            dx = True
                if not dy:
                    try:
                        next(gy)
                    except StopIteration:
                        dy = True

        def ln_update_multi(subs, final):
            for _ in ln_update_gen(subs, final):
                pass

        def ln_update_gen(subs, final):
            assert len(subs) <= 2
            info = [(s, sub_rows(s), i % 2) for i, s in enumerate(subs)]
            for s, rows, par in info:
                def bn(e, s=s, rows=rows, par=par):
                    e.bn_stats(out=stats[0:rows, par, 0, :], in_=resid[0:rows, s, 0:512])
                    return e.bn_stats(out=stats[0:rows, par, 1, :], in_=resid[0:rows, s, 512:1024])
                S.add(DVE, bn, reads=[resid_b[s]], writes=[st_b[par]])
            yield
            for s, rows, par in info:
                S.add(DVE, lambda e, rows=rows, par=par: e.bn_aggr(out=mv[0:rows, par, :], in_=stats[0:rows, par, :, :]), reads=[st_b[par]], writes=[mv_b[par]])
            yield
            for s, rows, par in info:
                S.add(ACT, lambda e, rows=rows, par=par: e.activation(out=sd[0:rows, par, :], in_=mv[0:rows, par, 1:2], func=AF.Sqrt, bias=epst[0:rows, 0:1], scale=1.0),
                      reads=[mv_b[par], const_b], writes=[sd_b[par]])
            yield
            for s, rows, par in info:
                S.add(DVE, lambda e, s=s, rows=rows, par=par: e.scalar_tensor_tensor(
                    out=resid[0:rows, s, :], in0=resid[0:rows, s, :], scalar=mv[0:rows, par, 0:1], in1=gb[0:rows, 0, :], op0=ALU.subtract, op1=ALU.mult),
                    reads=[resid_b[s], mv_b[par], gb_b[0]], writes=[resid_b[s]])
            yield
            for s, rows, par in info:
                S.add(DVE, lambda e, rows=rows, par=par: e.reciprocal(out=rstd[0:rows, par, :], in_=sd[0:rows, par, :]), reads=[sd_b[par]], writes=[rstd_b[par]])
            yield
            for s, rows, par in info:
                S.add(DVE, lambda e, s=s, rows=rows, par=par: e.scalar_tensor_tensor(
                    out=resid[0:rows, s, :], in0=resid[0:rows, s, :], scalar=rstd[0:rows, par, :], in1=gb[0:rows, 1, :], op0=ALU.mult, op1=ALU.add),
                    reads=[resid_b[s], rstd_b[par], gb_b[1]], writes=[resid_b[s]])
            yield
            for s, rows, par in info:
                if final:
                    if s >= 1:
                        op = S.add(SP, lambda e, s=s: e.dma_start(out=out_d[128 * (s - 1):128 * s, :], in_=resid[:, s, :]), reads=[resid_b[s]], dma_key=("out", s))
                        out_ops.append(op)
                else:
                    S.add(ACT, lambda e, s=s, rows=rows, par=par: e.activation(out=xb[0:rows, par, :], in_=resid[0:rows, s, :], func=AF.Identity, scale=1.0 / ALPHA),
                          reads=[resid_b[s]], writes=[xb_b[par]])
            yield
            if not final:
                for s, rows, par in info:
                    make_xT(s, par)
                    yield

        def ln_update(s, final):
            ln_update_multi([s], final)

        def wload(dst, src, wbufs, key):
            return S.add(POOL, lambda e: e.dma_start(out=dst, in_=src), writes=wbufs, dma_key=key)

        def fresh_bufs(n, barrier):
            bl = []
            for i in range(n):
                b = Buf()
                b.readers = list(barrier)
                bl.append(b)
            return bl

        for s in range(17):
            rows, c0 = sub_rows(s), sub_col0(s)
            par = s % 2
            S.add(SP, lambda e, s=s, rows=rows, c0=c0: e.dma_start(out=resid[0:rows, s, :], in_=x_c[c0:c0 + rows, :]), writes=[resid_b[s]], dma_key=("x", s))
            S.add(ACT, lambda e, s=s, rows=rows, par=par: e.copy(out=xb[0:rows, par, :], in_=resid[0:rows, s, :]), reads=[resid_b[s]], writes=[xb_b[par]])
            make_xT(s, par)
            S.add(DVE, lambda e, s=s, rows=rows: e.tensor_scalar(out=resid[0:rows, s, :], in0=resid[0:rows, s, :], scalar1=ALPHA, scalar2=None, op0=ALU.mult),
                  reads=[resid_b[s]], writes=[resid_b[s]])

        CW, CB, NG, NB_, PS, CC = 0, 124, 128, 132, 136, 140

        def out_proj(ycat, ycat_b, wout, wout_b, subs, loc0):
            for _ in out_proj_gen(ycat, ycat_b, wout, wout_b, subs, loc0):
                pass

        def out_proj_gen(ycat, ycat_b, wout, wout_b, subs, loc0):
            for si, s in enumerate(subs):
                rows = sub_rows(s)
                loc = loc0 + 128 * si
                for half in range(2):
                    bk, bkb = ps_alloc()

                    def mm(e, bk=bk, rows=rows, loc=loc, half=half):
                        ins = None
                        for k in range(8):
                            ins = e.matmul(bk[0:rows, :], lhsT=ycat[:, k, loc:loc + rows], rhs=wout[:, k, half * 512:(half + 1) * 512],
                                           start=(k == 0), stop=(k == 7))
                        return ins
                    S.add(PE, mm, reads=[ycat_b, wout_b], writes=[bkb])
                    S.add(DVE, lambda e, bk=bk, rows=rows, s=s, half=half: e.tensor_tensor(
                        out=resid[0:rows, s, half * 512:(half + 1) * 512], in0=resid[0:rows, s, half * 512:(half + 1) * 512], in1=bk[0:rows, :], op=ALU.add),
                        reads=[bkb, resid_b[s]], writes=[resid_b[s]])
                    yield

        def mixer_even(final):
            bar = []
            win_b, wout_b = win0_b, wout0_b
            tmp_bar = bar
            win, wout = win0, wout0
            load_gb(0, final)
            W = 256
            offA = 24576
            aT = [mview(offA + i * 2288, BF16, 4 * 286).rearrange("p (c t) -> p c t", t=286) for i in range(2)]
            acc = mview(offA + 4576, F32, 4 * W).rearrange("p (c t) -> p c t", t=W)
            sigt = [mview(offA + 8672 + i * 1024, F32, W) for i in range(2)]
            NRD, NRA = 6, 4
            ringD = [mview(offA + 10720 + i * 512, BF16, W) for i in range(NRD)]
            ringA = [mview(offA + 13792 + i * 512, BF16, W) for i in range(NRA)]
            assert 15840 <= 16384
            ring_n[0] = 5
            cbank = [banks[5], banks[6]]
            cbank_b = [bank_bufs[5], bank_bufs[6]]
            offB = 57344
            sqt = mview(offB, F32, 4 * W).rearrange("p (c t) -> p c t", t=W)
            meant = mview(offB + 4096, F32, W)
            vart = mview(offB + 5120, F32, W)
            rstt = mview(offB + 6144, F32, W)
            hbT = [mview(offB + 7168 + i * 4336, F32, 4 * 271).rearrange("p (c t) -> p c t", t=271) for i in range(2)]
            tmpA = mview(offB + 15840, F32, 271)
            tmpB = mview(offB + 16924, F32, 271)
            pooled = mview(offB + 18008, BF16, 4 * W).rearrange("p (c t) -> p c t", t=W)
            ycat = mview(offB + 20056, BF16, 8 * W).rearrange("p (c t) -> p c t", t=W)
            assert 24152 <= 24576
            tb = lambda: fresh_bufs(1, tmp_bar)[0]
            aT_b = [[tb() for _ in range(4)] for _ in range(2)]
            acc_b = [tb() for _ in range(4)]
            sig_b = [tb(), tb()]
            sq_b = [tb() for _ in range(4)]
            mean_b, var_b, rst_b = tb(), tb(), tb()
            hb_b = [[tb() for _ in range(4)] for _ in range(2)]
            tA_b, tB_b = tb(), tb()
            pooled_b = [tb() for _ in range(4)]
            ringD_b = [tb() for _ in range(NRD)]
            ringA_b = [tb() for _ in range(NRA)]
            ucnt = {"d": 0, "a": 0}
            ycat_bs = [tb(), sgg_b]
            ycats = [ycat, sgg.bitcast(BF16)[:, :, :].rearrange("p a b -> p (a b)").rearrange("p (c t) -> p c t", t=W)]
            S.add(DVE, lambda e: e.memset(aT[0][:, :, 0:30], 0.0), writes=aT_b[0])
            S.add(DVE, lambda e: e.memset(hbT[0][:, :, 0:15], 0.0), writes=hb_b[0])
            tiles = [(0, HALO, [0])] + [(HALO + W * i, W, [1 + 2 * i, 2 + 2 * i]) for i in range(8)]
            def Y_even(ti):
                c0, w, subs = tiles[ti]
                yield from out_proj_gen(ycats[ti % 2], ycat_bs[ti % 2], wout, wout_b, subs, 0)
                yield from ln_update_gen(subs, final)

            def X_even(ti):
                c0, w, subs = tiles[ti]
                cur, nxt = ti % 2, (ti + 1) % 2
                xr = [xT_b[s] for s in subs]
                ycat, ycat_b = ycats[ti % 2], ycat_bs[ti % 2]
                for ch in range(4):
                    bk, bkb = ps_alloc()

                    def mm(e, bk=bk, ch=ch, c0=c0, w=w):
                        ins = None
                        for part, oc in ((0, ch), (1, 4 + ch)):
                            for k in range(8):
                                ins = e.matmul(bk[:, part * 256: part * 256 + w], lhsT=win[:, k, oc * 128:(oc + 1) * 128], rhs=xT[:, k, c0:c0 + w],
                                               start=(k == 0), stop=(k == 7))
                        return ins
                    S.add(PE, mm, reads=[win_b] + xr, writes=[bkb])
                    sp = ch % 2
                    S.add(ACT, lambda e, bk=bk, w=w, sp=sp: e.activation(out=sigt[sp][:, 0:w], in_=bk[:, 256:256 + w], func=AF.Sigmoid), reads=[bkb], writes=[sig_b[sp]])
                    S.add(DVE, lambda e, bk=bk, w=w, sp=sp, ch=ch, cur=cur: e.tensor_tensor(out=aT[cur][:, ch, 30:30 + w], in0=bk[:, 0:w], in1=sigt[sp][:, 0:w], op=ALU.mult),
                          reads=[bkb, sig_b[sp]], writes=[aT_b[cur][ch]])
                    yield
                for gp in range(4):
                    if gp % 2 == 0:
                        bkB, bkBb = ps_alloc()

                    def mm(e, bk=bkB, gp=gp, c0=c0, w=w):
                        ins = None
                        for k in range(8):
                            ins = e.matmul(bk[:, (gp % 2) * 256:(gp % 2) * 256 + w], lhsT=win[:, k, (8 + gp) * 128:(9 + gp) * 128], rhs=xT[:, k, c0:c0 + w],
                                           start=(k == 0), stop=(k == 7))
                        return ins
                    S.add(PE, mm, reads=[win_b] + xr, writes=[bkBb])
                    S.add(ACT, lambda e, bk=bkB, gp=gp, w=w, cur=cur: e.copy(out=hbT[cur][:, gp, 15:15 + w], in_=bk[:, (gp % 2) * 256:(gp % 2) * 256 + w]),
                          reads=[bkBb], writes=[hb_b[cur][gp]])
                    if gp % 2 == 1:
                        yield
                for j in range(31):
                    for ch in range(4):
                        col = CW + ch * 31 + j
                        src = aT[cur][:, ch, j:j + w]
                        if j % 2 == 1 and ch != 3:
                            sl = ucnt["a"] % NRA
                            ucnt["a"] += 1
                            bt, btb = ringA[sl], ringA_b[sl]
                            S.add(ACT, lambda e, bt=bt, src=src, col=col, w=w: e.activation(out=bt[:, 0:w], in_=src, func=AF.Identity, scale=cvec[:, col:col + 1]),
                                  reads=[aT_b[cur][ch], const_b], writes=[btb])
                        else:
                            sl = ucnt["d"] % NRD
                            ucnt["d"] += 1
                            bt, btb = ringD[sl], ringD_b[sl]
                            S.add(DVE, lambda e, bt=bt, src=src, col=col, w=w: e.tensor_scalar(out=bt[:, 0:w], in0=src, scalar1=cvec[:, col:col + 1], scalar2=None, op0=ALU.mult),
                                  reads=[aT_b[cur][ch], const_b], writes=[btb])
                        cb = cbank[ch // 2]
                        S.add(PE, lambda e, cb=cb, ch=ch, bt=bt, w=w, j=j: e.matmul(cb[:, (ch % 2) * 256:(ch % 2) * 256 + w], lhsT=identb[:], rhs=bt[:, 0:w], start=(j == 0 and ch % 2 == 0), stop=(j == 30 and ch % 2 == 1),
                                                                                    skip_group_check=True),
                              reads=[btb, identb_b], writes=[cbank_b[ch // 2]])
                        if j % 2 == 1 and ch == 3:
                            yield
                for ch in range(4):
                    cb = cbank[ch // 2]
                    S.add(ACT, lambda e, cb=cb, ch=ch, w=w: e.activation(out=acc[:, ch, 0:w], in_=cb[:, (ch % 2) * 256:(ch % 2) * 256 + w], func=AF.Identity, bias=cvec[:, CB + ch: CB + ch + 1], scale=1.0),
                          reads=[cbank_b[ch // 2], const_b], writes=[acc_b[ch]])
                yield
                if ti + 1 < len(tiles):
                    S.add(ACT, lambda e, w=w, cur=cur, nxt=nxt: e.copy(out=aT[nxt][:, :, 0:30], in_=aT[cur][:, :, w:w + 30]),
                          reads=aT_b[cur], writes=aT_b[nxt])
                for ch in range(4):
                    S.add(ACT, lambda e, ch=ch, w=w: e.activation(out=sqt[:, ch, 0:w], in_=acc[:, ch, 0:w], func=AF.Square), reads=[acc_b[ch]], writes=[sq_b[ch]])
                bk, bkb = ps_alloc()

                def mmst(e, bk=bk, w=w):
                    ins = None
                    for ch in range(4):
                        ins = e.matmul(bk[:, 0:w], lhsT=onesf[:], rhs=acc[:, ch, 0:w], start=(ch == 0), stop=(ch == 3))
                    for ch in range(4):
                        ins = e.matmul(bk[:, 256:256 + w], lhsT=onesf[:], rhs=sqt[:, ch, 0:w], start=(ch == 0), stop=(ch == 3))
                    return ins
                S.add(PE, mmst, reads=acc_b + sq_b + [const_b], writes=[bkb])
                S.add(DVE, lambda e, bk=bk, w=w: e.tensor_scalar(out=meant[:, 0:w], in0=bk[:, 0:w], scalar1=1.0 / 512, scalar2=None, op0=ALU.mult), reads=[bkb], writes=[mean_b])
                S.add(DVE, lambda e, w=w: e.tensor_tensor(out=vart[:, 0:w], in0=meant[:, 0:w], in1=meant[:, 0:w], op=ALU.mult), reads=[mean_b], writes=[var_b])
                S.add(DVE, lambda e, bk=bk, w=w: e.scalar_tensor_tensor(out=vart[:, 0:w], in0=bk[:, 256:256 + w], scalar=1.0 / 512, in1=vart[:, 0:w], op0=ALU.mult, op1=ALU.subtract),
                      reads=[bkb, var_b], writes=[var_b])
                yield
                S.add(ACT, lambda e, w=w: e.activation(out=rstt[:, 0:w], in_=vart[:, 0:w], func=AF.Sqrt, bias=epst[:, 0:1], scale=1.0), reads=[var_b, const_b], writes=[rst_b])
                S.add(DVE, lambda e, w=w: e.reciprocal(out=rstt[:, 0:w], in_=rstt[:, 0:w]), reads=[rst_b], writes=[rst_b])
                yield
                for ch in range(4):
                    S.add(DVE, lambda e, ch=ch, w=w: e.tensor_tensor(out=acc[:, ch, 0:w], in0=acc[:, ch, 0:w], in1=meant[:, 0:w], op=ALU.subtract),
                          reads=[acc_b[ch], mean_b], writes=[acc_b[ch]])
                yield
                for ch in range(4):
                    S.add(DVE, lambda e, ch=ch, w=w: e.tensor_tensor(out=acc[:, ch, 0:w], in0=acc[:, ch, 0:w], in1=rstt[:, 0:w], op=ALU.mult),
                          reads=[acc_b[ch], rst_b], writes=[acc_b[ch]])
                yield
                for ch in range(4):
                    S.add(ACT, lambda e, ch=ch, w=w, ycat=ycat: e.activation(out=ycat[:, ch, 0:w], in_=acc[:, ch, 0:w], func=AF.Silu, bias=cvec[:, NB_ + ch: NB_ + ch + 1],
                                                                 scale=cvec[:, NG + ch: NG + ch + 1]), reads=[acc_b[ch], const_b], writes=[ycat_b])
                yield
                E = 15 + w
                for gp in range(4):
                    hsrc = hbT[cur]
                    S.add(DVE, lambda e, gp=gp, E=E, hsrc=hsrc: e.tensor_tensor(out=tmpA[:, 1:E], in0=hsrc[:, gp, 1:E], in1=hsrc[:, gp, 0:E - 1], op=ALU.add),
                          reads=[hb_b[cur][gp]], writes=[tA_b])
                    wsrc, wsb = tmpA, tA_b
                    if gp >= 1:
                        S.add(DVE, lambda e, E=E: e.tensor_tensor(out=tmpB[:, 3:E], in0=tmpA[:, 3:E], in1=tmpA[:, 1:E - 2], op=ALU.add), reads=[tA_b], writes=[tB_b])
                        wsrc, wsb = tmpB, tB_b
                    if gp >= 2:
                        S.add(DVE, lambda e, E=E: e.tensor_tensor(out=tmpA[:, 7:E], in0=tmpB[:, 7:E], in1=tmpB[:, 3:E - 4], op=ALU.add), reads=[tB_b], writes=[tA_b])
                        wsrc, wsb = tmpA, tA_b
                    if gp >= 3:
                        S.add(DVE, lambda e, E=E: e.tensor_tensor(out=tmpB[:, 15:E], in0=tmpA[:, 15:E], in1=tmpA[:, 7:E - 8], op=ALU.add), reads=[tA_b], writes=[tB_b])
                        wsrc, wsb = tmpB, tB_b
                    if ti == 1:
                        S.add(DVE, lambda e, gp=gp, wsrc=wsrc: e.tensor_tensor(out=wsrc[:, 15:31], in0=wsrc[:, 15:31], in1=poolcorr[:, gp * 16:(gp + 1) * 16], op=ALU.mult),
                              reads=[wsb, const_b], writes=[wsb])
                    S.add(DVE, lambda e, gp=gp, wsrc=wsrc, E=E, w=w, hsrc=hsrc: e.scalar_tensor_tensor(
                        out=pooled[:, gp, 0:w], in0=wsrc[:, 15:E], scalar=1.0 / (2 ** (gp + 1)), in1=hsrc[:, gp, 15:E], op0=ALU.mult, op1=ALU.subtract),
                        reads=[wsb, hb_b[cur][gp]], writes=[pooled_b[gp]])
                    if gp % 2 == 0:
                        bkP, bkPb = ps_alloc()
                    S.add(PE, lambda e, bk=bkP, gp=gp, w=w: e.matmul(bk[:, (gp % 2) * 256:(gp % 2) * 256 + w], lhsT=poolw[:, gp, :], rhs=pooled[:, gp, 0:w], start=True, stop=True),
                          reads=[pooled_b[gp], const_b], writes=[bkPb])
                    S.add(ACT, lambda e, bk=bkP, gp=gp, w=w, ycat=ycat: e.activation(out=ycat[:, 4 + gp, 0:w], in_=bk[:, (gp % 2) * 256:(gp % 2) * 256 + w], func=AF.Identity,
                                                                          scale=cvec[:, PS + gp: PS + gp + 1]), reads=[bkPb, const_b], writes=[ycat_b])
                    if gp % 2 == 1:
                        yield
                if ti + 1 < len(tiles):
                    S.add(ACT, lambda e, w=w, cur=cur, nxt=nxt: e.copy(out=hbT[nxt][:, :, 0:15], in_=hbT[cur][:, :, w:w + 15]),
                          reads=hb_b[cur], writes=hb_b[nxt])
                yield

            for ti in range(len(tiles)):
                if not PIPE:
                    interleave(X_even(ti), None, 1)
                    interleave(Y_even(ti), None, 1)
                else:
                    interleave(X_even(ti), Y_even(ti - 1) if ti >= 1 else None, 2)
            if PIPE:
                interleave(Y_even(len(tiles) - 1), None, 1)
            ring_n[0] = 7

        def ffn(experts, with_halo, final):
            bar = S.last_ops()
            wg_b = fresh_bufs(2, bar)
            wu_b = fresh_bufs(2, bar)
            wd_b = fresh_bufs(2, bar)
            hT_b = [fresh_bufs(4, bar) for _ in range(2)]
            sg_b = fresh_bufs(2, bar)
            wgv = [mview(i * 24576, BF16, 4096).rearrange("p (k f) -> p k f", f=512) for i in range(2)]
            wuv = [mview(i * 24576 + 8192, BF16, 4096).rearrange("p (k f) -> p k f", f=512) for i in range(2)]
            wdv = [mview(i * 24576 + 16384, BF16, 4096).rearrange("p (c d) -> p c d", d=1024) for i in range(2)]
            hTv = [mview(49152 + i * 4096, BF16, 2048).rearrange("p (c t) -> p c t", t=512) for i in range(2)]
            sgv = [mview(57344 + i * 2048, F32, 512) for i in range(2)]
            tiles = ([(0, HALO, [0])] if with_halo else []) + [(HALO + 512 * i, 512, [1 + 4 * i + q for q in range(4)]) for i in range(4)]
            items = []
            groups = []
            for (wg_ap, wu_ap, wd_ap, F, eidx) in experts:
                nfc = F // 128
                for f0 in range(0, nfc, 4):
                    n = min(4, nfc - f0)
                    gi = len(groups)
                    slot = gi % 2
                    groups.append((wg_ap, wu_ap, wd_ap, f0, n, slot))
                    for ti_, tl in enumerate(tiles):
                        items.append((slot, n, tl, eidx, gi if ti_ == 0 else None))

            def load_group(gi):
                wg_ap, wu_ap, wd_ap, f0, n, slot = groups[gi]
                wload(wgv[slot][:, :, 0:n * 128], wg_ap[:, f0 * 128:(f0 + n) * 128].rearrange("(k p) f -> p k f", p=128), [wg_b[slot]], ("wg", slot))
                wload(wuv[slot][:, :, 0:n * 128], wu_ap[:, f0 * 128:(f0 + n) * 128].rearrange("(k p) f -> p k f", p=128), [wu_b[slot]], ("wu", slot))
                wload(wdv[slot][:, 0:n, :], wd_ap[f0 * 128:(f0 + n) * 128, :].rearrange("(c p) d -> p c d", p=128), [wd_b[slot]], ("wd", slot))

            def step1(it, hp):
                slot, n, (c0, w, subs), eidx, _g = it
                xr = [xT_b[s] for s in subs]
                for fc in range(n):
                    bg, bgb = ps_alloc()
                    bu, bub = ps_alloc()

                    def mm(e, bg=bg, bu=bu, fc=fc, slot=slot, c0=c0, w=w):
                        ins = None
                        for k in range(8):
                            ins = e.matmul(bg[:, 0:w], lhsT=wgv[slot][:, k, fc * 128:(fc + 1) * 128], rhs=xT[:, k, c0:c0 + w], start=(k == 0), stop=(k == 7))
                        for k in range(8):
                            ins = e.matmul(bu[:, 0:w], lhsT=wuv[slot][:, k, fc * 128:(fc + 1) * 128], rhs=xT[:, k, c0:c0 + w], start=(k == 0), stop=(k == 7))
                        return ins
                    S.add(PE, mm, reads=[wg_b[slot], wu_b[slot]] + xr, writes=[bgb, bub])
                    sp = fc % 2
                    S.add(ACT, lambda e, bg=bg, w=w, sp=sp: e.activation(out=sgv[sp][:, 0:w], in_=bg[:, 0:w], func=AF.Silu), reads=[bgb], writes=[sg_b[sp]])
                    S.add(DVE, lambda e, bu=bu, w=w, sp=sp, fc=fc, hp=hp: e.tensor_tensor(out=hTv[hp][:, fc, 0:w], in0=bu[:, 0:w], in1=sgv[sp][:, 0:w], op=ALU.mult),
                          reads=[bub, sg_b[sp]], writes=[hT_b[hp][fc]])

            def step2(it, hp):
                slot, n, (c0, w, subs), eidx, _g = it
                for si, s in enumerate(subs):
                    rows = sub_rows(s)
                    loc = 128 * si
                    for half in range(2):
                        bk, bkb = ps_alloc()

                        def mm(e, bk=bk, rows=rows, loc=loc, half=half, slot=slot, n=n, hp=hp):
                            ins = None
                            for fc in range(n):
                                ins = e.matmul(bk[0:rows, :], lhsT=hTv[hp][:, fc, loc:loc + rows], rhs=wdv[slot][:, fc, half * 512:(half + 1) * 512],
                                               start=(fc == 0), stop=(fc == n - 1))
                            return ins
                        S.add(PE, mm, reads=hT_b[hp][0:n] + [wd_b[slot]], writes=[bkb])
                        rs = resid[0:rows, s, half * 512:(half + 1) * 512]
                        if eidx is None:
                            S.add(DVE, lambda e, bk=bk, rows=rows, rs=rs: e.tensor_tensor(out=rs, in0=rs, in1=bk[0:rows, :], op=ALU.add),
                                  reads=[bkb, resid_b[s]], writes=[resid_b[s]])
                        else:
                            S.add(DVE, lambda e, bk=bk, rows=rows, rs=rs, s=s, eidx=eidx: e.scalar_tensor_tensor(
                                out=rs, in0=bk[0:rows, :], scalar=gates[0:rows, s - 1, eidx:eidx + 1], in1=rs, op0=ALU.mult, op1=ALU.add),
                                reads=[bkb, resid_b[s], gates_b[s - 1]], writes=[resid_b[s]])

            load_group(0)
            for i, it in enumerate(items):
                step1(it, i % 2)
                if i >= 1:
                    step2(items[i - 1], (i - 1) % 2)
                if it[4] is not None and it[4] + 1 < len(groups):
                    load_group(it[4] + 1)
            step2(items[-1], (len(items) - 1) % 2)

        def mixer_odd(final, bar):
            win_b, wout_b = fresh_bufs(2, bar)
            win = mview(0, BF16, 8 * 2560).rearrange("p (k f) -> p k f", f=2560)
            wout = mview(40960, BF16, 8 * 1024).rearrange("p (k f) -> p k f", f=1024)
            for k0 in (0, 4):
                wload(win[:, k0:k0 + 4, :], o_w_in[k0 * 128:(k0 + 4) * 128, :].rearrange("(k p) f -> p k f", p=128), [win_b], ("win", k0))
            for k0 in (0, 4):
                wload(wout[:, k0:k0 + 4, :], o_w_out[k0 * 128:(k0 + 4) * 128, :].rearrange("(k p) f -> p k f", p=128), [wout_b], ("wout", k0))
            load_gb(2, final)
            W = 256
            offB = 57344
            cvT = [mview(offB + i * 4128, F32, 4 * 258).rearrange("p (c t) -> p c t", t=258) for i in range(2)]
            vct = [mview(offB + 8256 + i * 1024, F32, W) for i in range(2)]
            acc = mview(offB + 10304, F32, 4 * W).rearrange("p (c t) -> p c t", t=W)
            uT = mview(offB + 14400, BF16, 4 * W).rearrange("p (c t) -> p c t", t=W)
            vg = mview(offB + 16448, F32, 512)
            vln = [mview(offB + 18496 + i * 1024, BF16, 512) for i in range(2)]
            ycat = mview(offB + 20544, BF16, 8 * W).rearrange("p (c t) -> p c t", t=W)
            assert offB + 24640 <= M_BYTES
            tb = lambda: fresh_bufs(1, bar)[0]
            cv_b = [[tb() for _ in range(4)] for _ in range(2)]
            vct_b = [tb(), tb()]
            acc_b = [tb() for _ in range(4)]
            uT_b = [tb() for _ in range(4)]
            vg_b = tb()
            vln_b = [tb(), tb()]
            ycat_bs = [tb(), resid_b[0]]
            ycats = [ycat, resid.bitcast(BF16)[:, 0, :].rearrange("p (c t) -> p c t", t=W)]
            cload(sgg[:, 0, :], sgrows[0:1, :].partition_broadcast(128), bufs=[sgg_b])
            cload(sgg[:, 1, :], sgrows[1:2, :].partition_broadcast(128), bufs=[sgg_b])
            tiles = [(0, HALO, [0])] + [(HALO + W * i, W, [1 + 2 * i, 2 + 2 * i]) for i in range(8)]
            S.add(DVE, lambda e: e.memset(cvT[0][:, :, 0:2], 0.0), writes=cv_b[0])

            def Y_odd(ti):
                c0, w, subs = tiles[ti]
                yield from out_proj_gen(ycats[ti % 2], ycat_bs[ti % 2], wout, wout_b, subs, 0)
                yield from ln_update_gen(subs, final)

            xst_b = [tb(), tb()]
            xmv_b = [tb(), tb()]
            xsd_b = [tb(), tb()]
            xrstd_b = [tb(), tb()]
            vstate = {"v": 0}

            def X_odd(ti):
                c0, w, subs = tiles[ti]
                cur, nxt = ti % 2, (ti + 1) % 2
                xr = [xT_b[s] for s in subs]
                halo = (ti == 0)
                ycat, ycat_b = ycats[ti % 2], ycat_bs[ti % 2]
                for ch in range(4):
                    bk, bkb = ps_alloc()

                    def mm(e, bk=bk, ch=ch, c0=c0, w=w):
                        ins = None
                        for part, oc in ((0, 4 + ch), (1, 8 + ch)):
                            for k in range(8):
                                ins = e.matmul(bk[:, part * 256: part * 256 + w], lhsT=win[:, k, oc * 128:(oc + 1) * 128], rhs=xT[:, k, c0:c0 + w],
                                               start=(k == 0), stop=(k == 7))
                        return ins
                    S.add(PE, mm, reads=[win_b] + xr, writes=[bkb])
                    sp = ch % 2
                    if halo:
                        S.add(ACT, lambda e, bk=bk, w=w, sp=sp: e.activation(out=vct[sp][:, 0:w], in_=bk[:, 256:256 + w], func=AF.Identity, scale=halomask[:, 0:1]),
                              reads=[bkb, const_b], writes=[vct_b[sp]])
                    else:
                        S.add(ACT, lambda e, bk=bk, w=w, sp=sp: e.copy(out=vct[sp][:, 0:w], in_=bk[:, 256:256 + w]), reads=[bkb], writes=[vct_b[sp]])
                    S.add(DVE, lambda e, bk=bk, w=w, sp=sp, ch=ch, cur=cur: e.tensor_tensor(out=cvT[cur][:, ch, 2:2 + w], in0=bk[:, 0:w], in1=vct[sp][:, 0:w], op=ALU.mult),
                          reads=[bkb, vct_b[sp]], writes=[cv_b[cur][ch]])
                    yield
                if ti + 1 < len(tiles):
                    S.add(ACT, lambda e, w=w, cur=cur, nxt=nxt: e.copy(out=cvT[nxt][:, :, 0:2], in_=cvT[cur][:, :, w:w + 2]), reads=cv_b[cur], writes=cv_b[nxt])
                if halo:
                    return
                for ch in range(4):
                    S.add(DVE, lambda e, ch=ch, w=w, cur=cur: e.tensor_scalar(out=acc[:, ch, 0:w], in0=cvT[cur][:, ch, 0:w], scalar1=cvec[:, CC + ch * 3: CC + ch * 3 + 1],
                                                                             scalar2=None, op0=ALU.mult), reads=[cv_b[cur][ch], const_b], writes=[acc_b[ch]])
                    for j in (1, 2):
                        S.add(DVE, lambda e, ch=ch, w=w, cur=cur, j=j: e.scalar_tensor_tensor(
                            out=acc[:, ch, 0:w], in0=cvT[cur][:, ch, j:j + w], scalar=cvec[:, CC + ch * 3 + j: CC + ch * 3 + j + 1], in1=acc[:, ch, 0:w],
                            op0=ALU.mult, op1=ALU.add), reads=[cv_b[cur][ch], acc_b[ch], const_b], writes=[acc_b[ch]])
                    yield
                for ch in range(4):
                    if ch % 2 == 0:
                        bk, bkb = ps_alloc()

                    def mm(e, bk=bk, ch=ch, c0=c0, w=w):
                        ins = None
                        for k in range(8):
                            ins = e.matmul(bk[:, (ch % 2) * 256:(ch % 2) * 256 + w], lhsT=win[:, k, ch * 128:(ch + 1) * 128], rhs=xT[:, k, c0:c0 + w],
                                           start=(k == 0), stop=(k == 7))
                        return ins
                    S.add(PE, mm, reads=[win_b] + xr, writes=[bkb])
                    S.add(DVE, lambda e, bk=bk, ch=ch, w=w, ycat=ycat: e.tensor_tensor(out=ycat[:, ch, 0:w], in0=bk[:, (ch % 2) * 256:(ch % 2) * 256 + w], in1=acc[:, ch, 0:w], op=ALU.mult),
                          reads=[bkb, acc_b[ch]], writes=[ycat_b])
                    if ch % 2 == 1:
                        yield
                for ch in range(4):
                    if ch % 2 == 0:
                        bk, bkb = ps_alloc()

                    def mm(e, bk=bk, ch=ch, c0=c0, w=w):
                        ins = None
                        for k in range(8):
                            ins = e.matmul(bk[:, (ch % 2) * 256:(ch % 2) * 256 + w], lhsT=win[:, k, (12 + ch) * 128:(13 + ch) * 128], rhs=xT[:, k, c0:c0 + w],
                                           start=(k == 0), stop=(k == 7))
                        return ins
                    S.add(PE, mm, reads=[win_b] + xr, writes=[bkb])
                    S.add(ACT, lambda e, bk=bk, ch=ch, w=w: e.activation(out=uT[:, ch, 0:w], in_=bk[:, (ch % 2) * 256:(ch % 2) * 256 + w], func=AF.Gelu_apprx_tanh),
                          reads=[bkb], writes=[uT_b[ch]])
                    if ch % 2 == 1:
                        yield
                for si, s in enumerate(subs):
                    cs = sub_col0(s)
                    vp = vstate["v"] % 2
                    vstate["v"] += 1
                    par = vp
                    bk, bkb = ps_alloc()

                    def mm(e, bk=bk, cs=cs):
                        ins = None
                        for k in range(8):
                            ins = e.matmul(bk[:, :], lhsT=xT[:, k, cs:cs + 128], rhs=win[:, k, 2048:2560], start=(k == 0), stop=(k == 7))
                        return ins
                    S.add(PE, mm, reads=[win_b, xT_b[s]], writes=[bkb])
                    S.add(ACT, lambda e, bk=bk: e.activation(out=vg[:, :], in_=bk[:, :], func=AF.Gelu_apprx_tanh), reads=[bkb], writes=[vg_b])
                    yield
                    S.add(DVE, lambda e, par=par: e.bn_stats(out=xstats[:, par, :], in_=vg[:, :]), reads=[vg_b], writes=[xst_b[par]])
                    yield
                    S.add(DVE, lambda e, par=par: e.bn_aggr(out=xmv[:, par, :], in_=xstats[:, par:par + 1, :]), reads=[xst_b[par]], writes=[xmv_b[par]])
                    yield
                    S.add(ACT, lambda e, par=par: e.activation(out=xsd[:, par, :], in_=xmv[:, par, 1:2], func=AF.Sqrt, bias=epst[:, 0:1], scale=1.0),
                          reads=[xmv_b[par], const_b], writes=[xsd_b[par]])
                    S.add(DVE, lambda e, par=par: e.scalar_tensor_tensor(out=vg[:, :], in0=vg[:, :], scalar=xmv[:, par, 0:1], in1=sgg[:, 0, :], op0=ALU.subtract, op1=ALU.mult),
                          reads=[vg_b, xmv_b[par], sgg_b], writes=[vg_b])
                    yield
                    S.add(DVE, lambda e, par=par: e.reciprocal(out=xrstd[:, par, :], in_=xsd[:, par, :]), reads=[xsd_b[par]], writes=[xrstd_b[par]])
                    yield
                    S.add(DVE, lambda e, vp=vp, par=par: e.scalar_tensor_tensor(out=vln[vp][:, :], in0=vg[:, :], scalar=xrstd[:, par, :], in1=sgg[:, 1, :], op0=ALU.mult, op1=ALU.add),
                          reads=[vg_b, xrstd_b[par], sgg_b], writes=[vln_b[vp]])
                    yield
                    bk2, bk2b = ps_alloc()

                    def mm2(e, bk2=bk2, vp=vp):
                        ins = None
                        for h in range(4):
                            e.matmul(bk2[:, h * 128:(h + 1) * 128], lhsT=vln[vp][:, h * 128:(h + 1) * 128], rhs=wmT[:, h, :], start=True, stop=False)
                            e.matmul(bk2[:, h * 128:(h + 1) * 128], lhsT=onesb[0:1, :], rhs=bshl[0:1, 0, h * 128:(h + 1) * 128], start=False, stop=False)
                            ins = e.matmul(bk2[:, h * 128:(h + 1) * 128], lhsT=onesb[0:1, :], rhs=bshl[0:1, 1, h * 128:(h + 1) * 128], start=False, stop=True)
                        return ins
                    S.add(PE, mm2, reads=[vln_b[vp], wmT_b, const_b], writes=[bk2b])
                    loc = 128 * si
                    S.add(DVE, lambda e, bk2=bk2, loc=loc, ycat=ycat: e.tensor_tensor(out=ycat[:, 4:8, loc:loc + 128], in0=bk2[:, :].rearrange("p (h i) -> p h i", i=128),
                                                                           in1=uT[:, :, loc:loc + 128], op=ALU.mult), reads=[bk2b] + uT_b, writes=[ycat_b])
                    yield

            for ti in range(len(tiles)):
                if not PIPE:
                    interleave(X_odd(ti), None, 1)
                    if ti >= 1:
                        interleave(Y_odd(ti), None, 1)
                else:
                    interleave(X_odd(ti), Y_odd(ti - 1) if ti >= 2 else None, 2)
            if PIPE:
                interleave(Y_odd(len(tiles) - 1), None, 1)

        def gating_all():
            lgall = gbt[:, 0:128].rearrange("p (s e) -> p s e", e=NEXP)
            lg2 = gbt[:, 128:256].rearrange("p (s e) -> p s e", e=NEXP)
            exa = gbt[:, 256:384].rearrange("p (s e) -> p s e", e=NEXP)
            mx1 = gbt[:, 384:400].rearrange("p (s o) -> p s o", o=1)
            mx2 = gbt[:, 400:416].rearrange("p (s o) -> p s o", o=1)
            ssm = gbt[:, 416:432].rearrange("p (s o) -> p s o", o=1)
            G = gb_b[0]
            bk, bkb = ps_alloc()

            def mm(e):
                ins = None
                for s in range(1, 17):
                    cs = sub_col0(s)
                    for k in range(8):
                        ins = e.matmul(bk[:, (s - 1) * 8:s * 8], lhsT=xT[:, k, cs:cs + 128], rhs=rtr[:, k, :], start=(k == 0), stop=(k == 7))
                return ins
            S.add(PE, mm, reads=xT_b[1:] + [const_b], writes=[bkb])
            bc = lambda t: t.to_broadcast([128, 16, NEXP])
            S.add(DVE, lambda e: e.tensor_copy(out=lgall.rearrange("p s e -> p (s e)"), in_=bk[:, 0:128]), reads=[bkb, G], writes=[G])
            S.add(DVE, lambda e: e.tensor_reduce(out=mx1, in_=lgall, axis=AX.X, op=ALU.max), reads=[G], writes=[G])
            S.add(DVE, lambda e: e.tensor_tensor(out=m0all[:, :, :], in0=lgall, in1=bc(mx1), op=ALU.is_equal), reads=[G], writes=[route_b])
            S.add(DVE, lambda e: e.scalar_tensor_tensor(out=lg2, in0=m0all[:, :, :], scalar=-1e30, in1=lgall, op0=ALU.mult, op1=ALU.add), reads=[G, route_b], writes=[G])
            S.add(DVE, lambda e: e.tensor_reduce(out=mx2, in_=lg2, axis=AX.X, op=ALU.max), reads=[G], writes=[G])
            S.add(DVE, lambda e: e.tensor_tensor(out=maskall[:, :, :], in0=lgall, in1=bc(mx2), op=ALU.is_ge), reads=[G, route_b], writes=[route_b])
            S.add(DVE, lambda e: e.tensor_tensor(out=exa, in0=lgall, in1=bc(mx1), op=ALU.subtract), reads=[G], writes=[G])
            S.add(ACT, lambda e: e.activation(out=exa, in_=exa, func=AF.Exp), reads=[G], writes=[G])
            S.add(DVE, lambda e: e.tensor_tensor(out=exa, in0=exa, in1=maskall[:, :, :], op=ALU.mult), reads=[G, route_b], writes=[G])
            S.add(DVE, lambda e: e.tensor_reduce(out=ssm, in_=exa, axis=AX.X, op=ALU.add), reads=[G], writes=[G])
            S.add(DVE, lambda e: e.reciprocal(out=ssm, in_=ssm), reads=[G], writes=[G])
            S.add(DVE, lambda e: e.tensor_tensor(out=gates[:, :, :], in0=exa, in1=bc(ssm), op=ALU.mult), reads=[G], writes=gates_b)
            S.add(DVE, lambda e: e.tensor_tensor(out=lg2, in0=gates[:, :, :], in1=m0all[:, :, :], op=ALU.mult), reads=gates_b + [route_b, G], writes=[G])
            S.add(DVE, lambda e: e.tensor_reduce(out=gsel[:, :, 0:1], in_=lg2, axis=AX.X, op=ALU.add), reads=[G], writes=[route_b])
            S.add(DVE, lambda e: e.tensor_tensor(out=lg2, in0=maskall[:, :, :], in1=m0all[:, :, :], op=ALU.subtract), reads=[G, route_b], writes=[G])
            S.add(DVE, lambda e: e.tensor_tensor(out=lg2, in0=lg2, in1=gates[:, :, :], op=ALU.mult), reads=gates_b + [G], writes=[G])
            S.add(DVE, lambda e: e.tensor_reduce(out=gsel[:, :, 1:2], in_=lg2, axis=AX.X, op=ALU.add), reads=[G], writes=[route_b])

        def routing_finalize():
            cnts = gbt[:, 0:128].rearrange("p (s e) -> p s e", e=NEXP)
            offs = gbt[:, 128:256].rearrange("p (s e) -> p s e", e=NEXP)
            posc = gbt[:, 256:384]
            eoff = gbt[:, 384:512]
            tmpm = gbt[:, 512:640]
            dstf = gbt[:, 640:672].rearrange("p (r s) -> p r s", s=16)
            nmx = gbt[:, 672:673]
            S.add(SP, lambda e: e.dma_start(out=eoff, in_=eoff_d), writes=[gb_b[0]], dma_key=("c", "eoff"))
            S.add(DVE, lambda e: e.tensor_copy(out=maskb[:], in_=maskall[:, :, :].rearrange("p s e -> p (s e)")), reads=[route_b], writes=[route_b])
            bk, bkb = ps_alloc()

            def mm(e):
                e.matmul(bk[:, 0:128], lhsT=ustb[:], rhs=maskb[:], start=True, stop=True)
                return e.matmul(bk[:, 128:256], lhsT=ones128b[:], rhs=maskb[:], start=True, stop=True)
            S.add(PE, mm, reads=[route_b, const_b], writes=[bkb])
            S.add(DVE, lambda e: e.tensor_copy(out=cnts.rearrange("p s e -> p (s e)"), in_=bk[:, 128:256]), reads=[bkb, gb_b[0]], writes=[gb_b[0]])
            S.add(DVE, lambda e: e.memset(offs[:, 0, :], 0.0), reads=[gb_b[0]], writes=[gb_b[0]])
            for q in range(1, 16):
                S.add(DVE, lambda e, q=q: e.tensor_tensor(out=offs[:, q, :], in0=offs[:, q - 1, :], in1=cnts[:, q - 1, :], op=ALU.add), reads=[gb_b[0]], writes=[gb_b[0]])
            S.add(DVE, lambda e: e.tensor_tensor(out=posc, in0=bk[:, 0:128], in1=offs.rearrange("p s e -> p (s e)"), op=ALU.add), reads=[bkb, gb_b[0]], writes=[gb_b[0]])
            S.add(DVE, lambda e: e.tensor_tensor(out=posc, in0=posc, in1=eoff, op=ALU.add), reads=[gb_b[0]], writes=[gb_b[0]])
            S.add(DVE, lambda e: e.tensor_tensor(out=tmpm, in0=posc, in1=m0all[:, :, :].rearrange("p s e -> p (s e)"), op=ALU.mult), reads=[gb_b[0], route_b], writes=[gb_b[0]])
            S.add(DVE, lambda e: e.tensor_reduce(out=dstf[:, 0, :], in_=tmpm.rearrange("p (s e) -> p s e", e=NEXP), axis=AX.X, op=ALU.add), reads=[gb_b[0]], writes=[gb_b[0]])
            S.add(DVE, lambda e: e.tensor_tensor(out=tmpm, in0=maskall[:, :, :].rearrange("p s e -> p (s e)"), in1=m0all[:, :, :].rearrange("p s e -> p (s e)"), op=ALU.subtract),
                  reads=[gb_b[0], route_b], writes=[gb_b[0]])
            S.add(DVE, lambda e: e.tensor_tensor(out=tmpm, in0=tmpm, in1=posc, op=ALU.mult), reads=[gb_b[0]], writes=[gb_b[0]])
            S.add(DVE, lambda e: e.tensor_reduce(out=dstf[:, 1, :], in_=tmpm.rearrange("p (s e) -> p s e", e=NEXP), axis=AX.X, op=ALU.add), reads=[gb_b[0]], writes=[gb_b[0]])
            S.add(DVE, lambda e: e.tensor_copy(out=desti[:, :, :], in_=dstf), reads=[gb_b[0]], writes=[route_b])
            S.add(DVE, lambda e: e.tensor_tensor(out=tmpm[:, 0:8], in0=offs[:, 15, :], in1=cnts[:, 15, :], op=ALU.add), reads=[gb_b[0]], writes=[gb_b[0]])
            S.add(DVE, lambda e: e.reduce_max(out=nmx, in_=tmpm[:, 0:8], axis=AX.X), reads=[gb_b[0]], writes=[gb_b[0]])
            thr = -1.0 if force_dense else float(CAP)
            S.add(DVE, lambda e: e.tensor_scalar(out=nmx, in0=nmx, scalar1=thr, scalar2=None, op0=ALU.is_gt), reads=[gb_b[0]], writes=[gb_b[0]])
            return S.add(DVE, lambda e: e.tensor_copy(out=flagi[:, :], in_=nmx), reads=[gb_b[0]], writes=[route_b])

        def moe_sparse():
            bar = S.last_ops()
            wg_b = fresh_bufs(2, bar)
            wu_b = fresh_bufs(2, bar)
            wd_b = fresh_bufs(2, bar)
            hT_b = [fresh_bufs(4, bar) for _ in range(2)]
            sg_b = fresh_bufs(2, bar)
            yacc_b = fresh_bufs(NSL, bar)
            xcT_b = fresh_bufs(2, bar)
            xcs_b = fresh_bufs(1, bar)[0]
            wgv = [mview(i * 24576, BF16, 4096).rearrange("p (k f) -> p k f", f=512) for i in range(2)]
            wuv = [mview(i * 24576 + 8192, BF16, 4096).rearrange("p (k f) -> p k f", f=512) for i in range(2)]
            wdv = [mview(i * 24576 + 16384, BF16, 4096).rearrange("p (c d) -> p c d", d=1024) for i in range(2)]
            hTv = [mview(49152 + i * 5120, BF16, 4 * CAP).rearrange("p (c t) -> p c t", t=CAP) for i in range(2)]
            yacc = mview(59392, F32, NSL * D).rearrange("p (j d) -> p j d", d=D)
            assert 79872 <= M_BYTES
            sgv = [sgg[:, i, :] for i in range(2)]
            xTf = xT[:, :, :].rearrange("p k t -> p (k t)")
            xcT = [xTf[:, i * 8 * CAP:(i + 1) * 8 * CAP].rearrange("p (k t) -> p k t", t=CAP) for i in range(2)]
            xcs = xTf[:, 16 * CAP:16 * CAP + NSL * D].rearrange("p (j d) -> p j d", d=D)
            assert 16 * CAP + NSL * D <= 8 * NT
            sc_bufs = []
            for s in range(1, 17):
                par = cnt["ln"] % 2
                cnt["ln"] += 1
                S.add(ACT, lambda e, s=s, par=par: e.activation(out=xb[:, par, :], in_=resid[:, s, :], func=AF.Identity, scale=1.0 / ALPHA),
                      reads=[resid_b[s]], writes=[xb_b[par]])
                for r in range(2):
                    b = fresh_bufs(1, bar)[0]
                    sc_bufs.append(b)
                    S.add(POOL, lambda e, s=s, par=par, r=r: e.indirect_dma_start(
                        out=xc_d, out_offset=bass.IndirectOffsetOnAxis(ap=desti[:, r, s - 1:s], axis=0), in_=xb[:, par, :], in_offset=None),
                        reads=[xb_b[par], route_b], writes=[b], dma_key=("xsc", par, r))
            ftiles = [(0, 512), (512, CAP - 512)]
            groups = []
            items = []
            for ex in range(NEXP):
                for f0 in range(0, FF_EXP // 128, 4):
                    gi = len(groups)
                    groups.append((ex, f0, gi % 2))
                    for ti_, tl in enumerate(ftiles):
                        items.append((gi % 2, tl, ex, f0, gi if ti_ == 0 else None, ti_ == len(ftiles) - 1))

            def load_group(gi):
                ex, f0, slot = groups[gi]
                wload(wgv[slot][:, :, :], o_wg[ex][:, f0 * 128:(f0 + 4) * 128].rearrange("(k p) f -> p k f", p=128), [wg_b[slot]], ("wg", slot))
                wload(wuv[slot][:, :, :], o_wu[ex][:, f0 * 128:(f0 + 4) * 128].rearrange("(k p) f -> p k f", p=128), [wu_b[slot]], ("wu", slot))
                wload(wdv[slot][:, :, :], o_wd[ex][f0 * 128:(f0 + 4) * 128, :].rearrange("(c p) d -> p c d", p=128), [wd_b[slot]], ("wd", slot))

            def load_tokens_dma(ex):
                S.add(SP, lambda e, ex=ex: e.dma_start(out=xcs, in_=xc_d[ex * CAP:(ex + 1) * CAP, :].rearrange("(j p) d -> p j d", p=128)),
                      reads=sc_bufs, writes=[xcs_b], dma_key="xcs")

            def load_tokens_tr(ex):
                xp = ex % 2
                for j in range(NSL):
                    def tr(e, j=j):
                        ins = None
                        for c in range(8):
                            ins = e.transpose(out=pT[:, c, :], in_=xcs[:, j, c * 128:(c + 1) * 128], identity=identb[:])
                        return ins
                    S.add(PE, tr, reads=[xcs_b, const_b], writes=[pT_b])
                    S.add(ACT, lambda e, j=j, xp=xp: e.copy(out=xcT[xp][:, :, j * 128:(j + 1) * 128], in_=pT[:, :, :]), reads=[pT_b], writes=[xcT_b[xp]])

            def step1(it, hp):
                slot, (c0, w), ex, f0, _g, _l = it
                xp = ex % 2
                for fc in range(4):
                    bg, bgb = ps_alloc()
                    bu, bub = ps_alloc()

                    def mm(e, bg=bg, bu=bu, fc=fc, slot=slot, c0=c0, w=w, xp=xp):
                        ins = None
                        for k in range(8):
                            ins = e.matmul(bg[:, 0:w], lhsT=wgv[slot][:, k, fc * 128:(fc + 1) * 128], rhs=xcT[xp][:, k, c0:c0 + w], start=(k == 0), stop=(k == 7))
                        for k in range(8):
                            ins = e.matmul(bu[:, 0:w], lhsT=wuv[slot][:, k, fc * 128:(fc + 1) * 128], rhs=xcT[xp][:, k, c0:c0 + w], start=(k == 0), stop=(k == 7))
                        return ins
                    S.add(PE, mm, reads=[wg_b[slot], wu_b[slot], xcT_b[xp]], writes=[bgb, bub])
                    sp = fc % 2
                    S.add(ACT, lambda e, bg=bg, w=w, sp=sp: e.activation(out=sgv[sp][:, 0:w], in_=bg[:, 0:w], func=AF.Silu), reads=[bgb], writes=[sg_b[sp]])
                    S.add(DVE, lambda e, bu=bu, w=w, sp=sp, fc=fc, hp=hp, c0=c0: e.tensor_tensor(out=hTv[hp][:, fc, c0:c0 + w], in0=bu[:, 0:w], in1=sgv[sp][:, 0:w], op=ALU.mult),
                          reads=[bub, sg_b[sp]], writes=[hT_b[hp][fc]])

            def step2(gi, hp):
                ex, f0, slot = groups[gi]
                first = (f0 == 0)
                last = (f0 + 4 == FF_EXP // 128)
                for j in range(NSL):
                    for half in range(2):
                        bk, bkb = ps_alloc()

                        def mm(e, bk=bk, j=j, half=half, slot=slot, hp=hp):
                            ins = None
                            for fc in range(4):
                                ins = e.matmul(bk[:, :], lhsT=hTv[hp][:, fc, j * 128:(j + 1) * 128], rhs=wdv[slot][:, fc, half * 512:(half + 1) * 512],
                                               start=(fc == 0), stop=(fc == 3))
                            return ins
                        S.add(PE, mm, reads=hT_b[hp] + [wd_b[slot]], writes=[bkb])
                        ya = yacc[:, j, half * 512:(half + 1) * 512]
                        if first:
                            S.add(ACT, lambda e, bk=bk, ya=ya: e.copy(out=ya, in_=bk[:, :]), reads=[bkb], writes=[yacc_b[j]])
                        else:
                            S.add(DVE, lambda e, bk=bk, ya=ya: e.tensor_tensor(out=ya, in0=ya, in1=bk[:, :], op=ALU.add), reads=[bkb, yacc_b[j]], writes=[yacc_b[j]])
                if last:
                    b = fresh_bufs(1, [])[0]
                    yst_bufs.append(b)
                    S.add(SP, lambda e, ex=ex: e.dma_start(out=yc_d[ex * CAP:(ex + 1) * CAP, :].rearrange("(j p) d -> p j d", p=128), in_=yacc[:, :, :]),
                          reads=yacc_b, writes=[b], dma_key="yst")

            yst_bufs = []
            load_group(0)
            load_tokens_dma(0)
            load_tokens_tr(0)
            pend = None
            for i, it in enumerate(items):
                slot, tl, ex, f0, gfirst, glast = it
                gi = ex * (FF_EXP // 512) + f0 // 4
                step1(it, gi % 2)
                if gfirst is not None:
                    if pend is not None:
                        step2(pend, pend % 2)
                        pend = None
                    if gi + 1 < len(groups):
                        load_group(gi + 1)
                    if f0 == 0 and ex + 1 < NEXP:
                        load_tokens_dma(ex + 1)
                    if f0 == 12 and ex + 1 < NEXP:
                        load_tokens_tr(ex + 1)
                if glast:
                    pend = gi
            step2(pend, pend % 2)
            for s in range(1, 17):
                for r in range(2):
                    j = (2 * s + r) % NSL
                    S.add(POOL, lambda e, s=s, r=r, j=j: e.indirect_dma_start(
                        out=yacc[:, j, :], out_offset=None, in_=yc_d, in_offset=bass.IndirectOffsetOnAxis(ap=desti[:, r, s - 1:s], axis=0)),
                        reads=yst_bufs + [route_b], writes=[yacc_b[j]], dma_key=("ygt", j))
                    S.add(DVE, lambda e, s=s, r=r, j=j: e.scalar_tensor_tensor(out=resid[:, s, :], in0=yacc[:, j, :], scalar=gsel[:, s - 1, r:r + 1], in1=resid[:, s, :],
                                                                               op0=ALU.mult, op1=ALU.add), reads=[yacc_b[j], resid_b[s], route_b], writes=[resid_b[s]])

        mixer_even(final=(n_sub == 1))
        if n_sub >= 2:
            load_gb(1, n_sub == 2)
            ffn([(e_wg, e_wu, e_wd, FF_DENSE, None)], with_halo=True, final=(n_sub == 2))
            bar_ffn0 = S.last_ops()
            ln_update_multi([0], n_sub == 2)
            for s in range(1, 17, 2):
                ln_update_multi([s, s + 1], n_sub == 2)
        if n_sub >= 3:
            mixer_odd(final=(n_sub == 3), bar=bar_ffn0)
        if n_sub >= 4:
            gating_all()
            flag_op = routing_finalize()
            load_gb(3, True)
            if moe_mode in ("both", "dense"):
                if moe_mode == "both":
                    S.begin_cond(flagi[0:1, 0:1], flag_op, True)
                ffn([(o_wg[e], o_wu[e], o_wd[e], FF_EXP, e) for e in range(NEXP)], with_halo=False, final=True)
                S.end_cond()
            if moe_mode in ("both", "sparse"):
                if moe_mode == "both":
                    S.begin_cond(flagi[0:1, 0:1], flag_op, False)
                moe_sparse()
                S.end_cond()
            for s in range(1, 17, 2):
                ln_update_multi([s, s + 1], True)
        S.emit(final_wait_ops=out_ops)
    return nc


def _make_in_maps(inp):
    f = lambda a: np.ascontiguousarray(np.asarray(a, dtype=np.float32))
    x = f(inp["x"])

    def pc(v):
        v = f(v)
        return v.reshape(-1, 128).T

    conv_a_w = f(inp["even_conv_a_w"])[0]
    cw = conv_a_w.T.reshape(4, 128, 31).transpose(1, 0, 2).reshape(128, 124)
    conv_c_w = f(inp["odd_conv_c_w"])[0]
    cc = conv_c_w.T.reshape(4, 128, 3).transpose(1, 0, 2).reshape(128, 12)
    cvec = np.concatenate([cw, pc(inp["even_conv_a_b"][0]), pc(inp["even_norm_a_g"][0]), pc(inp["even_norm_a_b"][0]),
                           pc(inp["even_pool_scale"][0]), cc], axis=1)
    cvec = np.ascontiguousarray(cvec, dtype=np.float32)
    assert cvec.shape == (128, 152)
    lnrows = np.stack([f(inp[k])[0] for k in ("even_ln1_g", "even_ln1_b", "even_ln2_g", "even_ln2_b", "odd_ln1_g", "odd_ln1_b", "odd_ln2_g", "odd_ln2_b")])
    sgrows = np.stack([f(inp["odd_sgu_norm_g"])[0], f(inp["odd_sgu_norm_b"])[0], f(inp["odd_sgu_b"])[0].reshape(512)])
    ident = np.eye(128, dtype=np.float32)
    tril = np.triu(np.ones((128, 128), dtype=np.float32))
    ustrict = np.triu(np.ones((128, 128), dtype=np.float32), k=1)
    eoff = np.ascontiguousarray(np.broadcast_to(np.tile(np.arange(NEXP, dtype=np.float32) * CAP, 16).reshape(1, 128), (128, 128)))
    common = {
        "e_w_in": f(inp["even_w_in"])[0], "e_w_out": f(inp["even_w_out"])[0],
        "e_wg": f(inp["even_ffn_w_gate"])[0], "e_wu": f(inp["even_ffn_w_up"])[0], "e_wd": f(inp["even_ffn_w_down"])[0],
        "o_w_in": f(inp["odd_w_in"])[0], "o_w_out": f(inp["odd_w_out"])[0], "o_router": f(inp["odd_router"])[0],
        "o_wg": f(inp["odd_moe_w_gate"])[0], "o_wu": f(inp["odd_moe_w_up"])[0], "o_wd": f(inp["odd_moe_w_down"])[0],
        "lnrows": np.ascontiguousarray(lnrows), "sgrows": np.ascontiguousarray(sgrows), "cvec": cvec,
        "pool_w": f(inp["even_pool_w"])[0], "sgu_w": f(inp["odd_sgu_w"])[0], "ident": ident, "tril": tril, "ustrict": ustrict, "eoff": eoff,
    }
    corr0 = np.ones((4, 16), dtype=np.float32)
    for g in range(4):
        win = 2 ** (g + 1)
        for t in range(16):
            corr0[g, t] = win / min(t + 1, win)
    maps = []
    for c in range(8):
        b, half = c // 2, c % 2
        xc = np.zeros((NT, D), dtype=np.float32)
        if half == 0:
            xc[HALO:] = x[b, 0:T]
            corr = corr0
            hm = 0.0
        else:
            xc[:] = x[b, T - HALO:2 * T]
            corr = np.ones((4, 16), dtype=np.float32)
            hm = 1.0
        m = dict(common)
        m["x_c"] = xc
        m["poolcorr"] = np.ascontiguousarray(np.broadcast_to(corr.reshape(1, 64), (128, 64)), dtype=np.float32)
        m["halomask"] = np.full((128, 1), hm, dtype=np.float32)
        maps.append(m)
    return maps


_NC_CACHE = {}


N_SUB = 4


def kernel(**inputs):
    maps = _make_in_maps(inputs)
    if "nc" not in _NC_CACHE:
        _NC_CACHE["nc"] = build_nc(N_SUB)
    res = run_bass_kernel_spmd(_NC_CACHE["nc"], maps, core_ids=list(range(8)))
    out = np.empty((4, 2 * T, D), dtype=np.float32)
    for c in range(8):
        b, half = c // 2, c % 2
        out[b, half * T:(half + 1) * T] = res.results[c]["out"]
    return out
```
